# Optimizing a Trainium2 kernel written in Bass

```python
import jax, jax.numpy as jnp
from jax import lax
import numpy as np

D_MODEL = 1024
BATCH = 4
SEQ = 8192
DEPTH = 2

GRID_W = 64
CTX_LEN = 256
EPS = 1e-6
CHUNK = 64
CONV_W = 4
CONV_LEFT = 2

GLA_HEADS = 4
GLA_DK = 64
GLA_DV = 128
GLA_KW = GLA_HEADS * GLA_DK
GLA_VW = GLA_HEADS * GLA_DV
GLA_RANK = 16
GLA_TAU = 16.0

RG_WIDTH = 512
RG_BLOCKS = 8
RG_BS = RG_WIDTH // RG_BLOCKS
RG_C = 8.0

AB_SIZES = (GLA_KW, GLA_KW, GLA_VW, GLA_VW, GLA_RANK, GLA_RANK, RG_WIDTH, RG_WIDTH)
AB_IN = sum(AB_SIZES)
AB_SPLITS = tuple(int(s) for s in np.cumsum(AB_SIZES)[:-1])
AB_MIX = GLA_VW + RG_WIDTH

M_INNER = 2 * D_MODEL
M_HEADS = 4
M_DH = M_INNER // M_HEADS
M_QKV_BS = 4
M_QKV_NB = M_INNER // M_QKV_BS

N_EXPERTS = 16
EXPERT_FF = D_MODEL
EC_CAPACITY = 2

kernel_name = 'hybrid_gla_rglru_mlstm_ec_moe_prefix'

f32 = jnp.float32


def rmsnorm(t, g):
    t32 = t.astype(f32)
    y = t32 * lax.rsqrt(jnp.mean(t32 * t32, axis=-1, keepdims=True) + EPS)
    return (y * g.astype(f32)).astype(t.dtype)


def head_norm(t, g, n_heads, center):
    B, L, W = t.shape
    th = t.astype(f32).reshape(B, L, n_heads, W // n_heads)
    if center:
        th = th - jnp.mean(th, axis=-1, keepdims=True)
    th = th * lax.rsqrt(jnp.mean(th * th, axis=-1, keepdims=True) + EPS)
    return th.reshape(B, L, W) * g.astype(f32)


def short_conv(t, w, b, n_seg, seg_len):
    B, L, C = t.shape
    ts = t.reshape(B, n_seg, seg_len, C)
    tp = jnp.pad(ts, ((0, 0), (0, 0), (CONV_LEFT, CONV_W - 1 - CONV_LEFT), (0, 0)))
    y = b + tp[:, :, 0:seg_len] * w[0]
    for j in range(1, CONV_W):
        y = y + tp[:, :, j:j + seg_len] * w[j]
    return y.reshape(B, L, C)


def block_diag(t, w):
    B, L, _ = t.shape
    nb, bi, bo = w.shape
    return jnp.einsum('blni,nio->blno', t.reshape(B, L, nb, bi), w).reshape(B, L, nb * bo)


def to_chunks(t):
    B, L = t.shape[:2]
    return jnp.moveaxis(t.reshape((B, L // CHUNK, CHUNK) + t.shape[2:]), 1, 0)


def from_chunks(t):
    n, B = t.shape[:2]
    return jnp.moveaxis(t, 0, 1).reshape((B, n * CHUNK) + t.shape[3:])


def gla_run(inputs, S0):
    causal = jnp.tril(jnp.ones((CHUNK, CHUNK), bool))

    def step(S, inp):
        qc, kc, vc, lac = inp
        b = jnp.cumsum(lac, axis=1)
        o_inter = jnp.einsum('bthk,bhkv->bthv', qc * jnp.exp(b), S)
        diff = b[:, :, None] - b[:, None]
        decay = jnp.exp(jnp.where(causal[None, :, :, None, None], diff, -jnp.inf))
        attn = jnp.einsum('bthk,bshk,btshk->btsh', qc, kc, decay)
        o = o_inter + jnp.einsum('btsh,bshv->bthv', attn, vc)
        b_last = b[:, -1]
        S_new = jnp.exp(b_last)[..., None] * S + jnp.einsum(
            'bshk,bshv->bhkv', kc * jnp.exp(b_last[:, None] - b), vc)
        return S_new, o

    S, o = lax.scan(step, S0, tuple(to_chunks(t) for t in inputs))
    return from_chunks(o), S


def lru_run(inputs, h0):
    a, u = inputs

    def comb(p, q):
        a1, u1 = p
        a2, u2 = q
        return a1 * a2, a2 * u1 + u2

    A, H = lax.associative_scan(comb, (a, u), axis=1)
    h = H + A * h0[:, None]
    return h, h[:, -1]


def mlstm_run(inputs, state):
    causal = jnp.tril(jnp.ones((CHUNK, CHUNK), bool))

    def step(carry, inp):
        C, nv, m = carry
        qc, kc, vc, lic, lfc = inp
        b = jnp.cumsum(lfc, axis=1)
        w_inter = b + m[:, None]
        w_intra = b[:, :, None] - b[:, None] + lic[:, None]
        w_intra = jnp.where(causal[None, :, :, None], w_intra, -jnp.inf)
        m_t = jnp.maximum(w_inter, jnp.max(w_intra, axis=2))
        d_inter = jnp.exp(w_inter - m_t)
        s = jnp.einsum('bthd,bshd->btsh', qc, kc) * jnp.exp(w_intra - m_t[:, :, None])
        num = d_inter[..., None] * jnp.einsum('bthk,bhkv->bthv', qc, C) + jnp.einsum('btsh,bshv->bthv', s, vc)
        den = d_inter * jnp.einsum('bthk,bhk->bth', qc, nv) + jnp.sum(s, axis=2)
        h = num / jnp.maximum(jnp.abs(den), jnp.exp(-m_t))[..., None]
        m_new = m_t[:, -1]
        g_inter = jnp.exp(b[:, -1] + m - m_new)
        kw = kc * jnp.exp(b[:, -1:] - b + lic - m_new[:, None])[..., None]
        C_new = g_inter[..., None, None] * C + jnp.einsum('bshk,bshv->bhkv', kw, vc)
        n_new = g_inter[..., None] * nv + jnp.sum(kw, axis=1)
        return (C_new, n_new, m_new), h

    state, h = lax.scan(step, state, tuple(to_chunks(t) for t in inputs))
    return from_chunks(h), state


def bidirectional_prefix_scan(run, ctx_f, lat_f, ctx_b, lat_b, state0, need_ctx):
    rev = lambda ts: tuple(jnp.flip(t, axis=1) for t in ts)
    out_cf, st_f = run(ctx_f, state0)
    out_lf, _ = run(lat_f, st_f)
    out_cb, st_b = run(rev(ctx_b), state0)
    out_lb, _ = run(rev(lat_b), st_b)
    lat = out_lf + jnp.flip(out_lb, axis=1)
    ctx = (out_cf + jnp.flip(out_cb, axis=1)) if need_ctx else None
    return ctx, lat


def gla_rglru_mixer(cn, xn, rows, need_ctx, w_in, gla_wa2, gla_ba, gla_norm_g, rg_conv_w, rg_conv_b,
                    rg_wr, rg_br, rg_wi, rg_bi, rg_lambda, w_out):
    def features(t, n_seg, seg_len):
        B, L, _ = t.shape
        q, k, v, g, lr_f, lr_b, rg_g, rg_x = jnp.split(t @ w_in, AB_SPLITS, axis=-1)
        q = (q.reshape(B, L, GLA_HEADS, GLA_DK) * GLA_DK ** -0.5).astype(f32)
        k = k.reshape(B, L, GLA_HEADS, GLA_DK).astype(f32)
        v = v.reshape(B, L, GLA_HEADS, GLA_DV).astype(f32)

        def log_decay(lr, d):
            z = (lr @ gla_wa2[d] + gla_ba[d]).astype(f32)
            return (jax.nn.log_sigmoid(z) / GLA_TAU).reshape(B, L, GLA_HEADS, GLA_DK)

        xb = short_conv(rg_x, rg_conv_w, rg_conv_b, n_seg, seg_len).astype(f32)

        def lru_inputs(d):
            r = jax.nn.sigmoid(block_diag(xb, rg_wr[d].astype(f32)) + rg_br[d])
            i = jax.nn.sigmoid(block_diag(xb, rg_wi[d].astype(f32)) + rg_bi[d])
            log_a = -RG_C * jax.nn.softplus(-rg_lambda[d].astype(f32)) * r
            return (jnp.exp(log_a), jnp.sqrt(-jnp.expm1(2.0 * log_a)) * (i * xb))

        return ((q, k, v, log_decay(lr_f, 0)), (q, k, v, log_decay(lr_b, 1)),
                lru_inputs(0), lru_inputs(1), g, rg_g)

    cf = features(cn, 1, cn.shape[1])
    lf = features(xn, rows, GRID_W)
    B = xn.shape[0]
    s0 = jnp.zeros((B, GLA_HEADS, GLA_DK, GLA_DV), f32)
    gla_c, gla_l = bidirectional_prefix_scan(gla_run, cf[0], lf[0], cf[1], lf[1], s0, need_ctx)
    h0 = jnp.zeros((B, RG_WIDTH), f32)
    lru_c, lru_l = bidirectional_prefix_scan(lru_run, cf[2], lf[2], cf[3], lf[3], h0, need_ctx)

    def merge(o_gla, h_lru, g, rg_g):
        B_, L = g.shape[:2]
        o = head_norm(o_gla.reshape(B_, L, GLA_VW), gla_norm_g, GLA_HEADS, False).astype(g.dtype) * jax.nn.silu(g)
        r = h_lru.astype(g.dtype) * jax.nn.gelu(rg_g)
        return jnp.concatenate([o, r], axis=-1) @ w_out

    y_lat = merge(gla_l, lru_l, lf[4], lf[5])
    y_ctx = merge(gla_c, lru_c, cf[4], cf[5]) if need_ctx else None
    return y_ctx, y_lat


def mlstm_mixer(cn, xn, rows, need_ctx, w_up, conv_w, conv_b, wq, wk, wv, w_gates, b_gates, skip, norm_g, w_down):
    def features(t, n_seg, seg_len):
        B, L, _ = t.shape
        xm, z = jnp.split(t @ w_up, 2, axis=-1)
        xc = jax.nn.silu(short_conv(xm, conv_w, conv_b, n_seg, seg_len))
        q = block_diag(xc, wq)
        k = block_diag(xc, wk) * M_DH ** -0.5
        v = block_diag(xm, wv)
        qkv = jnp.concatenate([q, k, v], axis=-1)
        heads = lambda a: a.reshape(B, L, M_HEADS, M_DH).astype(f32)

        def gates(d):
            gt = (qkv @ w_gates[d] + b_gates[d]).astype(f32)
            return gt[..., :M_HEADS], jax.nn.log_sigmoid(gt[..., M_HEADS:])

        qh, kh, vh = heads(q), heads(k), heads(v)
        li_f, lf_f = gates(0)
        li_b, lf_b = gates(1)
        return (qh, kh, vh, li_f, lf_f), (qh, kh, vh, li_b, lf_b), xc, z

    cf = features(cn, 1, cn.shape[1])
    lf = features(xn, rows, GRID_W)
    B = xn.shape[0]
    state0 = (jnp.zeros((B, M_HEADS, M_DH, M_DH), f32), jnp.zeros((B, M_HEADS, M_DH), f32),
              jnp.zeros((B, M_HEADS), f32))
    h_c, h_l = bidirectional_prefix_scan(mlstm_run, cf[0], lf[0], cf[1], lf[1], state0, need_ctx)

    def merge(h, xc, z):
        B_, L = z.shape[:2]
        hn = head_norm(h.reshape(B_, L, M_INNER), norm_g, M_HEADS, True).astype(z.dtype)
        return ((hn + skip * xc) * jax.nn.silu(z)) @ w_down

    y_lat = merge(h_l, lf[2], lf[3])
    y_ctx = merge(h_c, cf[2], cf[3]) if need_ctx else None
    return y_ctx, y_lat


def expert_choice_ffn(t, router_w, w_gate, w_up, w_down):
    B, L, D = t.shape
    cap = EC_CAPACITY * L // N_EXPERTS
    probs = jax.nn.softmax((t @ router_w).astype(f32), axis=-1)
    gate, idx = lax.top_k(jnp.swapaxes(probs, 1, 2), cap)
    xe = jax.vmap(lambda tb, ib: tb[ib])(t, idx)
    hdn = jax.nn.silu(jnp.einsum('becd,edf->becf', xe, w_gate)) * jnp.einsum('becd,edf->becf', xe, w_up)
    ye = jnp.einsum('becf,efd->becd', hdn, w_down) * gate[..., None].astype(t.dtype)
    return jax.vmap(lambda ib, yb: jnp.zeros((L, D), yb.dtype).at[ib.reshape(-1)].add(yb.reshape(-1, D)))(idx, ye)


def setup_inputs(seed: int = 0) -> dict:
    key = jax.random.key(seed)
    keys = jax.random.split(key, 40)
    D = D_MODEL
    NE = (DEPTH + 1) // 2
    NO = DEPTH // 2

    def nrm(i, shape, scale=1.0):
        return jax.random.normal(keys[i], shape, f32) * scale

    def gain(i, shape):
        return 1.0 + nrm(i, shape, 0.05)

    a_c = jax.random.uniform(keys[17], (NE, 2, RG_WIDTH), f32, 0.9, 0.999)
    a_base = a_c ** (1.0 / RG_C)
    rg_lambda = jnp.log(a_base) - jnp.log1p(-a_base)
    f_bias = jnp.broadcast_to(jnp.linspace(3.0, 6.0, M_HEADS, dtype=f32), (NO, 2, M_HEADS)) + nrm(27, (NO, 2, M_HEADS), 0.01)
    i_bias = nrm(28, (NO, 2, M_HEADS), 0.1)
    return {
        'x': nrm(0, (BATCH, SEQ, D)),
        'c': nrm(1, (BATCH, D)),
        'ctx': nrm(2, (BATCH, CTX_LEN, D)),
        'c_ctx': nrm(3, (D,)),
        'mod_w': nrm(4, (DEPTH, D, 6 * D), 0.5 * D ** -0.5),
        'mod_b': nrm(5, (DEPTH, 6 * D), 0.02),
        'norm1_g': gain(6, (DEPTH, D)),
        'norm2_g': gain(7, (DEPTH, D)),
        'ab_w_in': nrm(8, (NE, D, AB_IN), D ** -0.5),
        'gla_wa2': nrm(9, (NE, 2, GLA_RANK, GLA_KW), GLA_RANK ** -0.5),
        'gla_ba': nrm(10, (NE, 2, GLA_KW), 0.1),
        'gla_norm_g': gain(11, (NE, GLA_VW)),
        'rg_conv_w': nrm(12, (NE, CONV_W, RG_WIDTH), CONV_W ** -0.5),
        'rg_conv_b': nrm(13, (NE, RG_WIDTH), 0.01),
        'rg_wr': nrm(14, (NE, 2, RG_BLOCKS, RG_BS, RG_BS), RG_BS ** -0.5),
        'rg_br': nrm(15, (NE, 2, RG_WIDTH), 0.1),
        'rg_wi': nrm(16, (NE, 2, RG_BLOCKS, RG_BS, RG_BS), RG_BS ** -0.5),
        'rg_bi': nrm(18, (NE, 2, RG_WIDTH), 0.1),
        'rg_lambda': rg_lambda,
        'ab_w_out': nrm(19, (NE, AB_MIX, D), AB_MIX ** -0.5),
        'm_w_up': nrm(20, (NO, D, 2 * M_INNER), D ** -0.5),
        'm_conv_w': nrm(21, (NO, CONV_W, M_INNER), CONV_W ** -0.5),
        'm_conv_b': nrm(22, (NO, M_INNER), 0.01),
        'm_wq': nrm(23, (NO, M_QKV_NB, M_QKV_BS, M_QKV_BS), M_QKV_BS ** -0.5),
        'm_wk': nrm(24, (NO, M_QKV_NB, M_QKV_BS, M_QKV_BS), M_QKV_BS ** -0.5),
        'm_wv': nrm(25, (NO, M_QKV_NB, M_QKV_BS, M_QKV_BS), M_QKV_BS ** -0.5),
        'm_w_gates': nrm(26, (NO, 2, 3 * M_INNER, 2 * M_HEADS), 0.1 * (3 * M_INNER) ** -0.5),
        'm_b_gates': jnp.concatenate([i_bias, f_bias], axis=-1),
        'm_skip': gain(29, (NO, M_INNER)),
        'm_norm_g': gain(30, (NO, M_INNER)),
        'm_w_down': nrm(31, (NO, M_INNER, D), M_INNER ** -0.5),
        'router_w': nrm(32, (DEPTH, D, N_EXPERTS), D ** -0.5),
        'exp_w_gate': nrm(33, (DEPTH, N_EXPERTS, D, EXPERT_FF), D ** -0.5),
        'exp_w_up': nrm(34, (DEPTH, N_EXPERTS, D, EXPERT_FF), D ** -0.5),
        'exp_w_down': nrm(35, (DEPTH, N_EXPERTS, EXPERT_FF, D), EXPERT_FF ** -0.5),
        'final_g': gain(36, (D,)),
    }


def reference(x, c, ctx, c_ctx, mod_w, mod_b, norm1_g, norm2_g, ab_w_in, gla_wa2, gla_ba, gla_norm_g,
              rg_conv_w, rg_conv_b, rg_wr, rg_br, rg_wi, rg_bi, rg_lambda, ab_w_out, m_w_up, m_conv_w,
              m_conv_b, m_wq, m_wk, m_wv, m_w_gates, m_b_gates, m_skip, m_norm_g, m_w_down, router_w,
              exp_w_gate, exp_w_up, exp_w_down, final_g):
    rows = x.shape[1] // GRID_W
    for layer in range(DEPTH):
        need_ctx = layer < DEPTH - 1
        mods = jnp.split(jax.nn.silu(c) @ mod_w[layer] + mod_b[layer], 6, axis=-1)
        sh1, sc1, g1, sh2, sc2, g2 = [m[:, None, :] for m in mods]
        csh1, csc1, cg1, csh2, csc2, cg2 = jnp.split(jax.nn.silu(c_ctx) @ mod_w[layer] + mod_b[layer], 6, axis=-1)
        xn = rmsnorm(x, norm1_g[layer]) * (1 + sc1) + sh1
        cn = rmsnorm(ctx, norm1_g[layer]) * (1 + csc1) + csh1
        if layer % 2 == 0:
            e = layer // 2
            y_ctx, y_lat = gla_rglru_mixer(cn, xn, rows, need_ctx, ab_w_in[e], gla_wa2[e], gla_ba[e], gla_norm_g[e],
                                           rg_conv_w[e], rg_conv_b[e], rg_wr[e], rg_br[e], rg_wi[e], rg_bi[e],
                                           rg_lambda[e], ab_w_out[e])
        else:
            o = layer // 2
            y_ctx, y_lat = mlstm_mixer(cn, xn, rows, need_ctx, m_w_up[o], m_conv_w[o], m_conv_b[o], m_wq[o], m_wk[o],
                                       m_wv[o], m_w_gates[o], m_b_gates[o], m_skip[o], m_norm_g[o], m_w_down[o])
        x = x + g1 * y_lat
        x = x + g2 * expert_choice_ffn(rmsnorm(x, norm2_g[layer]) * (1 + sc2) + sh2, router_w[layer],
                                       exp_w_gate[layer], exp_w_up[layer], exp_w_down[layer])
        if need_ctx:
            ctx = ctx + cg1 * y_ctx
            ctx = ctx + cg2 * expert_choice_ffn(rmsnorm(ctx, norm2_g[layer]) * (1 + csc2) + csh2, router_w[layer],
                                                exp_w_gate[layer], exp_w_up[layer], exp_w_down[layer])
    return rmsnorm(x, final_g)
```

```python
import numpy as np
from contextlib import ExitStack
import concourse.bass as bass
import concourse.mybir as mybir
from concourse.bass_utils import run_bass_kernel_spmd

F32 = mybir.dt.float32
BF16 = mybir.dt.bfloat16
I32 = mybir.dt.int32
U32 = mybir.dt.uint32
AF = mybir.ActivationFunctionType
ALU = mybir.AluOpType
AX = mybir.AxisListType


class Tok:
    __slots__ = ("w", "r", "name")

    def __init__(self, name=""):
        self.w = None
        self.r = []
        self.name = name


class Tile:
    def __init__(self, t, name):
        self.t = t
        self.name = name
        self.toks = {}

    def tok(self, key=None):
        k = self.toks.get(key)
        if k is None:
            k = self.toks[key] = Tok(f"{self.name}:{key}")
        return k

    def __getitem__(self, idx):
        return self.t[idx]


class Sched:
    ENGS = ("pe", "dve", "act", "pool", "sp")
    EPOCH = 1500

    def __init__(self, nc, stack):
        self.nc = nc
        self.stack = stack
        self.ops = {e: [] for e in self.ops_engines()}
        self.count = {e: 0 for e in self.ops_engines()}
        self.sems = {}
        self.dma_count = {}
        self.waited = {e: {} for e in self.ops_engines()}
        self.n_sem = 0
        self.final = []

    def ops_engines(self):
        return self.ENGS

    def sbuf(self, name, shape, dt):
        t = self.stack.enter_context(self.nc.sbuf_tensor("sb_" + name, list(shape), dt))
        return Tile(t, name)

    def psum(self, name, shape, dt):
        t = self.stack.enter_context(self.nc.psum_tensor("ps_" + name, list(shape), dt))
        return Tile(t, name)

    def dsem(self, name):
        key = ("dma", name)
        if key not in self.sems:
            self.sems[key] = self.stack.enter_context(self.nc.semaphore(f"d_{name}"))
            self.dma_count[key] = 0
        return key

    def _deps(self, reads, writes):
        deps = []
        for t in reads:
            if t.w is not None:
                deps.append(t.w)
        for t in writes:
            if t.w is not None:
                deps.append(t.w)
            deps.extend(t.r)
        return deps

    def _commit(self, comp, reads, writes):
        for t in writes:
            t.w = comp
            t.r = []
        for t in reads:
            if t not in writes:
                t.r.append(comp)

    def op(self, eng, fn, reads=(), writes=()):
        reads = [r for r in reads]
        writes = [w for w in writes]
        deps = self._deps(reads, writes)
        self.count[eng] += 1
        ep = (self.count[eng] - 1) // self.EPOCH
        key = ("eng", eng, ep)
        if key not in self.sems:
            self.sems[key] = self.stack.enter_context(self.nc.semaphore(f"s_{eng}_{ep}"))
        comp = (key, self.count[eng] - ep * self.EPOCH)
        waits = self._filter_waits(eng, deps, own=None, own_val=None)
        self.ops[eng].append((waits, fn, comp))
        self._commit(comp, reads, writes)
        return comp

    def dma(self, eng, fn, semname, reads=(), writes=()):
        key = self.dsem(semname)
        deps = self._deps(list(reads), list(writes))
        self.dma_count[key] += 16
        comp = (key, self.dma_count[key])
        waits = self._filter_waits(eng, deps, own=None, own_val=None)
        self.ops[eng].append((waits, fn, comp))
        self._commit(comp, list(reads), list(writes))
        return comp

    def _filter_waits(self, eng, deps, own, own_val):
        need = {}
        for (k, v) in deps:
            if need.get(k, -1) < v:
                need[k] = v
        out = []
        wd = self.waited[eng]
        for k, v in need.items():
            if wd.get(k, -1) >= v:
                continue
            wd[k] = v
            out.append((k, v))
        return out

    def wait_all_final(self, eng, comps):
        waits = self._filter_waits(eng, comps, None, None)
        self.ops[eng].append((waits, None, None))

    def emit(self):
        nc = self.nc
        sems = self.sems
        ops = self.ops
        with nc.Block() as block:
            def mk(e):
                def body(engine):
                    for (waits, fn, comp) in ops[e]:
                        for (k, v) in waits:
                            engine.wait_ge(sems[k], v)
                        if fn is None:
                            continue
                        ins = fn(engine)
                        if comp[0][0] == "eng":
                            ins.then_inc(sems[comp[0]], 1)
                        else:
                            ins.then_inc(sems[comp[0]], 16)
                return body
            block.tensor(mk("pe"))
            block.vector(mk("dve"))
            block.scalar(mk("act"))
            block.gpsimd(mk("pool"))
            block.sync(mk("sp"))


D = 1024
NCTX = 256
NLAT = 8192
NSEQ = NCTX + NLAT
EPS = 1e-6
BLK = 256
DEBUG_NBLK = 0


def col_layout(v):
    v = np.asarray(v, np.float32)
    return np.ascontiguousarray(v.reshape(-1, 128).T)


class PsumPool:
    def __init__(self, S, names, shape=(128, 512), dt=F32):
        self.tiles = [S.psum(nm, list(shape), dt) for nm in names]
        self.i = 0

    def next(self):
        t = self.tiles[self.i % len(self.tiles)]
        self.i += 1
        return t


def load_const(S, dram_ap, tile, name, eng="sp"):
    S.dma(eng, lambda e: e.dma_start(out=tile[:], in_=dram_ap), name, writes=[tile.tok()])


def norm_fm(S, X, n, gs, sh, mi, XN, W):
    sq, rstd, sd, tmpn, ones32, pN = W["sq"], W["rstd"], W["sd"], W["tmpn"], W["ones32"], W["pool"].next()
    S.op("act", lambda e: e.activation(out=sq[:, :, :n], in_=X[:, :, :n], func=AF.Square),
         reads=[X.tok()], writes=[sq.tok()])
    for c in range(8):
        S.op("pe", lambda e, c=c: e.matmul(pN[:, :n], lhsT=ones32[:, :], rhs=sq[:, c, :n], start=(c == 0), stop=(c == 7)),
             reads=[ones32.tok(), sq.tok()], writes=[pN.tok()])
    S.op("act", lambda e: e.activation(out=sd[:, :n], in_=pN[:, :n], func=AF.Sqrt, bias=W["epsc"][:, 0:1], scale=1.0 / D),
         reads=[pN.tok(), W["epsc"].tok()], writes=[sd.tok()])
    S.op("dve", lambda e: e.reciprocal(out=rstd[:, :n], in_=sd[:, :n]), reads=[sd.tok()], writes=[rstd.tok()])
    S.op("pool", lambda e: e.tensor_tensor(out=tmpn[:, :, :n], in0=X[:, :, :n],
                                           in1=rstd[:, :n].unsqueeze(1).to_broadcast([128, 8, n]), op=ALU.mult),
         reads=[X.tok(), rstd.tok()], writes=[tmpn.tok()])
    for c in range(8):
        S.op("act", lambda e, c=c: e.activation(out=XN[:, c, :n], in_=tmpn[:, c, :n], func=AF.Identity,
                                                bias=sh[:, mi, c:c + 1], scale=gs[:, mi, c:c + 1]),
             reads=[tmpn.tok(), gs.tok(), sh.tok()], writes=[XN.tok()])


def norm_work(S, pool, ones32, epsc):
    tmpn = S.sbuf("n_tmp", [128, 8, BLK], F32)
    return dict(sq=tmpn, rstd=S.sbuf("n_rstd", [128, BLK], F32),
                sd=S.sbuf("n_sd", [128, BLK], F32), tmpn=tmpn,
                ones32=ones32, pool=pool, epsc=epsc)


def make_gs(S, modv, gs, nsets):
    for i in range(nsets):
        S.op("dve", lambda e, i=i: e.scalar_tensor_tensor(out=gs[:, i, :], in0=modv[:, 1 + 2 * i, :], scalar=1.0,
                                                          in1=modv[:, 0, :], op0=ALU.add, op1=ALU.mult),
             reads=[modv.tok()], writes=[gs.tok()])


def load_cast_weight(S, w_dram, ncols, wb, st, stage_name, chunk=BLK, kchunks=8):
    wv = w_dram.rearrange("(c p) n -> p c n", p=128)
    i = 0
    for c0 in range(0, ncols, chunk):
        cw = min(chunk, ncols - c0)
        s = st[i % 2]
        S.dma("sp", lambda e, s=s, c0=c0, cw=cw: e.dma_start(out=s[:, :, :cw], in_=wv[:, :, c0:c0 + cw]),
              f"{stage_name}{i % 2}", writes=[s.tok()])
        eng = "dve" if i % 2 == 0 else "pool"
        S.op(eng, lambda e, s=s, c0=c0, cw=cw: e.tensor_copy(out=wb[:, :, c0:c0 + cw], in_=s[:, :, :cw]),
             reads=[s.tok()], writes=[wb.tok()])
        i += 1


P1_WCOLS = 1552


def seq_blocks():
    blocks = [(0, NCTX, 1)]
    for i in range(NLAT // BLK):
        blocks.append((NCTX + i * BLK, BLK, 0))
    if DEBUG_NBLK:
        blocks = blocks[:DEBUG_NBLK]
    return blocks


def build_p1():
    nc = bass.Bass("TRN2", target_bir_lowering=False)
    dt = lambda name, shape, kind="ExternalInput", d=F32: nc.dram_tensor(name, list(shape), d, kind=kind).ap()
    xT = dt("xT", [D, NSEQ])
    w1 = dt("w1", [D, P1_WCOLS])
    modv_d = dt("modv", [128, 5, 8])
    wa2_d = dt("wa2", [16, 256])
    ba_d = dt("ba", [64, 4])
    lruc_d = dt("lruc", [128, 4, 9])
    wbd_d = dt("wbd", [128, 2, 4, 128])
    ones_d = dt("ones", [128, 128])
    mask_d = dt("mask4", [64, 256])
    ident_d = dt("ident", [64, 64])
    rmask_d = dt("rmask", [128, BLK])
    og = dt("og", [NSEQ, 512], kind="ExternalOutput")
    hlT = dt("hlT", [512, NSEQ], kind="ExternalOutput")
    with ExitStack() as st:
        S = Sched(nc, st)
        pool = PsumPool(S, ["pp0", "pp1", "pp2"])
        pAT = S.psum("pAT", [128, 512], F32)
        pO = S.psum("pO", [128, 512], F32)
        pU = S.psum("pU", [128, 512], F32)
        pTR = S.psum("pTR", [64, 4, 256], BF16)
        ones32 = S.sbuf("ones32", [128, 128], F32); load_const(S, ones_d, ones32, "c_ones")
        mask4 = S.sbuf("mask4", [64, 256], F32); load_const(S, mask_d, mask4, "c_mask")
        ident32 = S.sbuf("ident32", [64, 64], F32); load_const(S, ident_d, ident32, "c_ident")
        rmask = S.sbuf("rmask", [128, BLK], F32); load_const(S, rmask_d, rmask, "c_rmask")
        modv = S.sbuf("modv", [128, 5, 8], F32); load_const(S, modv_d, modv, "c_modv")
        wa2 = S.sbuf("wa2", [16, 256], F32); load_const(S, wa2_d, wa2, "c_wa2")
        ba = S.sbuf("ba", [64, 4], F32); load_const(S, ba_d, ba, "c_ba")
        lruc = S.sbuf("lruc", [128, 4, 9], F32); load_const(S, lruc_d, lruc, "c_lruc")
        wbd32 = S.sbuf("wbd32", [128, 2, 4, 128], F32); load_const(S, wbd_d, wbd32, "c_wbd")
        identb = S.sbuf("identb", [64, 64], BF16)
        S.op("dve", lambda e: e.tensor_copy(out=identb[:], in_=ident32[:]), reads=[ident32.tok()], writes=[identb.tok()])
        wbdb = S.sbuf("wbdb", [128, 2, 4, 128], BF16)
        S.op("dve", lambda e: e.tensor_copy(out=wbdb[:], in_=wbd32[:]), reads=[wbd32.tok()], writes=[wbdb.tok()])
        nba = S.sbuf("nba", [64, 4], F32)
        S.op("dve", lambda e: e.tensor_scalar(out=nba[:], in0=ba[:], scalar1=-1.0, scalar2=None, op0=ALU.mult),
             reads=[ba.tok()], writes=[nba.tok()])
        epsc = S.sbuf("epsc", [128, 1], F32)
        S.op("dve", lambda e: e.memset(epsc[:], EPS), writes=[epsc.tok()])
        onec = S.sbuf("onec", [128, 1], F32)
        S.op("dve", lambda e: e.memset(onec[:], 1.0), writes=[onec.tok()])
        gs = S.sbuf("gs", [128, 2, 8], F32)
        make_gs(S, modv, gs, 2)
        sh = S.sbuf("sh", [128, 2, 8], F32)
        for i in range(2):
            S.op("dve", lambda e, i=i: e.tensor_copy(out=sh[:, i, :], in_=modv[:, 2 + 2 * i, :]), reads=[modv.tok()], writes=[sh.tok()])
        clam = S.sbuf("clam", [128, 4], F32)
        ctmp = S.sbuf("ctmp", [128, 4], F32)
        S.op("act", lambda e: e.activation(out=ctmp[:], in_=lruc[:, :, 8], func=AF.Exp, scale=-1.0), reads=[lruc.tok()], writes=[ctmp.tok()])
        S.op("act", lambda e: e.activation(out=ctmp[:], in_=ctmp[:], func=AF.Ln, bias=onec[:, 0:1], scale=1.0),
             reads=[ctmp.tok(), onec.tok()], writes=[ctmp.tok()])
        S.op("dve", lambda e: e.tensor_scalar(out=clam[:], in0=ctmp[:], scalar1=-8.0, scalar2=None, op0=ALU.mult),
             reads=[ctmp.tok()], writes=[clam.tok()])
        wb = S.sbuf("wb", [128, 8, P1_WCOLS], BF16)
        X = [S.sbuf(f"X{i}", [128, 8, BLK], F32) for i in range(2)]
        load_cast_weight(S, w1, P1_WCOLS, wb, X, "X")
        NW = norm_work(S, pool, ones32, epsc)
        XN = [S.sbuf(f"XN{i}", [128, 8, BLK], BF16) for i in range(2)]
        q32 = S.sbuf("q32", [64, 4, BLK], F32); k32 = S.sbuf("k32", [64, 4, BLK], F32)
        lr32 = S.sbuf("lr32", [16, BLK], F32)
        e1 = S.sbuf("e1", [64, 4, BLK], F32); sp = e1; csp = S.sbuf("csp", [64, 4, BLK], F32)
        eb = S.sbuf("eb", [64, 4, BLK], F32); enb = S.sbuf("enb", [64, 4, BLK], F32)
        dec = [S.sbuf(f"dec{i}", [64, 4, 4], F32) for i in range(2)]
        qt = [S.sbuf(f"qt{i}", [64, 4, BLK], BF16) for i in range(2)]
        kt = [S.sbuf(f"kt{i}", [64, 4, BLK], BF16) for i in range(2)]
        kh = [S.sbuf(f"kh{i}", [64, 4, BLK], BF16) for i in range(2)]
        vtok = [S.sbuf(f"vtok{i}", [64, 4, 512], BF16) for i in range(2)]
        khtok = [S.sbuf(f"khtok{i}", [64, 4, 256], BF16) for i in range(2)]
        xr = S.sbuf("xr", [128, 4, BLK], F32); xb = S.sbuf("xb", [128, 4, BLK], F32); xbb = S.sbuf("xbb", [128, 4, BLK], BF16)
        rr = S.sbuf("rr", [128, 4, BLK], F32); ii = S.sbuf("ii", [128, 4, BLK], F32)
        aa = S.sbuf("aa", [128, 4, BLK], F32); a2 = rr; uu = ii
        hl = [S.sbuf(f"hl{i}", [128, 4, BLK], F32) for i in range(2)]
        ost = [S.sbuf(f"ost{i}", [64, 4, 512], F32) for i in range(2)]
        ATs = S.sbuf("ATs", [64, 256], BF16)
        S32 = S.sbuf("S32", [64, 512], F32); Sb = S.sbuf("Sb", [64, 512], BF16)
        S.op("dve", lambda e: e.memset(S32[:], 0.0), writes=[S32.tok()])
        S.op("pool", lambda e: e.memset(Sb[:], 0.0), writes=[Sb.tok()])
        xTv = xT.rearrange("(c p) n -> p c n", p=128)
        hlv = hlT.rearrange("(c p) n -> p c n", p=128)
        outs = []
        prev_hl = None
        def do_block(bi, t0, n, mi, prev_hl):
            b2 = bi % 2
            nch = n // 64
            seg = 256 if mi == 1 else 64
            Xb, XNb = X[b2], XN[b2]
            S.dma("sp", lambda e, Xb=Xb, t0=t0, n=n: e.dma_start(out=Xb[:, :, :n], in_=xTv[:, :, t0:t0 + n]), f"X{b2}", writes=[Xb.tok()])
            norm_fm(S, Xb, n, gs, sh, mi, XNb, NW)
            for h in range(4):
                p = pool.next()
                for c in range(8):
                    S.op("pe", lambda e, p=p, c=c, h=h: e.matmul(p[0:64, :n], lhsT=wb[:, c, h * 64:(h + 1) * 64], rhs=XNb[:, c, :n], start=(c == 0), stop=(c == 7)),
                         reads=[wb.tok(), XNb.tok()], writes=[p.tok()])
                S.op("dve", lambda e, p=p, h=h: e.tensor_scalar(out=q32[:, h, :n], in0=p[0:64, :n], scalar1=0.125, scalar2=None, op0=ALU.mult),
                     reads=[p.tok()], writes=[q32.tok()])
            for h in range(4):
                p = pool.next()
                for c in range(8):
                    S.op("pe", lambda e, p=p, c=c, h=h: e.matmul(p[0:64, :n], lhsT=wb[:, c, 256 + h * 64:256 + (h + 1) * 64], rhs=XNb[:, c, :n], start=(c == 0), stop=(c == 7)),
                         reads=[wb.tok(), XNb.tok()], writes=[p.tok()])
                S.op("act", lambda e, p=p, h=h: e.activation(out=k32[:, h, :n], in_=p[0:64, :n], func=AF.Copy),
                     reads=[p.tok()], writes=[k32.tok()])
            p = pool.next()
            for c in range(8):
                S.op("pe", lambda e, p=p, c=c: e.matmul(p[0:16, :n], lhsT=wb[:, c, 1024:1040], rhs=XNb[:, c, :n], start=(c == 0), stop=(c == 7)),
                     reads=[wb.tok(), XNb.tok()], writes=[p.tok()])
            S.op("dve", lambda e, p=p: e.tensor_copy(out=lr32[:, :n], in_=p[0:16, :n]), reads=[p.tok()], writes=[lr32.tok()])
            for h in range(4):
                p = pool.next()
                S.op("pe", lambda e, p=p, h=h: e.matmul(p[0:64, :n], lhsT=wa2[:, h * 64:(h + 1) * 64], rhs=lr32[:, :n], start=True, stop=True),
                     reads=[wa2.tok(), lr32.tok()], writes=[p.tok()])
                S.op("act", lambda e, p=p, h=h: e.activation(out=e1[:, h, :n], in_=p[0:64, :n], func=AF.Exp, bias=nba[:, h:h + 1], scale=-1.0),
                     reads=[p.tok(), nba.tok()], writes=[e1.tok()])
            S.op("act", lambda e: e.activation(out=sp[:, :, :n], in_=e1[:, :, :n], func=AF.Ln, bias=onec[0:64, 0:1], scale=1.0),
                 reads=[e1.tok(), onec.tok()], writes=[sp.tok()])
            for h in range(4):
                S.op("dve", lambda e, h=h: e.tensor_tensor_scan(out=csp[:, h, :n], data0=rmask[0:64, :n], data1=sp[:, h, :n], initial=0.0, op0=ALU.mult, op1=ALU.add),
                     reads=[rmask.tok(), sp.tok()], writes=[csp.tok()])
            S.op("act", lambda e: e.activation(out=eb[:, :, :n], in_=csp[:, :, :n], func=AF.Exp, scale=-1.0 / 16), reads=[csp.tok()], writes=[eb.tok()])
            S.op("act", lambda e: e.activation(out=enb[:, :, :n], in_=csp[:, :, :n], func=AF.Exp, scale=1.0 / 16), reads=[csp.tok()], writes=[enb.tok()])
            decb = dec[b2]
            for h in range(4):
                S.op("dve", lambda e, h=h: e.tensor_copy(out=decb[:, h, :nch], in_=eb[:, h, :n].rearrange("p (c t) -> p c t", t=64)[:, :, 63]),
                     reads=[eb.tok()], writes=[decb.tok()])
            qtb, ktb, khb = qt[b2], kt[b2], kh[b2]
            S.op("dve", lambda e: e.tensor_tensor(out=qtb[:, :, :n], in0=q32[:, :, :n], in1=eb[:, :, :n], op=ALU.mult), reads=[q32.tok(), eb.tok()], writes=[qtb.tok()])
            S.op("pool", lambda e: e.tensor_tensor(out=ktb[:, :, :n], in0=k32[:, :, :n], in1=enb[:, :, :n], op=ALU.mult), reads=[k32.tok(), enb.tok()], writes=[ktb.tok()])
            for h in range(4):
                S.op("dve", lambda e, h=h: e.tensor_tensor(out=khb[:, h, :n].rearrange("p (c t) -> p c t", t=64),
                                                            in0=ktb[:, h, :n].rearrange("p (c t) -> p c t", t=64),
                                                            in1=decb[:, h, :nch].unsqueeze(2).to_broadcast([64, nch, 64]), op=ALU.mult),
                     reads=[ktb.tok(), decb.tok()], writes=[khb.tok()])
            vb = vtok[b2]
            for c in range(nch):
                p = pool.next()
                for kc in range(8):
                    S.op("pe", lambda e, p=p, kc=kc, c=c: e.matmul(p[0:64, :], lhsT=XNb[:, kc, c * 64:(c + 1) * 64], rhs=wb[:, kc, 1040:1552], start=(kc == 0), stop=(kc == 7)),
                         reads=[wb.tok(), XNb.tok()], writes=[p.tok()])
                S.op("act", lambda e, p=p, c=c: e.activation(out=vb[:, c, :], in_=p[0:64, :], func=AF.Copy), reads=[p.tok()], writes=[vb.tok(c)])
            khtb = khtok[b2]
            for c in range(nch):
                r = c % 4
                for h in range(4):
                    S.op("pe", lambda e, r=r, h=h, c=c: e.transpose(out=pTR[:, r, h * 64:(h + 1) * 64], in_=khb[:, h, c * 64:(c + 1) * 64], identity=identb[:]),
                         reads=[khb.tok(), identb.tok()], writes=[pTR.tok()])
                S.op("dve", lambda e, r=r, c=c: e.tensor_copy(out=khtb[:, c, :], in_=pTR[:, r, :]), reads=[pTR.tok()], writes=[khtb.tok(c)])
            for c4 in range(4):
                p = pool.next()
                for kc in range(8):
                    S.op("pe", lambda e, p=p, kc=kc, c4=c4: e.matmul(p[:, :n], lhsT=wb[:, kc, 512 + c4 * 128:512 + (c4 + 1) * 128], rhs=XNb[:, kc, :n], start=(kc == 0), stop=(kc == 7)),
                         reads=[wb.tok(), XNb.tok()], writes=[p.tok()])
                S.op("act", lambda e, p=p, c4=c4: e.activation(out=xr[:, c4, :n], in_=p[:, :n], func=AF.Copy), reads=[p.tok()], writes=[xr.tok()])
            for c4 in range(4):
                S.op("pool", lambda e, c4=c4: e.tensor_scalar(out=xb[:, c4, :n], in0=xr[:, c4, :n], scalar1=lruc[:, c4, 2:3], scalar2=lruc[:, c4, 5:6], op0=ALU.mult, op1=ALU.add),
                     reads=[xr.tok(), lruc.tok()], writes=[xb.tok()])
                for off in (-2, -1, 1, 2):
                    j = off + 2
                    xrv = xr[:, c4, :n].rearrange("p (s t) -> p s t", t=seg)
                    xbv = xb[:, c4, :n].rearrange("p (s t) -> p s t", t=seg)
                    if off < 0:
                        o_sl, i_sl = xbv[:, :, -off:seg], xrv[:, :, 0:seg + off]
                    else:
                        o_sl, i_sl = xbv[:, :, 0:seg - off], xrv[:, :, off:seg]
                    S.op("dve", lambda e, o_sl=o_sl, i_sl=i_sl, c4=c4, j=j: e.scalar_tensor_tensor(out=o_sl, in0=i_sl, scalar=lruc[:, c4, j:j + 1], in1=o_sl, op0=ALU.mult, op1=ALU.add),
                         reads=[xr.tok(), lruc.tok(), xb.tok()], writes=[xb.tok()])
            S.op("pool", lambda e: e.tensor_copy(out=xbb[:, :, :n], in_=xb[:, :, :n]), reads=[xb.tok()], writes=[xbb.tok()])
            for c4 in range(4):
                for (j, dst, bcol) in ((0, rr, 6), (1, ii, 7)):
                    p = pool.next()
                    S.op("pe", lambda e, p=p, j=j, c4=c4: e.matmul(p[:, :n], lhsT=wbdb[:, j, c4, :], rhs=xbb[:, c4, :n], start=True, stop=True),
                         reads=[wbdb.tok(), xbb.tok()], writes=[p.tok()])
                    S.op("act", lambda e, p=p, dst=dst, c4=c4, bcol=bcol: e.activation(out=dst[:, c4, :n], in_=p[:, :n], func=AF.Sigmoid, bias=lruc[:, c4, bcol:bcol + 1], scale=1.0),
                         reads=[p.tok(), lruc.tok()], writes=[dst.tok()])
            for c4 in range(4):
                S.op("act", lambda e, c4=c4: e.activation(out=aa[:, c4, :n], in_=rr[:, c4, :n], func=AF.Exp, scale=clam[:, c4:c4 + 1]),
                     reads=[rr.tok(), clam.tok()], writes=[aa.tok()])
            S.op("pool", lambda e: e.tensor_tensor(out=a2[:, :, :n], in0=aa[:, :, :n], in1=aa[:, :, :n], op=ALU.mult), reads=[aa.tok()], writes=[a2.tok()])
            S.op("act", lambda e: e.activation(out=a2[:, :, :n], in_=a2[:, :, :n], func=AF.Sqrt, bias=onec[:, 0:1], scale=-1.0),
                 reads=[a2.tok(), onec.tok()], writes=[a2.tok()])
            S.op("pool", lambda e: e.tensor_tensor(out=uu[:, :, :n], in0=a2[:, :, :n], in1=ii[:, :, :n], op=ALU.mult), reads=[a2.tok(), ii.tok()], writes=[uu.tok()])
            S.op("pool", lambda e: e.tensor_tensor(out=uu[:, :, :n], in0=uu[:, :, :n], in1=xb[:, :, :n], op=ALU.mult), reads=[uu.tok(), xb.tok()], writes=[uu.tok()])
            hlb = hl[b2]
            for c4 in range(4):
                if prev_hl is None:
                    init, rd = 0.0, []
                else:
                    ph, pn = prev_hl
                    init, rd = ph[:, c4, pn - 1:pn], [ph.tok()]
                S.op("dve", lambda e, c4=c4, init=init: e.tensor_tensor_scan(out=hlb[:, c4, :n], data0=aa[:, c4, :n], data1=uu[:, c4, :n], initial=init, op0=ALU.mult, op1=ALU.add),
                     reads=[aa.tok(), uu.tok()] + rd, writes=[hlb.tok()])
            prev_hl = (hlb, n)
            outs.append(S.dma("sp", lambda e, hlb=hlb, t0=t0, n=n: e.dma_start(out=hlv[:, :, t0:t0 + n], in_=hlb[:, :, :n]), f"hl{b2}", reads=[hlb.tok()]))
            ob = ost[b2]
            for c in range(nch):
                cs = slice(c * 64, (c + 1) * 64)
                for h in range(4):
                    S.op("pe", lambda e, h=h, cs=cs: e.matmul(pAT[0:64, h * 64:(h + 1) * 64], lhsT=ktb[:, h, cs], rhs=qtb[:, h, cs], start=True, stop=True),
                         reads=[ktb.tok(), qtb.tok()], writes=[pAT.tok()])
                S.op("dve", lambda e: e.tensor_tensor(out=ATs[:], in0=pAT[0:64, 0:256], in1=mask4[:], op=ALU.mult), reads=[pAT.tok(), mask4.tok()], writes=[ATs.tok()])
                for h in range(4):
                    hv = slice(h * 128, (h + 1) * 128)
                    S.op("pe", lambda e, h=h, hv=hv, c=c: e.matmul(pO[0:64, hv], lhsT=ATs[:, h * 64:(h + 1) * 64], rhs=vb[:, c, hv], start=True, stop=False),
                         reads=[ATs.tok(), vb.tok(c)], writes=[pO.tok()])
                    S.op("pe", lambda e, h=h, hv=hv, cs=cs: e.matmul(pO[0:64, hv], lhsT=qtb[:, h, cs], rhs=Sb[:, hv], start=False, stop=True),
                         reads=[qtb.tok(), Sb.tok()], writes=[pO.tok()])
                S.op("act", lambda e, c=c: e.activation(out=ob[:, c, :], in_=pO[0:64, :], func=AF.Copy), reads=[pO.tok()], writes=[ob.tok()])
                for h in range(4):
                    hv = slice(h * 128, (h + 1) * 128)
                    S.op("pe", lambda e, h=h, hv=hv, c=c: e.matmul(pU[0:64, hv], lhsT=khtb[:, c, h * 64:(h + 1) * 64], rhs=vb[:, c, hv], start=True, stop=True),
                         reads=[khtb.tok(c), vb.tok(c)], writes=[pU.tok()])
                for h in range(4):
                    hv = slice(h * 128, (h + 1) * 128)
                    S.op("dve", lambda e, h=h, hv=hv, c=c: e.scalar_tensor_tensor(out=S32[:, hv], in0=S32[:, hv], scalar=decb[:, h, c:c + 1], in1=pU[0:64, hv], op0=ALU.mult, op1=ALU.add),
                         reads=[S32.tok(), decb.tok(), pU.tok()], writes=[S32.tok()])
                S.op("pool", lambda e: e.tensor_copy(out=Sb[:], in_=S32[:]), reads=[S32.tok()], writes=[Sb.tok()])
            outs.append(S.dma("sp", lambda e, ob=ob, t0=t0, n=n, nch=nch: e.dma_start(out=og[t0:t0 + n, :].rearrange("(c s) v -> s c v", s=64), in_=ob[:, :nch, :]),
                              f"ost{b2}", reads=[ob.tok()]))
            return prev_hl

        for bi, (t0, n, mi) in enumerate(seq_blocks()):
            prev_hl = do_block(bi, t0, n, mi, prev_hl)
        S.wait_all_final("sp", outs)
        S.emit()
    return nc


def mods_split(m):
    names = ["sh1", "sc1", "g1", "sh2", "sc2", "g2"]
    return {nm: m[..., i * D:(i + 1) * D] for i, nm in enumerate(names)}


def consts_p1():
    s = np.arange(64)[:, None]
    t = np.arange(64)[None, :]
    m = (s <= t).astype(np.float32)
    rm = np.ones((128, BLK), np.float32)
    rm[:, ::64] = 0.0
    return dict(ones=np.ones((128, 128), np.float32), mask4=np.ascontiguousarray(np.tile(m, (1, 4))),
                ident=np.eye(64, dtype=np.float32), rmask=rm)


def seq_T(ctx_b, x_b, d):
    if d == 1:
        ctx_b, x_b = ctx_b[::-1], x_b[::-1]
    return np.ascontiguousarray(np.concatenate([ctx_b, x_b], 0).T)


def unseq(a, d):
    c, l = a[:NCTX], a[NCTX:]
    if d == 1:
        c, l = c[::-1], l[::-1]
    return c, l


def prep_p1(inp, mods0, cmods0, b, d):
    w_in = inp["ab_w_in"][0]
    lr = w_in[:, 1536:1552] if d == 0 else w_in[:, 1552:1568]
    w1 = np.ascontiguousarray(np.concatenate([w_in[:, 0:512], w_in[:, 2080:2592], lr, w_in[:, 512:1024]], 1))
    ml, mc = mods_split(mods0[b]), mods_split(cmods0)
    modv = np.stack([col_layout(inp["norm1_g"][0]), col_layout(ml["sc1"]), col_layout(ml["sh1"]),
                     col_layout(mc["sc1"]), col_layout(mc["sh1"])], 1)
    cw = inp["rg_conv_w"][0]
    z = np.zeros_like(cw[0])
    taps = [cw[0], cw[1], cw[2], cw[3], z] if d == 0 else [z, cw[3], cw[2], cw[1], cw[0]]
    vecs = taps + [inp["rg_conv_b"][0], inp["rg_br"][0, d], inp["rg_bi"][0, d], inp["rg_lambda"][0, d]]
    lruc = np.stack([col_layout(v) for v in vecs], 2)
    wbd = np.zeros((128, 2, 4, 128), np.float32)
    for j, W in enumerate([inp["rg_wr"][0, d], inp["rg_wi"][0, d]]):
        for c4 in range(4):
            wbd[0:64, j, c4, 0:64] = W[2 * c4]
            wbd[64:128, j, c4, 64:128] = W[2 * c4 + 1]
    m = dict(xT=seq_T(inp["ctx"][b], inp["x"][b], d), w1=w1, modv=np.ascontiguousarray(modv),
             wa2=np.ascontiguousarray(inp["gla_wa2"][0, d]), ba=np.ascontiguousarray(inp["gla_ba"][0, d].reshape(4, 64).T),
             lruc=np.ascontiguousarray(lruc), wbd=wbd)
    m.update(consts_p1())
    return m


NTOK = 4096 + 128


def tok_blocks():
    blocks = [(i * BLK, BLK, 0) for i in range(4096 // BLK)]
    blocks.append((4096, 128, 1))
    if DEBUG_NBLK:
        blocks = blocks[:DEBUG_NBLK - 1] + blocks[-1:]
    return blocks


def router_softmax(S, T2, n, t0, rw, probs_d, pool, W, outs, tag):
    for tt in range(n // 128):
        p = pool.next()
        for kc in range(8):
            S.op("pe", lambda e, p=p, kc=kc, tt=tt: e.matmul(p[:, 0:16], lhsT=T2[:, kc, tt * 128:(tt + 1) * 128], rhs=rw[:, kc, :], start=(kc == 0), stop=(kc == 7)),
                 reads=[T2.tok(), rw.tok()], writes=[p.tok()])
        mx, ex, ssum, pr = W["mx"], W["ex"], W["ssum"], W["pr"][tt % 2]
        S.op("dve", lambda e, p=p: e.tensor_reduce(out=mx[:], in_=p[:, 0:16], axis=AX.X, op=ALU.max, negate=True), reads=[p.tok()], writes=[mx.tok()])
        S.op("act", lambda e, p=p: e.activation(out=ex[:], in_=p[:, 0:16], func=AF.Exp, bias=mx[:, 0:1], scale=1.0, accum_out=ssum[:, 0:1]),
             reads=[p.tok(), mx.tok()], writes=[ex.tok(), ssum.tok()])
        S.op("dve", lambda e: e.reciprocal(out=ssum[:], in_=ssum[:]), reads=[ssum.tok()], writes=[ssum.tok()])
        S.op("dve", lambda e, pr=pr: e.tensor_scalar(out=pr[:], in0=ex[:], scalar1=ssum[:, 0:1], scalar2=None, op0=ALU.mult),
             reads=[ex.tok(), ssum.tok()], writes=[pr.tok()])
        r0 = t0 + tt * 128
        outs.append(S.dma("sp", lambda e, pr=pr, r0=r0: e.dma_start(out=probs_d[r0:r0 + 128, :], in_=pr[:]), f"{tag}pr{tt % 2}", reads=[pr.tok()]))


def router_work(S):
    return dict(mx=S.sbuf("r_mx", [128, 1], F32), ex=S.sbuf("r_ex", [128, 16], F32), ssum=S.sbuf("r_ss", [128, 1], F32),
                pr=[S.sbuf(f"r_pr{i}", [128, 16], F32) for i in range(2)])


def build_p2():
    nc = bass.Bass("TRN2", target_bir_lowering=False)
    dt = lambda name, shape, kind="ExternalInput", d=F32: nc.dram_tensor(name, list(shape), d, kind=kind).ap()
    xT = dt("xT", [D, NTOK])
    ogf_d, ogb_d = dt("ogf", [512, NTOK]), dt("ogb", [512, NTOK])
    hlf_d, hlb_d = dt("hlf", [512, NTOK]), dt("hlb", [512, NTOK])
    w2 = dt("w2", [D, 1024]); wout = dt("wout", [1024, D]); rw_d = dt("rw", [128, 8, 16])
    modv_d = dt("modv", [128, 5, 8]); modv2_d = dt("modv2", [128, 5, 8]); g1_d = dt("g1v", [128, 2, 8]); gng_d = dt("gng", [128, 4])
    ones_d = dt("ones", [128, 128])
    xmT = dt("xmT", [D, NTOK], kind="ExternalOutput")
    t2T = dt("t2T", [D, NTOK], kind="ExternalOutput")
    probs_d = dt("probs", [NTOK, 16], kind="ExternalOutput")
    with ExitStack() as st:
        S = Sched(nc, st)
        pool = PsumPool(S, ["pp0", "pp1", "pp2", "pp3", "pp4", "pp5"])
        ones32 = S.sbuf("ones32", [128, 128], F32); load_const(S, ones_d, ones32, "c_ones")
        modv = S.sbuf("modv", [128, 5, 8], F32); load_const(S, modv_d, modv, "c_modv")
        modv2 = S.sbuf("modv2", [128, 5, 8], F32); load_const(S, modv2_d, modv2, "c_modv2")
        g1v = S.sbuf("g1v", [128, 2, 8], F32); load_const(S, g1_d, g1v, "c_g1")
        gng = S.sbuf("gng", [128, 4], F32); load_const(S, gng_d, gng, "c_gng")
        rw = S.sbuf("rw", [128, 8, 16], F32); load_const(S, rw_d, rw, "c_rw")
        epsc = S.sbuf("epsc", [128, 1], F32)
        S.op("dve", lambda e: e.memset(epsc[:], EPS), writes=[epsc.tok()])
        gs1 = S.sbuf("gs1", [128, 2, 8], F32); make_gs(S, modv, gs1, 2)
        gs2 = S.sbuf("gs2", [128, 2, 8], F32); make_gs(S, modv2, gs2, 2)
        sh1 = S.sbuf("sh1", [128, 2, 8], F32); sh2 = S.sbuf("sh2", [128, 2, 8], F32)
        for i in range(2):
            S.op("dve", lambda e, i=i: e.tensor_copy(out=sh1[:, i, :], in_=modv[:, 2 + 2 * i, :]), reads=[modv.tok()], writes=[sh1.tok()])
            S.op("dve", lambda e, i=i: e.tensor_copy(out=sh2[:, i, :], in_=modv2[:, 2 + 2 * i, :]), reads=[modv2.tok()], writes=[sh2.tok()])
        X = [S.sbuf(f"X{i}", [128, 8, BLK], F32) for i in range(2)]
        w2b = S.sbuf("w2b", [128, 8, 1024], BF16); load_cast_weight(S, w2, 1024, w2b, X, "X")
        woutb = S.sbuf("woutb", [128, 8, 1024], BF16); load_cast_weight(S, wout, 1024, woutb, X, "X")
        NW = norm_work(S, pool, ones32, epsc)
        RW = router_work(S)
        XN = [S.sbuf(f"XN{i}", [128, 8, BLK], BF16) for i in range(2)]
        sg = S.sbuf("sg", [128, 4, BLK], F32); rgx = S.sbuf("rgx", [128, 4, BLK], F32)
        gt1 = S.sbuf("gt1", [128, 4, BLK], F32); gt2 = S.sbuf("gt2", [128, 4, BLK], F32)
        OG = [[S.sbuf(f"OG{j}{i}", [128, 4, BLK], F32) for i in range(2)] for j in range(2)]
        HL = [[S.sbuf(f"HL{j}{i}", [128, 4, BLK], F32) for i in range(2)] for j in range(2)]
        osum = S.sbuf("osum", [128, 4, BLK], F32); sqo = S.sbuf("sqo", [128, 4, BLK], F32)
        hrs = S.sbuf("hrs", [128, BLK], F32); hsd = S.sbuf("hsd", [128, BLK], F32); ht = S.sbuf("ht", [128, BLK], F32)
        mg = S.sbuf("mg", [128, 4, BLK], BF16); ml = S.sbuf("ml", [128, 4, BLK], BF16); hsum = S.sbuf("hsum", [128, 4, BLK], F32)
        XM = [S.sbuf(f"XM{i}", [128, 8, BLK], F32) for i in range(2)]
        T2 = [S.sbuf(f"T2{i}", [128, 8, BLK], F32) for i in range(2)]
        xTv = xT.rearrange("(c p) n -> p c n", p=128)
        v4 = lambda a: a.rearrange("(c p) n -> p c n", p=128)
        outs = []

        def do_block(bi, t0, n, mi):
            b2 = bi % 2
            Xb, XNb, XMb, T2b = X[b2], XN[b2], XM[b2], T2[b2]
            S.dma("sp", lambda e: e.dma_start(out=Xb[:, :, :n], in_=xTv[:, :, t0:t0 + n]), f"X{b2}", writes=[Xb.tok()])
            ogs, hls = [OG[0][b2], OG[1][b2]], [HL[0][b2], HL[1][b2]]
            for j, (src, dst) in enumerate([(ogf_d, ogs[0]), (ogb_d, ogs[1]), (hlf_d, hls[0]), (hlb_d, hls[1])]):
                S.dma("sp", lambda e, src=src, dst=dst: e.dma_start(out=dst[:, :, :n], in_=v4(src)[:, :, t0:t0 + n]), f"in{j}{b2}", writes=[dst.tok()])
            norm_fm(S, Xb, n, gs1, sh1, mi, XNb, NW)
            for c4 in range(4):
                p = pool.next()
                for kc in range(8):
                    S.op("pe", lambda e, p=p, kc=kc, c4=c4: e.matmul(p[:, :n], lhsT=w2b[:, kc, c4 * 128:(c4 + 1) * 128], rhs=XNb[:, kc, :n], start=(kc == 0), stop=(kc == 7)),
                         reads=[w2b.tok(), XNb.tok()], writes=[p.tok()])
                S.op("act", lambda e, p=p, c4=c4: e.activation(out=sg[:, c4, :n], in_=p[:, :n], func=AF.Silu), reads=[p.tok()], writes=[sg.tok()])
            for c4 in range(4):
                p = pool.next()
                for kc in range(8):
                    S.op("pe", lambda e, p=p, kc=kc, c4=c4: e.matmul(p[:, :n], lhsT=w2b[:, kc, 512 + c4 * 128:512 + (c4 + 1) * 128], rhs=XNb[:, kc, :n], start=(kc == 0), stop=(kc == 7)),
                         reads=[w2b.tok(), XNb.tok()], writes=[p.tok()])
                S.op("act", lambda e, p=p, c4=c4: e.activation(out=rgx[:, c4, :n], in_=p[:, :n], func=AF.Copy), reads=[p.tok()], writes=[rgx.tok()])
            S.op("pool", lambda e: e.tensor_tensor(out=gt1[:, :, :n], in0=rgx[:, :, :n], in1=rgx[:, :, :n], op=ALU.mult), reads=[rgx.tok()], writes=[gt1.tok()])
            S.op("pool", lambda e: e.tensor_scalar(out=gt1[:, :, :n], in0=gt1[:, :, :n], scalar1=0.044715, scalar2=1.0, op0=ALU.mult, op1=ALU.add), reads=[gt1.tok()], writes=[gt1.tok()])
            S.op("pool", lambda e: e.tensor_tensor(out=gt1[:, :, :n], in0=gt1[:, :, :n], in1=rgx[:, :, :n], op=ALU.mult), reads=[gt1.tok(), rgx.tok()], writes=[gt1.tok()])
            S.op("act", lambda e: e.activation(out=gt2[:, :, :n], in_=gt1[:, :, :n], func=AF.Sigmoid, scale=1.5957691216), reads=[gt1.tok()], writes=[gt2.tok()])
            S.op("pool", lambda e: e.tensor_tensor(out=gt2[:, :, :n], in0=gt2[:, :, :n], in1=rgx[:, :, :n], op=ALU.mult), reads=[gt2.tok(), rgx.tok()], writes=[gt2.tok()])
            S.op("pool", lambda e: e.tensor_tensor(out=osum[:, :, :n], in0=ogs[0][:, :, :n], in1=ogs[1][:, :, :n], op=ALU.add), reads=[ogs[0].tok(), ogs[1].tok()], writes=[osum.tok()])
            S.op("act", lambda e: e.activation(out=sqo[:, :, :n], in_=osum[:, :, :n], func=AF.Square), reads=[osum.tok()], writes=[sqo.tok()])
            for c4 in range(4):
                p = pool.next()
                S.op("pe", lambda e, p=p, c4=c4: e.matmul(p[:, :n], lhsT=ones32[:, :], rhs=sqo[:, c4, :n], start=True, stop=True), reads=[ones32.tok(), sqo.tok()], writes=[p.tok()])
                S.op("act", lambda e, p=p: e.activation(out=hsd[:, :n], in_=p[:, :n], func=AF.Sqrt, bias=epsc[:, 0:1], scale=1.0 / 128), reads=[p.tok(), epsc.tok()], writes=[hsd.tok()])
                S.op("dve", lambda e: e.reciprocal(out=hrs[:, :n], in_=hsd[:, :n]), reads=[hsd.tok()], writes=[hrs.tok()])
                S.op("dve", lambda e, c4=c4: e.tensor_tensor(out=ht[:, :n], in0=osum[:, c4, :n], in1=hrs[:, :n], op=ALU.mult), reads=[osum.tok(), hrs.tok()], writes=[ht.tok()])
                S.op("dve", lambda e, c4=c4: e.scalar_tensor_tensor(out=mg[:, c4, :n], in0=ht[:, :n], scalar=gng[:, c4:c4 + 1], in1=sg[:, c4, :n], op0=ALU.mult, op1=ALU.mult),
                     reads=[ht.tok(), gng.tok(), sg.tok()], writes=[mg.tok()])
            S.op("pool", lambda e: e.tensor_tensor(out=hsum[:, :, :n], in0=hls[0][:, :, :n], in1=hls[1][:, :, :n], op=ALU.add), reads=[hls[0].tok(), hls[1].tok()], writes=[hsum.tok()])
            S.op("pool", lambda e: e.tensor_tensor(out=ml[:, :, :n], in0=hsum[:, :, :n], in1=gt2[:, :, :n], op=ALU.mult), reads=[hsum.tok(), gt2.tok()], writes=[ml.tok()])
            for oc in range(8):
                p = pool.next()
                for kc in range(8):
                    src = mg if kc < 4 else ml
                    S.op("pe", lambda e, p=p, kc=kc, oc=oc, src=src: e.matmul(p[:, :n], lhsT=woutb[:, kc, oc * 128:(oc + 1) * 128], rhs=src[:, kc % 4, :n], start=(kc == 0), stop=(kc == 7)),
                         reads=[woutb.tok(), src.tok()], writes=[p.tok()])
                S.op("dve", lambda e, p=p, oc=oc: e.scalar_tensor_tensor(out=XMb[:, oc, :n], in0=p[:, :n], scalar=g1v[:, mi, oc:oc + 1], in1=Xb[:, oc, :n], op0=ALU.mult, op1=ALU.add),
                     reads=[p.tok(), g1v.tok(), Xb.tok()], writes=[XMb.tok()])
            outs.append(S.dma("sp", lambda e: e.dma_start(out=v4(xmT)[:, :, t0:t0 + n], in_=XMb[:, :, :n]), f"XM{b2}", reads=[XMb.tok()]))
            norm_fm(S, XMb, n, gs2, sh2, mi, T2b, NW)
            outs.append(S.dma("sp", lambda e: e.dma_start(out=v4(t2T)[:, :, t0:t0 + n], in_=T2b[:, :, :n]), f"T2{b2}", reads=[T2b.tok()]))
            router_softmax(S, T2b, n, t0, rw, probs_d, pool, RW, outs, "p2")

        for bi, (t0, n, mi) in enumerate(tok_blocks()):
            do_block(bi, t0, n, mi)
        S.wait_all_final("sp", outs)
        S.emit()
    return nc


def half_tokens(lat_b, ctx_b, half):
    return np.concatenate([lat_b[half * 4096:(half + 1) * 4096], ctx_b[half * 128:(half + 1) * 128]], 0)


def modv_pack(norm_g, ml, mc, sc, sh):
    return np.ascontiguousarray(np.stack([col_layout(norm_g), col_layout(ml[sc]), col_layout(ml[sh]), col_layout(mc[sc]), col_layout(mc[sh])], 1))


def prep_p2(inp, mods0, cmods0, b, half, og_f, og_b, hl_f, hl_b):
    w_in = inp["ab_w_in"][0]
    ml, mc = mods_split(mods0[b]), mods_split(cmods0)
    T = lambda lat, ctx: np.ascontiguousarray(half_tokens(lat, ctx, half).T)
    return dict(xT=T(inp["x"][b], inp["ctx"][b]),
                ogf=T(og_f["lat"], og_f["ctx"]), ogb=T(og_b["lat"], og_b["ctx"]),
                hlf=T(hl_f["lat"], hl_f["ctx"]), hlb=T(hl_b["lat"], hl_b["ctx"]),
                w2=np.ascontiguousarray(np.concatenate([w_in[:, 1024:1536], w_in[:, 1568:2080]], 1)),
                wout=np.ascontiguousarray(inp["ab_w_out"][0]),
                rw=np.ascontiguousarray(inp["router_w"][0].reshape(8, 128, 16).transpose(1, 0, 2)),
                modv=modv_pack(inp["norm1_g"][0], ml, mc, "sc1", "sh1"), modv2=modv_pack(inp["norm2_g"][0], ml, mc, "sc2", "sh2"),
                g1v=np.ascontiguousarray(np.stack([col_layout(ml["g1"]), col_layout(mc["g1"])], 1)),
                gng=col_layout(inp["gla_norm_g"][0]), ones=np.ones((128, 128), np.float32))


def build_p0():
    nc = bass.Bass("TRN2", target_bir_lowering=False)
    dt = lambda name, shape, kind="ExternalInput", d=F32: nc.dram_tensor(name, list(shape), d, kind=kind).ap()
    cT = dt("cT", [128, 8, 5])
    mw = dt("mw", [2, D, 768])
    mb = dt("mb", [2, 5, 768])
    out = dt("mods", [2, 5, 768], kind="ExternalOutput")
    with ExitStack() as st:
        S = Sched(nc, st)
        pool = PsumPool(S, ["pp0", "pp1"])
        c32 = S.sbuf("c32", [128, 8, 5], F32); load_const(S, cT, c32, "c_c")
        sc = S.sbuf("sc", [128, 8, 5], F32)
        S.op("act", lambda e: e.activation(out=sc[:], in_=c32[:], func=AF.Silu), reads=[c32.tok()], writes=[sc.tok()])
        outs = []
        for l in range(2):
            w = S.sbuf(f"w{l}", [128, 8, 768], F32)
            S.dma("sp", lambda e, w=w, l=l: e.dma_start(out=w[:], in_=mw[l].rearrange("(c p) n -> p c n", p=128)), f"w{l}", writes=[w.tok()])
            b = S.sbuf(f"b{l}", [5, 768], F32)
            S.dma("sp", lambda e, b=b, l=l: e.dma_start(out=b[:], in_=mb[l]), f"b{l}", writes=[b.tok()])
            o = S.sbuf(f"o{l}", [5, 768], F32)
            for j in range(2):
                p = pool.next()
                for kc in range(8):
                    S.op("pe", lambda e, p=p, kc=kc, j=j, w=w: e.matmul(p[0:5, 0:384], lhsT=sc[:, kc, :], rhs=w[:, kc, j * 384:(j + 1) * 384], start=(kc == 0), stop=(kc == 7)),
                         reads=[sc.tok(), w.tok()], writes=[p.tok()])
                S.op("dve", lambda e, p=p, j=j, o=o, b=b: e.tensor_tensor(out=o[:, j * 384:(j + 1) * 384], in0=p[0:5, 0:384], in1=b[:, j * 384:(j + 1) * 384], op=ALU.add),
                     reads=[p.tok(), b.tok()], writes=[o.tok()])
            outs.append(S.dma("sp", lambda e, o=o, l=l: e.dma_start(out=out[l], in_=o[:]), f"o{l}", reads=[o.tok()]))
        S.wait_all_final("sp", outs)
        S.emit()
    return nc


def run_p0(inp):
    cv = np.concatenate([inp["c"], inp["c_ctx"][None]], 0)
    cT = np.ascontiguousarray(cv.T.reshape(8, 128, 5).transpose(1, 0, 2))
    maps = []
    for i in range(8):
        sl = slice(i * 768, (i + 1) * 768)
        maps.append(dict(cT=cT, mw=np.ascontiguousarray(inp["mod_w"][:, :, sl]),
                         mb=np.ascontiguousarray(np.broadcast_to(inp["mod_b"][:, None, sl], (2, 5, 768)))))
    res = run_bass_kernel_spmd(build_p0(), maps, core_ids=list(range(8)))
    mods = np.concatenate([r["mods"] for r in res.results], 2)
    return mods


NITER = 26
PASSES = [[(i * BLK, BLK, 0) for i in range(4 * j, 4 * j + 4)] for j in range(4)]
PASSES[3] = PASSES[3] + [(4096, 128, 1)]
PASS_W = 1152


def build_p4(final):
    nc = bass.Bass("TRN2", target_bir_lowering=False)
    dt = lambda name, shape, kind="ExternalInput", d=F32: nc.dram_tensor(name, list(shape), d, kind=kind).ap()
    t2T = dt("t2T", [D, NTOK]); xmT = dt("xmT", [D, NTOK])
    pl_d = dt("pl", [128, 1024]); pc_d = dt("pc", [128, 32])
    po_d = dt("po", [16, NTOK])
    G_d = dt("G", [128, 128]); sel_d = dt("sel", [128, 16]); selE_d = dt("selE", [16, 16, 128]); kv_d = dt("kv", [128, 2])
    g2_d = dt("g2v", [128, 2, 8]); ones_d = dt("ones", [128, 128]); modf_d = dt("modf", [128, 3, 8])
    wg_d = dt("wg", [16, D, 1024]); wu_d = dt("wu", [16, D, 1024]); wd_d = dt("wd", [16, 1024, D])
    xoT = dt("xoT", [D, NTOK], kind="ExternalOutput")
    with ExitStack() as st:
        S = Sched(nc, st)
        pool = PsumPool(S, ["pp0", "pp1", "pp2", "pp3", "pp4", "pp5", "pp6"])
        pS = S.psum("pS", [128, 512], F32)
        PL = S.sbuf("PL", [128, 1024], F32); load_const(S, pl_d, PL, "c_pl")
        PC = S.sbuf("PC", [128, 32], F32); load_const(S, pc_d, PC, "c_pc")
        PO = S.sbuf("PO", [16, NTOK], F32); load_const(S, po_d, PO, "c_po")
        G = S.sbuf("G", [128, 128], F32); load_const(S, G_d, G, "c_G")
        sel = S.sbuf("sel", [128, 16], F32); load_const(S, sel_d, sel, "c_sel")
        selE = S.sbuf("selE", [16, 16, 128], F32); load_const(S, selE_d, selE, "c_selE")
        kv = S.sbuf("kv", [128, 2], F32); load_const(S, kv_d, kv, "c_kv")
        g2v = S.sbuf("g2v", [128, 2, 8], F32); load_const(S, g2_d, g2v, "c_g2")
        ones32 = S.sbuf("ones32", [128, 128], F32); load_const(S, ones_d, ones32, "c_ones")
        lo = S.sbuf("lo", [128, 2], F32); mid = S.sbuf("mid", [128, 2], F32); cnt = S.sbuf("cnt", [128, 2], F32)
        selm = S.sbuf("selm", [128, 2], F32); junk = S.sbuf("junk", [128, 1024], F32)
        S.op("dve", lambda e: e.memset(lo[:], 0.0), writes=[lo.tok()])
        for it in range(NITER):
            hk = 2.0 ** -(it + 1)
            S.op("dve", lambda e, hk=hk: e.tensor_scalar(out=mid[:], in0=lo[:], scalar1=hk, scalar2=None, op0=ALU.add), reads=[lo.tok()], writes=[mid.tok()])
            S.op("dve", lambda e: e.tensor_scalar(out=junk[:, :], in0=PL[:, :], scalar1=mid[:, 0:1], scalar2=None, op0=ALU.is_ge, op1=ALU.add, accum_out=cnt[:, 0:1]),
                 reads=[PL.tok(), mid.tok()], writes=[junk.tok(), cnt.tok()])
            S.op("dve", lambda e: e.tensor_scalar(out=junk[:, 0:32], in0=PC[:, :], scalar1=mid[:, 1:2], scalar2=None, op0=ALU.is_ge, op1=ALU.add, accum_out=cnt[:, 1:2]),
                 reads=[PC.tok(), mid.tok()], writes=[junk.tok(), cnt.tok()])
            S.op("pe", lambda e: e.matmul(pS[:, 0:2], lhsT=G[:, :], rhs=cnt[:, :], start=True, stop=True), reads=[G.tok(), cnt.tok()], writes=[pS.tok()])
            S.op("dve", lambda e: e.tensor_tensor(out=selm[:], in0=pS[:, 0:2], in1=kv[:], op=ALU.is_ge), reads=[pS.tok(), kv.tok()], writes=[selm.tok()])
            S.op("dve", lambda e, hk=hk: e.scalar_tensor_tensor(out=lo[:], in0=selm[:], scalar=hk, in1=lo[:], op0=ALU.mult, op1=ALU.add), reads=[selm.tok(), lo.tok()], writes=[lo.tok()])
        thr = S.sbuf("thr", [16, 2], F32)
        S.op("pe", lambda e: e.matmul(pS[0:16, 0:2], lhsT=sel[:, :], rhs=lo[:, :], start=True, stop=True), reads=[sel.tok(), lo.tok()], writes=[pS.tok()])
        S.op("dve", lambda e: e.tensor_copy(out=thr[:], in_=pS[0:16, 0:2]), reads=[pS.tok()], writes=[thr.tok()])
        gateT = PO
        S.op("dve", lambda e: e.scalar_tensor_tensor(out=gateT[:, 0:4096], in0=PO[:, 0:4096], scalar=thr[:, 0:1], in1=PO[:, 0:4096], op0=ALU.is_ge, op1=ALU.mult),
             reads=[PO.tok(), thr.tok()], writes=[gateT.tok()])
        S.op("dve", lambda e: e.scalar_tensor_tensor(out=gateT[:, 4096:NTOK], in0=PO[:, 4096:NTOK], scalar=thr[:, 1:2], in1=PO[:, 4096:NTOK], op0=ALU.is_ge, op1=ALU.mult),
             reads=[PO.tok(), thr.tok()], writes=[gateT.tok()])
        stg = [S.sbuf(f"stg{i}", [128, 8, BLK], F32) for i in range(2)]
        wgb = S.sbuf("wgb", [128, 8, 1024], BF16); wub = S.sbuf("wub", [128, 8, 1024], BF16); wdb = S.sbuf("wdb", [128, 8, 1024], BF16)
        t2b = S.sbuf("t2b", [128, 8, PASS_W], BF16)
        yacc = S.sbuf("yacc", [128, 8, PASS_W], F32)
        gbc = S.sbuf("gbc", [128, BLK], F32)
        sgt = [S.sbuf(f"sgt{i}", [128, BLK], F32) for i in range(2)]
        hh = [S.sbuf(f"hh{i}", [128, BLK], F32) for i in range(2)]
        hgb = S.sbuf("hgb", [128, 8, BLK], BF16)
        XO = [S.sbuf(f"XO{i}", [128, 8, BLK], F32) for i in range(2)]
        v4 = lambda a: a.rearrange("(c p) n -> p c n", p=128)
        outs = []
        if final:
            modf = S.sbuf("modf", [128, 3, 8], F32); load_const(S, modf_d, modf, "c_modf")
            epsc = S.sbuf("epsc", [128, 1], F32)
            S.op("dve", lambda e: e.memset(epsc[:], EPS), writes=[epsc.tok()])
            gsf = S.sbuf("gsf", [128, 1, 8], F32); make_gs(S, modf, gsf, 1)
            shf = S.sbuf("shf", [128, 1, 8], F32)
            S.op("dve", lambda e: e.tensor_copy(out=shf[:, 0, :], in_=modf[:, 2, :]), reads=[modf.tok()], writes=[shf.tok()])
            NW = norm_work(S, pool, ones32, epsc)
            XF = XO
        stgi = [0]

        def load_w(src, dst):
            wv = src.rearrange("(c p) n -> p c n", p=128)
            for c0 in range(0, 1024, BLK):
                s_ = stg[stgi[0] % 2]
                S.dma("sp", lambda e, s_=s_, c0=c0: e.dma_start(out=s_[:], in_=wv[:, :, c0:c0 + BLK]), f"stg{stgi[0] % 2}", writes=[s_.tok()])
                S.op("pool", lambda e, s_=s_, c0=c0: e.tensor_copy(out=dst[:, :, c0:c0 + BLK], in_=s_[:]), reads=[s_.tok()], writes=[dst.tok()])
                stgi[0] += 1

        def expert_block(e_i, off, t0, n):
            p = pool.next()
            S.op("pe", lambda e: e.matmul(p[:, :n], lhsT=selE[:, e_i, :], rhs=gateT[:, t0:t0 + n], start=True, stop=True), reads=[selE.tok(), gateT.tok()], writes=[p.tok()])
            S.op("act", lambda e: e.activation(out=gbc[:, :n], in_=p[:, :n], func=AF.Copy), reads=[p.tok()], writes=[gbc.tok()])
            for fc in range(8):
                pg, pu = pool.next(), pool.next()
                for kc in range(8):
                    S.op("pe", lambda e, kc=kc, fc=fc, pg=pg: e.matmul(pg[:, :n], lhsT=wgb[:, kc, fc * 128:(fc + 1) * 128], rhs=t2b[:, kc, off:off + n], start=(kc == 0), stop=(kc == 7)),
                         reads=[wgb.tok(), t2b.tok()], writes=[pg.tok()])
                for kc in range(8):
                    S.op("pe", lambda e, kc=kc, fc=fc, pu=pu: e.matmul(pu[:, :n], lhsT=wub[:, kc, fc * 128:(fc + 1) * 128], rhs=t2b[:, kc, off:off + n], start=(kc == 0), stop=(kc == 7)),
                         reads=[wub.tok(), t2b.tok()], writes=[pu.tok()])
                sg_, hh_ = sgt[fc % 2], hh[fc % 2]
                S.op("act", lambda e, sg_=sg_, pg=pg: e.activation(out=sg_[:, :n], in_=pg[:, :n], func=AF.Silu), reads=[pg.tok()], writes=[sg_.tok()])
                S.op("dve", lambda e, hh_=hh_, pu=pu, sg_=sg_: e.tensor_tensor(out=hh_[:, :n], in0=pu[:, :n], in1=sg_[:, :n], op=ALU.mult), reads=[pu.tok(), sg_.tok()], writes=[hh_.tok()])
                S.op("pool", lambda e, fc=fc, hh_=hh_: e.tensor_tensor(out=hgb[:, fc, :n], in0=hh_[:, :n], in1=gbc[:, :n], op=ALU.mult), reads=[hh_.tok(), gbc.tok()], writes=[hgb.tok()])
            for dc in range(8):
                py = pool.next()
                for fc in range(8):
                    S.op("pe", lambda e, fc=fc, dc=dc, py=py: e.matmul(py[:, :n], lhsT=wdb[:, fc, dc * 128:(dc + 1) * 128], rhs=hgb[:, fc, :n], start=(fc == 0), stop=(fc == 7)),
                         reads=[wdb.tok(), hgb.tok()], writes=[py.tok()])
                if e_i == 0:
                    S.op("dve", lambda e, dc=dc, py=py: e.tensor_copy(out=yacc[:, dc, off:off + n], in_=py[:, :n]), reads=[py.tok()], writes=[yacc.tok((off, dc))])
                else:
                    S.op("dve", lambda e, dc=dc, py=py: e.tensor_tensor(out=yacc[:, dc, off:off + n], in0=py[:, :n], in1=yacc[:, dc, off:off + n], op=ALU.add),
                         reads=[py.tok(), yacc.tok((off, dc))], writes=[yacc.tok((off, dc))])

        def finish_block(bi, off, t0, n, mi):
            XOb = XO[bi % 2]
            S.dma("sp", lambda e: e.dma_start(out=XOb[:, :, :n], in_=v4(xmT)[:, :, t0:t0 + n]), f"XO{bi % 2}", writes=[XOb.tok()])
            for dc in range(8):
                S.op("dve", lambda e, dc=dc: e.scalar_tensor_tensor(out=XOb[:, dc, :n], in0=yacc[:, dc, off:off + n], scalar=g2v[:, mi, dc:dc + 1], in1=XOb[:, dc, :n], op0=ALU.mult, op1=ALU.add),
                     reads=[yacc.tok((off, dc)), g2v.tok(), XOb.tok()], writes=[XOb.tok()])
            if final:
                XFb = XF[bi % 2]
                norm_fm(S, XOb, n, gsf, shf, 0, XFb, NW)
                outs.append(S.dma("sp", lambda e: e.dma_start(out=v4(xoT)[:, :, t0:t0 + n], in_=XFb[:, :, :n]), f"XF{bi % 2}", reads=[XFb.tok()]))
            else:
                outs.append(S.dma("sp", lambda e: e.dma_start(out=v4(xoT)[:, :, t0:t0 + n], in_=XOb[:, :, :n]), f"XOo{bi % 2}", reads=[XOb.tok()]))

        npass = 1 if DEBUG_NBLK else 4
        nexp = DEBUG_NBLK if DEBUG_NBLK else 16
        for ps_i in range(npass):
            blocks = PASSES[ps_i]
            if DEBUG_NBLK:
                blocks = blocks[:1]
            offs = []
            off = 0
            for (t0, n, mi) in blocks:
                offs.append(off)
                s_ = stg[stgi[0] % 2]
                def ld(s_=s_, t0=t0, n=n, off=off):
                    S.dma("sp", lambda e: e.dma_start(out=s_[:, :, :n], in_=v4(t2T)[:, :, t0:t0 + n]), f"stg{stgi[0] % 2}", writes=[s_.tok()])
                    S.op("pool", lambda e: e.tensor_copy(out=t2b[:, :, off:off + n], in_=s_[:, :, :n]), reads=[s_.tok()], writes=[t2b.tok()])
                ld()
                stgi[0] += 1
                off += n
            for e_i in range(nexp):
                load_w(wg_d[e_i], wgb); load_w(wu_d[e_i], wub); load_w(wd_d[e_i], wdb)
                for (t0, n, mi), off in zip(blocks, offs):
                    expert_block(e_i, off, t0, n)
            for bi, ((t0, n, mi), off) in enumerate(zip(blocks, offs)):
                finish_block(bi, off, t0, n, mi)
        S.wait_all_final("sp", outs)
        S.emit()
    return nc


def consts_p4():
    p = np.arange(128)
    G = (p[:, None] // 8 == p[None, :] // 8).astype(np.float32)
    sel = np.zeros((128, 16), np.float32); sel[np.arange(16) * 8, np.arange(16)] = 1.0
    selE = np.zeros((16, 16, 128), np.float32)
    for e in range(16):
        selE[e, e, :] = 1.0
    kv = np.zeros((128, 2), np.float32); kv[:, 0] = 1024.0; kv[:, 1] = 32.0
    return dict(G=G, sel=sel, selE=selE, kv=kv, ones=np.ones((128, 128), np.float32))


def prep_p4(inp, layer, mods_l, cmods_l, b, half, t2T, xmT, probs_lat_b, probs_ctx_b, probs_own):
    ml, mc = mods_split(mods_l[b]), mods_split(cmods_l)
    m = dict(t2T=t2T, xmT=xmT,
             pl=np.ascontiguousarray(probs_lat_b.T.reshape(128, 1024)), pc=np.ascontiguousarray(probs_ctx_b.T.reshape(128, 32)),
             po=np.ascontiguousarray(probs_own.T),
             g2v=np.ascontiguousarray(np.stack([col_layout(ml["g2"]), col_layout(mc["g2"])], 1)),
             modf=np.ascontiguousarray(np.stack([col_layout(inp["final_g"]), np.zeros((128, 8), np.float32), np.zeros((128, 8), np.float32)], 1)),
             wg=inp["exp_w_gate"][layer], wu=inp["exp_w_up"][layer], wd=inp["exp_w_down"][layer])
    m.update(consts_p4())
    return m


KSCALE = 512 ** -0.5


def build_p5():
    nc = bass.Bass("TRN2", target_bir_lowering=False)
    dt = lambda name, shape, kind="ExternalInput", d=F32: nc.dram_tensor(name, list(shape), d, kind=kind).ap()
    xT = dt("xT", [D, NSEQ])
    wup = dt("wup", [D, 2048])
    modv_d = dt("modv", [128, 5, 8])
    mcv_d = dt("mcv", [128, 16, 6])
    wbd_d = dt("wbd", [3, 128, 16, 128])
    wgt_d = dt("wgt", [128, 48, 8])
    bg_d = dt("bg", [1, 8])
    ones_d = dt("ones", [128, 128]); mask_d = dt("mask4", [64, 256])
    hout = dt("hout", [NSEQ, 2048], kind="ExternalOutput")
    xcT = dt("xcT", [2048, NSEQ], kind="ExternalOutput")
    with ExitStack() as st:
        S = Sched(nc, st)
        pool = PsumPool(S, ["pp0", "pp1", "pp2", "pp3", "pp4"])
        pAT = S.psum("pAT", [128, 512], F32)
        psm = PsumPool(S, ["psm0", "psm1"])
        ones32 = S.sbuf("ones32", [128, 128], F32); load_const(S, ones_d, ones32, "c_ones")
        mask4 = S.sbuf("mask4", [64, 256], F32); load_const(S, mask_d, mask4, "c_mask")
        modv = S.sbuf("modv", [128, 5, 8], F32); load_const(S, modv_d, modv, "c_modv")
        mcv = S.sbuf("mcv", [128, 16, 6], F32); load_const(S, mcv_d, mcv, "c_mcv")
        bgb = S.sbuf("bgb", [64, 8], F32); load_const(S, bg_d.partition_broadcast(64), bgb, "c_bg")
        epsc = S.sbuf("epsc", [128, 1], F32)
        S.op("dve", lambda e: e.memset(epsc[:], EPS), writes=[epsc.tok()])
        onec = S.sbuf("onec", [128, 1], F32)
        S.op("dve", lambda e: e.memset(onec[:], 1.0), writes=[onec.tok()])
        gs = S.sbuf("gs", [128, 2, 8], F32); make_gs(S, modv, gs, 2)
        sh = S.sbuf("sh", [128, 2, 8], F32)
        for i in range(2):
            S.op("dve", lambda e, i=i: e.tensor_copy(out=sh[:, i, :], in_=modv[:, 2 + 2 * i, :]), reads=[modv.tok()], writes=[sh.tok()])
        X = [S.sbuf(f"X{i}", [128, 8, BLK], F32) for i in range(2)]
        wupb = S.sbuf("wupb", [128, 8, 2048], BF16); load_cast_weight(S, wup, 2048, wupb, X, "X")
        wbdb = []
        for j in range(3):
            stg_ = X[j % 2]
            S.dma("sp", lambda e, stg_=stg_, j=j: e.dma_start(out=stg_[:, :, :].rearrange("p a b -> p (a b)"), in_=wbd_d[j].rearrange("p a b -> p (a b)")), f"X{j % 2}", writes=[stg_.tok()])
            wt = S.sbuf(f"wbd{j}", [128, 16, 128], BF16)
            S.op("dve", lambda e, stg_=stg_, wt=wt: e.tensor_copy(out=wt[:, :, :].rearrange("p a b -> p (a b)"), in_=stg_[:, :, :].rearrange("p a b -> p (a b)")), reads=[stg_.tok()], writes=[wt.tok()])
            wbdb.append(wt)
        wg32 = S.sbuf("wg32", [128, 48, 8], F32); load_const(S, wgt_d, wg32, "c_wgt")
        wgb = S.sbuf("wgb", [128, 48, 8], BF16)
        S.op("dve", lambda e: e.tensor_copy(out=wgb[:], in_=wg32[:]), reads=[wg32.tok()], writes=[wgb.tok()])
        NW = norm_work(S, pool, ones32, epsc)
        XN = S.sbuf("XN", [128, 8, BLK], BF16)
        XMt = S.sbuf("XMt", [128, 16, BLK], F32); XCt = S.sbuf("XCt", [128, 16, BLK], F32)
        XMb = S.sbuf("XMb", [128, 16, BLK], BF16); XCb = S.sbuf("XCb", [128, 16, BLK], BF16)
        QT = S.sbuf("QT", [128, 16, BLK], BF16); KTf = S.sbuf("KTf", [128, 16, BLK], BF16); VT = S.sbuf("VT", [128, 16, BLK], BF16)
        C32 = S.sbuf("C32", [128, 16, 516], F32); Cb = S.sbuf("Cb", [128, 16, 516], BF16)
        S.op("dve", lambda e: e.memset(C32[:], 0.0), writes=[C32.tok()])
        S.op("pool", lambda e: e.memset(Cb[:], 0.0), writes=[Cb.tok()])
        VV = S.sbuf("VV", [64, 4, 516], BF16); KTk = S.sbuf("KTk", [64, 2048], BF16)
        HO = Tile(NW["tmpn"][0:64, :, :].rearrange("p a b -> p (a b)"), "HO")
        HO.toks = NW["tmpn"].toks
        gt = S.sbuf("gt", [64, 8], F32); sp_ = S.sbuf("sp", [64, 4], F32); csp = S.sbuf("csp", [64, 4], F32)
        eb = S.sbuf("eb", [64, 4], F32); ev = S.sbuf("ev", [64, 4], F32); dl = S.sbuf("dl", [128, 4], F32)
        dd = S.sbuf("dd", [64, 4], F32); scl = S.sbuf("scl", [64, 4], F32)
        ATs = S.sbuf("ATs", [64, 256], BF16)
        xTv = xT.rearrange("(c p) n -> p c n", p=128)
        xcv = xcT.rearrange("(c p) n -> p c n", p=128)
        outs = []

        def do_block(bi, t0, n, mi):
            Xb = X[bi % 2]
            nch = n // 64
            seg = 256 if mi == 1 else 64
            S.dma("sp", lambda e: e.dma_start(out=Xb[:, :, :n], in_=xTv[:, :, t0:t0 + n]), f"X{bi % 2}", writes=[Xb.tok()])
            norm_fm(S, Xb, n, gs, sh, mi, XN, NW)
            for c in range(16):
                p = pool.next()
                for kc in range(8):
                    S.op("pe", lambda e, p=p, kc=kc, c=c: e.matmul(p[:, :n], lhsT=wupb[:, kc, c * 128:(c + 1) * 128], rhs=XN[:, kc, :n], start=(kc == 0), stop=(kc == 7)),
                         reads=[wupb.tok(), XN.tok()], writes=[p.tok()])
                S.op("act", lambda e, p=p, c=c: e.activation(out=XMt[:, c, :n], in_=p[:, :n], func=AF.Copy), reads=[p.tok()], writes=[XMt.tok(c)])
                S.op("pool", lambda e, c=c: e.tensor_scalar(out=XCt[:, c, :n], in0=XMt[:, c, :n], scalar1=mcv[:, c, 2:3], scalar2=mcv[:, c, 5:6], op0=ALU.mult, op1=ALU.add),
                     reads=[XMt.tok(c), mcv.tok()], writes=[XCt.tok(c)])
                for off in (-2, -1, 1, 2):
                    j = off + 2
                    xrv = XMt[:, c, :n].rearrange("p (s t) -> p s t", t=seg)
                    xbv = XCt[:, c, :n].rearrange("p (s t) -> p s t", t=seg)
                    if off < 0:
                        o_sl, i_sl = xbv[:, :, -off:seg], xrv[:, :, 0:seg + off]
                    else:
                        o_sl, i_sl = xbv[:, :, 0:seg - off], xrv[:, :, off:seg]
                    S.op("dve", lambda e, o_sl=o_sl, i_sl=i_sl, c=c, j=j: e.scalar_tensor_tensor(out=o_sl, in0=i_sl, scalar=mcv[:, c, j:j + 1], in1=o_sl, op0=ALU.mult, op1=ALU.add),
                         reads=[XMt.tok(c), mcv.tok(), XCt.tok(c)], writes=[XCt.tok(c)])
                S.op("act", lambda e, c=c: e.activation(out=XCt[:, c, :n], in_=XCt[:, c, :n], func=AF.Silu), reads=[XCt.tok(c)], writes=[XCt.tok(c)])
                S.op("pool", lambda e, c=c: e.tensor_copy(out=XCb[:, c, :n], in_=XCt[:, c, :n]), reads=[XCt.tok(c)], writes=[XCb.tok(c)])
                S.op("pool", lambda e, c=c: e.tensor_copy(out=XMb[:, c, :n], in_=XMt[:, c, :n]), reads=[XMt.tok(c)], writes=[XMb.tok(c)])
            outs.append(S.dma("sp", lambda e: e.dma_start(out=xcv[:, :, t0:t0 + n], in_=XCt[:, :, :n]), "XCt", reads=[XCt.tok(c) for c in range(16)]))
            for c in range(16):
                for (j, src, dst, scale) in ((0, XCb, QT, 1.0), (1, XCb, KTf, KSCALE), (2, XMb, VT, 1.0)):
                    p = pool.next()
                    S.op("pe", lambda e, p=p, j=j, c=c, src=src: e.matmul(p[:, :n], lhsT=wbdb[j][:, c, :], rhs=src[:, c, :n], start=True, stop=True),
                         reads=[wbdb[j].tok(), src.tok(c)], writes=[p.tok()])
                    if j == 1:
                        S.op("dve", lambda e, p=p, c=c, dst=dst, scale=scale: e.tensor_scalar(out=dst[:, c, :n], in0=p[:, :n], scalar1=scale, scalar2=None, op0=ALU.mult),
                             reads=[p.tok()], writes=[dst.tok(c)])
                    else:
                        S.op("act", lambda e, p=p, c=c, dst=dst: e.activation(out=dst[:, c, :n], in_=p[:, :n], func=AF.Copy), reads=[p.tok()], writes=[dst.tok(c)])
            for cc in range(nch):
                do_chunk(t0, cc)

        def do_chunk(t0, cc):
            cs = slice(cc * 64, (cc + 1) * 64)
            allq = [QT.tok(c) for c in range(16)]; allk = [KTf.tok(c) for c in range(16)]; allv = [VT.tok(c) for c in range(16)]
            pg = psm.next()
            k = 0
            for (src, toks) in ((QT, allq), (KTf, allk), (VT, allv)):
                for c in range(16):
                    S.op("pe", lambda e, k=k, c=c, src=src: e.matmul(pg[0:64, 0:8], lhsT=src[:, c, cs], rhs=wgb[:, k, :], start=(k == 0), stop=(k == 47)),
                         reads=[toks[c], wgb.tok()], writes=[pg.tok()])
                    k += 1
            S.op("dve", lambda e: e.tensor_tensor(out=gt[:], in0=pg[0:64, 0:8], in1=bgb[:], op=ALU.add), reads=[pg.tok(), bgb.tok()], writes=[gt.tok()])
            S.op("act", lambda e: e.activation(out=sp_[:], in_=gt[:, 4:8], func=AF.Exp, scale=-1.0), reads=[gt.tok()], writes=[sp_.tok()])
            S.op("act", lambda e: e.activation(out=sp_[:], in_=sp_[:], func=AF.Ln, bias=onec[0:64, 0:1], scale=1.0), reads=[sp_.tok(), onec.tok()], writes=[sp_.tok()])
            pb = psm.next()
            S.op("pe", lambda e: e.matmul(pb[0:64, 0:4], lhsT=mask4[:, 0:64], rhs=sp_[:, :], start=True, stop=True), reads=[mask4.tok(), sp_.tok()], writes=[pb.tok()])
            S.op("pe", lambda e: e.matmul(pb[:, 8:12], lhsT=ones32[0:64, :], rhs=sp_[:, :], start=True, stop=True), reads=[ones32.tok(), sp_.tok()], writes=[pb.tok()])
            S.op("dve", lambda e: e.tensor_copy(out=csp[:], in_=pb[0:64, 0:4]), reads=[pb.tok()], writes=[csp.tok()])
            S.op("act", lambda e: e.activation(out=eb[:], in_=pb[0:64, 0:4], func=AF.Exp, scale=-1.0), reads=[pb.tok()], writes=[eb.tok()])
            S.op("act", lambda e: e.activation(out=dl[:], in_=pb[:, 8:12], func=AF.Exp, scale=-1.0), reads=[pb.tok()], writes=[dl.tok()])
            S.op("dve", lambda e: e.tensor_tensor(out=ev[:], in0=gt[:, 0:4], in1=csp[:], op=ALU.add), reads=[gt.tok(), csp.tok()], writes=[ev.tok()])
            S.op("act", lambda e: e.activation(out=ev[:], in_=ev[:], func=AF.Exp), reads=[ev.tok()], writes=[ev.tok()])
            for h in range(4):
                p = pool.next()
                for j in range(4):
                    c = 4 * h + j
                    S.op("pe", lambda e, p=p, j=j, c=c: e.matmul(p[0:64, j * 128:(j + 1) * 128], lhsT=XMb[:, c, cs], rhs=wbdb[2][:, c, :], start=True, stop=True),
                         reads=[XMb.tok(c), wbdb[2].tok()], writes=[p.tok()])
                S.op("dve", lambda e, p=p, h=h: e.tensor_scalar(out=VV[:, h, 0:512], in0=p[0:64, :], scalar1=ev[:, h:h + 1], scalar2=None, op0=ALU.mult),
                     reads=[p.tok(), ev.tok()], writes=[VV.tok(h)])
                S.op("pool", lambda e, h=h: e.tensor_copy(out=VV[:, h, 512:513], in_=ev[:, h:h + 1]), reads=[ev.tok()], writes=[VV.tok(h)])
            for h in range(4):
                p = pool.next()
                for j in range(4):
                    c = 4 * h + j
                    S.op("pe", lambda e, p=p, j=j, c=c: e.matmul(p[0:64, j * 128:(j + 1) * 128], lhsT=XCb[:, c, cs], rhs=wbdb[1][:, c, :], start=True, stop=True),
                         reads=[XCb.tok(c), wbdb[1].tok()], writes=[p.tok()])
                S.op("dve", lambda e, p=p, h=h: e.tensor_scalar(out=KTk[:, h * 512:(h + 1) * 512], in0=p[0:64, :], scalar1=dl[0:64, h:h + 1], scalar2=KSCALE, op0=ALU.mult, op1=ALU.mult),
                     reads=[p.tok(), dl.tok()], writes=[KTk.tok(h)])
            for h in range(4):
                for j in range(4):
                    c = 4 * h + j
                    S.op("pe", lambda e, h=h, j=j, c=c: e.matmul(pAT[0:64, h * 64:(h + 1) * 64], lhsT=KTf[:, c, cs], rhs=QT[:, c, cs], start=(j == 0), stop=(j == 3)),
                         reads=[KTf.tok(c), QT.tok(c)], writes=[pAT.tok()])
            S.op("dve", lambda e: e.tensor_tensor(out=ATs[:], in0=pAT[0:64, 0:256], in1=mask4[:], op=ALU.mult), reads=[pAT.tok(), mask4.tok()], writes=[ATs.tok()])
            pd = psm.next()
            for h in range(4):
                S.op("pe", lambda e, h=h: e.matmul(pd[0:64, h:h + 1], lhsT=ATs[:, h * 64:(h + 1) * 64], rhs=VV[:, h, 512:513], start=True, stop=False),
                     reads=[ATs.tok(), VV.tok(h)], writes=[pd.tok()])
                for j in range(4):
                    c = 4 * h + j
                    S.op("pe", lambda e, h=h, j=j, c=c: e.matmul(pd[0:64, h:h + 1], lhsT=QT[:, c, cs], rhs=Cb[:, c, 512:513], start=False, stop=(j == 3)),
                         reads=[QT.tok(c), Cb.tok(c)], writes=[pd.tok()])
            S.op("dve", lambda e: e.tensor_tensor(out=dd[:], in0=pd[0:64, 0:4], in1=eb[:], op=ALU.mult), reads=[pd.tok(), eb.tok()], writes=[dd.tok()])
            S.op("dve", lambda e: e.tensor_scalar(out=scl[:], in0=dd[:], scalar1=-1.0, scalar2=None, op0=ALU.mult), reads=[dd.tok()], writes=[scl.tok()])
            S.op("dve", lambda e: e.tensor_tensor(out=dd[:], in0=dd[:], in1=scl[:], op=ALU.max), reads=[dd.tok(), scl.tok()], writes=[dd.tok()])
            S.op("dve", lambda e: e.tensor_scalar(out=dd[:], in0=dd[:], scalar1=1.0, scalar2=None, op0=ALU.max), reads=[dd.tok()], writes=[dd.tok()])
            S.op("dve", lambda e: e.reciprocal(out=dd[:], in_=dd[:]), reads=[dd.tok()], writes=[dd.tok()])
            S.op("dve", lambda e: e.tensor_tensor(out=scl[:], in0=dd[:], in1=eb[:], op=ALU.mult), reads=[dd.tok(), eb.tok()], writes=[scl.tok()])
            for h in range(4):
                p = pool.next()
                S.op("pe", lambda e, p=p, h=h: e.matmul(p[0:64, :], lhsT=ATs[:, h * 64:(h + 1) * 64], rhs=VV[:, h, 0:512], start=True, stop=False),
                     reads=[ATs.tok(), VV.tok(h)], writes=[p.tok()])
                for j in range(4):
                    c = 4 * h + j
                    S.op("pe", lambda e, p=p, j=j, c=c: e.matmul(p[0:64, :], lhsT=QT[:, c, cs], rhs=Cb[:, c, 0:512], start=False, stop=(j == 3)),
                         reads=[QT.tok(c), Cb.tok(c)], writes=[p.tok()])
                if h % 2 == 0:
                    S.op("act", lambda e, p=p, h=h: e.activation(out=HO[:, h * 512:(h + 1) * 512], in_=p[0:64, :], func=AF.Copy, scale=scl[:, h:h + 1]),
                         reads=[p.tok(), scl.tok()], writes=[HO.tok()])
                else:
                    S.op("dve", lambda e, p=p, h=h: e.tensor_scalar(out=HO[:, h * 512:(h + 1) * 512], in0=p[0:64, :], scalar1=scl[:, h:h + 1], scalar2=None, op0=ALU.mult),
                         reads=[p.tok(), scl.tok()], writes=[HO.tok()])
            r0 = t0 + cc * 64
            outs.append(S.dma("sp", lambda e: e.dma_start(out=hout[r0:r0 + 64, :], in_=HO[:]), "HO", reads=[HO.tok()]))
            for h in range(4):
                for j in range(4):
                    c = 4 * h + j
                    p = pool.next()
                    S.op("pe", lambda e, p=p, h=h, c=c: e.matmul(p[:, :], lhsT=KTk[:, c * 128:(c + 1) * 128], rhs=VV[:, h, 0:512], start=True, stop=True),
                         reads=[KTk.tok(h), VV.tok(h)], writes=[p.tok()])
                    pn = psm.next()
                    S.op("pe", lambda e, pn=pn, h=h, c=c: e.matmul(pn[:, 0:1], lhsT=KTk[:, c * 128:(c + 1) * 128], rhs=VV[:, h, 512:513], start=True, stop=True),
                         reads=[KTk.tok(h), VV.tok(h)], writes=[pn.tok()])
                    S.op("dve", lambda e, p=p, h=h, c=c: e.scalar_tensor_tensor(out=C32[:, c, 0:512], in0=C32[:, c, 0:512], scalar=dl[:, h:h + 1], in1=p[:, :], op0=ALU.mult, op1=ALU.add),
                         reads=[C32.tok(c), dl.tok(), p.tok()], writes=[C32.tok(c)])
                    S.op("dve", lambda e, pn=pn, h=h, c=c: e.scalar_tensor_tensor(out=C32[:, c, 512:513], in0=C32[:, c, 512:513], scalar=dl[:, h:h + 1], in1=pn[:, 0:1], op0=ALU.mult, op1=ALU.add),
                         reads=[C32.tok(c), dl.tok(), pn.tok()], writes=[C32.tok(c)])
                    eng = "act" if c % 2 == 0 else "pool"
                    if eng == "act":
                        S.op("act", lambda e, c=c: e.activation(out=Cb[:, c, 0:513], in_=C32[:, c, 0:513], func=AF.Copy), reads=[C32.tok(c)], writes=[Cb.tok(c)])
                    else:
                        S.op("pool", lambda e, c=c: e.tensor_copy(out=Cb[:, c, 0:513], in_=C32[:, c, 0:513]), reads=[C32.tok(c)], writes=[Cb.tok(c)])

        for bi, (t0, n, mi) in enumerate(seq_blocks()):
            do_block(bi, t0, n, mi)
        S.wait_all_final("sp", outs)
        S.emit()
    return nc


def bd_tiles(w):
    out = np.zeros((128, 16, 128), np.float32)
    for c in range(16):
        for j in range(32):
            out[4 * j:4 * j + 4, c, 4 * j:4 * j + 4] = w[c * 32 + j]
    return out


def prep_p5(inp, mods1, cmods1, b, d):
    ml, mc = mods_split(mods1[b]), mods_split(cmods1)
    cw = inp["m_conv_w"][0]
    z = np.zeros_like(cw[0])
    taps = [cw[0], cw[1], cw[2], cw[3], z] if d == 0 else [z, cw[3], cw[2], cw[1], cw[0]]
    mcv = np.stack([col_layout(v) for v in taps + [inp["m_conv_b"][0]]], 2)
    wgt = inp["m_w_gates"][0, d]
    m = dict(wup=np.ascontiguousarray(inp["m_w_up"][0][:, :2048]),
             modv=modv_pack(inp["norm1_g"][1], ml, mc, "sc1", "sh1"), mcv=np.ascontiguousarray(mcv),
             wbd=np.ascontiguousarray(np.stack([bd_tiles(inp["m_wq"][0]), bd_tiles(inp["m_wk"][0]), bd_tiles(inp["m_wv"][0])], 0)),
             wgt=np.ascontiguousarray(wgt.reshape(48, 128, 8).transpose(1, 0, 2)),
             bg=np.ascontiguousarray(inp["m_b_gates"][0, d][None, :]))
    c = consts_p1()
    m["ones"] = c["ones"]; m["mask4"] = c["mask4"]
    return m


def build_p6():
    nc = bass.Bass("TRN2", target_bir_lowering=False)
    dt = lambda name, shape, kind="ExternalInput", d=F32: nc.dram_tensor(name, list(shape), d, kind=kind).ap()
    xT = dt("xT", [D, NTOK])
    hf_d, hb_d, xc_d = dt("hf", [2048, NTOK]), dt("hb", [2048, NTOK]), dt("xc", [2048, NTOK])
    wz = dt("wz", [D, 2048]); wdn = dt("wdn", [2048, D]); rw_d = dt("rw", [128, 8, 16])
    modv_d = dt("modv", [128, 5, 8]); modv2_d = dt("modv2", [128, 5, 8]); g1_d = dt("g1v", [128, 2, 8])
    sk_d = dt("skip", [128, 16]); ng_d = dt("ng", [128, 16]); ones_d = dt("ones", [128, 128])
    xmT = dt("xmT", [D, NTOK], kind="ExternalOutput")
    t2T = dt("t2T", [D, NTOK], kind="ExternalOutput")
    probs_d = dt("probs", [NTOK, 16], kind="ExternalOutput")
    with ExitStack() as st:
        S = Sched(nc, st)
        pool = PsumPool(S, ["pp0", "pp1", "pp2", "pp3", "pp4", "pp5"])
        ones32 = S.sbuf("ones32", [128, 128], F32); load_const(S, ones_d, ones32, "c_ones")
        modv = S.sbuf("modv", [128, 5, 8], F32); load_const(S, modv_d, modv, "c_modv")
        modv2 = S.sbuf("modv2", [128, 5, 8], F32); load_const(S, modv2_d, modv2, "c_modv2")
        g1v = S.sbuf("g1v", [128, 2, 8], F32); load_const(S, g1_d, g1v, "c_g1")
        skp = S.sbuf("skp", [128, 16], F32); load_const(S, sk_d, skp, "c_sk")
        ng = S.sbuf("ng", [128, 16], F32); load_const(S, ng_d, ng, "c_ng")
        rw = S.sbuf("rw", [128, 8, 16], F32); load_const(S, rw_d, rw, "c_rw")
        epsc = S.sbuf("epsc", [128, 1], F32)
        S.op("dve", lambda e: e.memset(epsc[:], EPS), writes=[epsc.tok()])
        gs1 = S.sbuf("gs1", [128, 2, 8], F32); make_gs(S, modv, gs1, 2)
        gs2 = S.sbuf("gs2", [128, 2, 8], F32); make_gs(S, modv2, gs2, 2)
        sh1 = S.sbuf("sh1", [128, 2, 8], F32); sh2 = S.sbuf("sh2", [128, 2, 8], F32)
        for i in range(2):
            S.op("dve", lambda e, i=i: e.tensor_copy(out=sh1[:, i, :], in_=modv[:, 2 + 2 * i, :]), reads=[modv.tok()], writes=[sh1.tok()])
            S.op("dve", lambda e, i=i: e.tensor_copy(out=sh2[:, i, :], in_=modv2[:, 2 + 2 * i, :]), reads=[modv2.tok()], writes=[sh2.tok()])
        X = [S.sbuf(f"X{i}", [128, 8, BLK], F32) for i in range(2)]
        wzb = S.sbuf("wzb", [128, 8, 2048], BF16); load_cast_weight(S, wz, 2048, wzb, X, "X")
        wdnb = S.sbuf("wdnb", [128, 16, 1024], BF16)
        wdv = wdn.rearrange("(c p) n -> p c n", p=128)
        for i, c0 in enumerate(range(0, 16, 4)):
            for j, n0 in enumerate(range(0, 1024, 512)):
                s_ = X[(2 * i + j) % 2]
                S.dma("sp", lambda e, s_=s_, c0=c0, n0=n0: e.dma_start(out=s_[:, :, :].rearrange("p a (b c) -> p (a b) c", b=2)[:, 0:4, :].rearrange("p a c -> p a c"), in_=wdv[:, c0:c0 + 4, n0:n0 + 512]) if False else
                      e.dma_start(out=s_[:, 0:4, :], in_=wdv[:, c0:c0 + 4, n0:n0 + 256]), f"X{(2 * i + j) % 2}", writes=[s_.tok()])
                S.dma("sp", lambda e, s_=s_, c0=c0, n0=n0: e.dma_start(out=s_[:, 4:8, :], in_=wdv[:, c0:c0 + 4, n0 + 256:n0 + 512]), f"X{(2 * i + j) % 2}", writes=[s_.tok()])
                S.op("dve", lambda e, s_=s_, c0=c0, n0=n0: e.tensor_copy(out=wdnb[:, c0:c0 + 4, n0:n0 + 256], in_=s_[:, 0:4, :]), reads=[s_.tok()], writes=[wdnb.tok()])
                S.op("pool", lambda e, s_=s_, c0=c0, n0=n0: e.tensor_copy(out=wdnb[:, c0:c0 + 4, n0 + 256:n0 + 512], in_=s_[:, 4:8, :]), reads=[s_.tok()], writes=[wdnb.tok()])
        NW = norm_work(S, pool, ones32, epsc)
        RW = router_work(S)
        XN = S.sbuf("XN", [128, 8, BLK], BF16)
        sz = S.sbuf("sz", [128, 16, BLK], F32)
        HF = S.sbuf("HF", [128, 16, BLK], F32); HB = S.sbuf("HB", [128, 16, BLK], F32); XC = S.sbuf("XC", [128, 16, BLK], F32)
        mean = S.sbuf("mean", [128, BLK], F32); hsd = S.sbuf("hsd", [128, BLK], F32); hrs = S.sbuf("hrs", [128, BLK], F32)
        sqh = S.sbuf("sqh", [128, 4, BLK], F32); t1 = S.sbuf("t1", [128, BLK], F32)
        mrg = S.sbuf("mrg", [128, 16, BLK], BF16)
        XM = [S.sbuf(f"XM{i}", [128, 8, BLK], F32) for i in range(2)]
        T2 = S.sbuf("T2", [128, 8, BLK], F32)
        xTv = xT.rearrange("(c p) n -> p c n", p=128)
        v4 = lambda a: a.rearrange("(c p) n -> p c n", p=128)
        outs = []

        def do_block(bi, t0, n, mi):
            b2 = bi % 2
            Xb, XMb = X[b2], XM[b2]
            S.dma("sp", lambda e: e.dma_start(out=Xb[:, :, :n], in_=xTv[:, :, t0:t0 + n]), f"X{b2}", writes=[Xb.tok()])
            for (src, dst, nm) in ((hf_d, HF, "HF"), (hb_d, HB, "HB"), (xc_d, XC, "XC")):
                S.dma("sp", lambda e, src=src, dst=dst: e.dma_start(out=dst[:, :, :n], in_=v4(src)[:, :, t0:t0 + n]), nm, writes=[dst.tok()])
            norm_fm(S, Xb, n, gs1, sh1, mi, XN, NW)
            for c in range(16):
                p = pool.next()
                for kc in range(8):
                    S.op("pe", lambda e, p=p, kc=kc, c=c: e.matmul(p[:, :n], lhsT=wzb[:, kc, c * 128:(c + 1) * 128], rhs=XN[:, kc, :n], start=(kc == 0), stop=(kc == 7)),
                         reads=[wzb.tok(), XN.tok()], writes=[p.tok()])
                S.op("act", lambda e, p=p, c=c: e.activation(out=sz[:, c, :n], in_=p[:, :n], func=AF.Silu), reads=[p.tok()], writes=[sz.tok()])
            S.op("pool", lambda e: e.tensor_tensor(out=HF[:, :, :n], in0=HF[:, :, :n], in1=HB[:, :, :n], op=ALU.add), reads=[HF.tok(), HB.tok()], writes=[HF.tok()])
            for c in range(16):
                S.op("pool", lambda e, c=c: e.tensor_scalar(out=XC[:, c, :n], in0=XC[:, c, :n], scalar1=skp[:, c:c + 1], scalar2=None, op0=ALU.mult), reads=[XC.tok(), skp.tok()], writes=[XC.tok()])
            for h in range(4):
                hs = slice(4 * h, 4 * h + 4)
                p = pool.next()
                for j in range(4):
                    S.op("pe", lambda e, p=p, j=j, h=h: e.matmul(p[:, :n], lhsT=ones32[:, :], rhs=HF[:, 4 * h + j, :n], start=(j == 0), stop=(j == 3)), reads=[ones32.tok(), HF.tok()], writes=[p.tok()])
                S.op("act", lambda e, p=p: e.activation(out=mean[:, :n], in_=p[:, :n], func=AF.Copy, scale=1.0 / 512), reads=[p.tok()], writes=[mean.tok()])
                for j in range(4):
                    S.op("dve", lambda e, j=j, h=h: e.tensor_tensor(out=HF[:, 4 * h + j, :n], in0=HF[:, 4 * h + j, :n], in1=mean[:, :n], op=ALU.subtract), reads=[HF.tok(), mean.tok()], writes=[HF.tok()])
                S.op("act", lambda e, hs=hs: e.activation(out=sqh[:, :, :n], in_=HF[:, hs, :n], func=AF.Square), reads=[HF.tok()], writes=[sqh.tok()])
                p = pool.next()
                for j in range(4):
                    S.op("pe", lambda e, p=p, j=j: e.matmul(p[:, :n], lhsT=ones32[:, :], rhs=sqh[:, j, :n], start=(j == 0), stop=(j == 3)), reads=[ones32.tok(), sqh.tok()], writes=[p.tok()])
                S.op("act", lambda e, p=p: e.activation(out=hsd[:, :n], in_=p[:, :n], func=AF.Sqrt, bias=epsc[:, 0:1], scale=1.0 / 512), reads=[p.tok(), epsc.tok()], writes=[hsd.tok()])
                S.op("dve", lambda e: e.reciprocal(out=hrs[:, :n], in_=hsd[:, :n]), reads=[hsd.tok()], writes=[hrs.tok()])
                for j in range(4):
                    c = 4 * h + j
                    S.op("dve", lambda e, c=c: e.tensor_tensor(out=t1[:, :n], in0=HF[:, c, :n], in1=hrs[:, :n], op=ALU.mult), reads=[HF.tok(), hrs.tok()], writes=[t1.tok()])
                    S.op("dve", lambda e, c=c: e.scalar_tensor_tensor(out=t1[:, :n], in0=t1[:, :n], scalar=ng[:, c:c + 1], in1=XC[:, c, :n], op0=ALU.mult, op1=ALU.add),
                         reads=[t1.tok(), ng.tok(), XC.tok()], writes=[t1.tok()])
                    S.op("pool", lambda e, c=c: e.tensor_tensor(out=mrg[:, c, :n], in0=t1[:, :n], in1=sz[:, c, :n], op=ALU.mult), reads=[t1.tok(), sz.tok()], writes=[mrg.tok()])
            for oc in range(8):
                p = pool.next()
                for c in range(16):
                    S.op("pe", lambda e, p=p, c=c, oc=oc: e.matmul(p[:, :n], lhsT=wdnb[:, c, oc * 128:(oc + 1) * 128], rhs=mrg[:, c, :n], start=(c == 0), stop=(c == 15)),
                         reads=[wdnb.tok(), mrg.tok()], writes=[p.tok()])
                S.op("dve", lambda e, p=p, oc=oc: e.scalar_tensor_tensor(out=XMb[:, oc, :n], in0=p[:, :n], scalar=g1v[:, mi, oc:oc + 1], in1=Xb[:, oc, :n], op0=ALU.mult, op1=ALU.add),
                     reads=[p.tok(), g1v.tok(), Xb.tok()], writes=[XMb.tok()])
            outs.append(S.dma("sp", lambda e: e.dma_start(out=v4(xmT)[:, :, t0:t0 + n], in_=XMb[:, :, :n]), f"XM{b2}", reads=[XMb.tok()]))
            norm_fm(S, XMb, n, gs2, sh2, mi, T2, NW)
            outs.append(S.dma("sp", lambda e: e.dma_start(out=v4(t2T)[:, :, t0:t0 + n], in_=T2[:, :, :n]), "T2", reads=[T2.tok()]))
            router_softmax(S, T2, n, t0, rw, probs_d, pool, RW, outs, "p6")

        for bi, (t0, n, mi) in enumerate(tok_blocks()):
            do_block(bi, t0, n, mi)
        S.wait_all_final("sp", outs)
        S.emit()
    return nc


def prep_p6(inp, mods1, cmods1, b, half, x_lat, x_ctx, hf, hb, xc):
    ml, mc = mods_split(mods1[b]), mods_split(cmods1)
    T = lambda lat, ctx: np.ascontiguousarray(half_tokens(lat, ctx, half).T)
    return dict(xT=T(x_lat, x_ctx), hf=T(hf["lat"], hf["ctx"]), hb=T(hb["lat"], hb["ctx"]), xc=T(xc["lat"], xc["ctx"]),
                wz=np.ascontiguousarray(inp["m_w_up"][0][:, 2048:]), wdn=np.ascontiguousarray(inp["m_w_down"][0]),
                rw=np.ascontiguousarray(inp["router_w"][1].reshape(8, 128, 16).transpose(1, 0, 2)),
                modv=modv_pack(inp["norm1_g"][1], ml, mc, "sc1", "sh1"), modv2=modv_pack(inp["norm2_g"][1], ml, mc, "sc2", "sh2"),
                g1v=np.ascontiguousarray(np.stack([col_layout(ml["g1"]), col_layout(mc["g1"])], 1)),
                skip=col_layout(inp["m_skip"][0]), ng=col_layout(inp["m_norm_g"][0]), ones=np.ones((128, 128), np.float32))


def _run(nc, maps):
    return run_bass_kernel_spmd(nc, maps, core_ids=list(range(len(maps)))).results


def _moe_layer(inp, layer, mods_l, cmods_l, res_merge, final):
    maps = []
    for b in range(4):
        pr0, pr1 = res_merge[2 * b]["probs"], res_merge[2 * b + 1]["probs"]
        pl = np.concatenate([pr0[:4096], pr1[:4096]], 0)
        pc = np.concatenate([pr0[4096:], pr1[4096:]], 0)
        for half in range(2):
            r = res_merge[2 * b + half]
            maps.append(prep_p4(inp, layer, mods_l, cmods_l, b, half, np.ascontiguousarray(r["t2T"]), np.ascontiguousarray(r["xmT"]), pl, pc, r["probs"]))
    res = _run(build_p4(final), maps)
    return [np.ascontiguousarray(r["xoT"].T) for r in res]


def kernel(**inp):
    inp = {k: np.asarray(v) for k, v in inp.items()}
    mods = run_p0(inp)
    mods0, cmods0, mods1, cmods1 = mods[0, :4], mods[0, 4], mods[1, :4], mods[1, 4]
    maps = [prep_p1(inp, mods0, cmods0, b, d) for b in range(4) for d in range(2)]
    r1 = _run(build_p1(), maps)
    del maps
    maps = []
    for b in range(4):
        og, hl = [], []
        for d in range(2):
            r = r1[2 * b + d]
            c, l = unseq(r["og"], d); og.append(dict(ctx=c, lat=l))
            c, l = unseq(r["hlT"].T, d); hl.append(dict(ctx=c, lat=l))
        for half in range(2):
            maps.append(prep_p2(inp, mods0, cmods0, b, half, og[0], og[1], hl[0], hl[1]))
    r2 = _run(build_p2(), maps)
    del maps, r1
    xo = _moe_layer(inp, 0, mods0, cmods0, r2, False)
    del r2
    x1 = [np.concatenate([xo[2 * b][:4096], xo[2 * b + 1][:4096]], 0) for b in range(4)]
    c1 = [np.concatenate([xo[2 * b][4096:], xo[2 * b + 1][4096:]], 0) for b in range(4)]
    maps = []
    for b in range(4):
        for d in range(2):
            m = prep_p5(inp, mods1, cmods1, b, d)
            m["xT"] = seq_T(c1[b], x1[b], d)
            maps.append(m)
    r5 = _run(build_p5(), maps)
    del maps
    maps = []
    for b in range(4):
        c, l = unseq(r5[2 * b]["hout"], 0); hf = dict(ctx=c, lat=l)
        c, l = unseq(r5[2 * b + 1]["hout"], 1); hb = dict(ctx=c, lat=l)
        c, l = unseq(r5[2 * b]["xcT"].T, 0); xc = dict(ctx=c, lat=l)
        for half in range(2):
            maps.append(prep_p6(inp, mods1, cmods1, b, half, x1[b], c1[b], hf, hb, xc))
    r6 = _run(build_p6(), maps)
    del maps, r5
    xo = _moe_layer(inp, 1, mods1, cmods1, r6, True)
    out = np.stack([np.concatenate([xo[2 * b][:4096], xo[2 * b + 1][:4096]], 0) for b in range(4)], 0)
    return np.ascontiguousarray(out.astype(np.float32))
```

```python
import numpy as np
from contextlib import ExitStack
import concourse.bass as bass
import concourse.mybir as mybir
from concourse.bass_utils import run_bass_kernel_spmd

F32 = mybir.dt.float32
BF16 = mybir.dt.bfloat16
I32 = mybir.dt.int32
U32 = mybir.dt.uint32
AF = mybir.ActivationFunctionType
ALU = mybir.AluOpType
AX = mybir.AxisListType


class Tok:
    __slots__ = ("w", "r", "name")

    def __init__(self, name=""):
        self.w = None
        self.r = []
        self.name = name


class Tile:
    def __init__(self, t, name):
        self.t = t
        self.name = name
        self.toks = {}

    def tok(self, key=None):
        k = self.toks.get(key)
        if k is None:
            k = self.toks[key] = Tok(f"{self.name}:{key}")
        return k

    def __getitem__(self, idx):
        return self.t[idx]


class Sched:
    ENGS = ("pe", "dve", "act", "pool", "sp")
    EPOCH = 1500

    def __init__(self, nc, stack):
        self.nc = nc
        self.stack = stack
        self.ops = {e: [] for e in self.ops_engines()}
        self.count = {e: 0 for e in self.ops_engines()}
        self.sems = {}
        self.dma_count = {}
        self.waited = {e: {} for e in self.ops_engines()}
        self.n_sem = 0
        self.final = []

    def ops_engines(self):
        return self.ENGS

    def sbuf(self, name, shape, dt):
        t = self.stack.enter_context(self.nc.sbuf_tensor("sb_" + name, list(shape), dt))
        return Tile(t, name)

    def psum(self, name, shape, dt):
        t = self.stack.enter_context(self.nc.psum_tensor("ps_" + name, list(shape), dt))
        return Tile(t, name)

    def dsem(self, name):
        key = ("dma", name)
        if key not in self.sems:
            self.sems[key] = self.stack.enter_context(self.nc.semaphore(f"d_{name}"))
            self.dma_count[key] = 0
        return key

    def _deps(self, reads, writes):
        deps = []
        for t in reads:
            if t.w is not None:
                deps.append(t.w)
        for t in writes:
            if t.w is not None:
                deps.append(t.w)
            deps.extend(t.r)
        return deps

    def _commit(self, comp, reads, writes):
        for t in writes:
            t.w = comp
            t.r = []
        for t in reads:
            if t not in writes:
                t.r.append(comp)

    def op(self, eng, fn, reads=(), writes=()):
        reads = [r for r in reads]
        writes = [w for w in writes]
        deps = self._deps(reads, writes)
        self.count[eng] += 1
        ep = (self.count[eng] - 1) // self.EPOCH
        key = ("eng", eng, ep)
        if key not in self.sems:
            self.sems[key] = self.stack.enter_context(self.nc.semaphore(f"s_{eng}_{ep}"))
        comp = (key, self.count[eng] - ep * self.EPOCH)
        waits = self._filter_waits(eng, deps, own=None, own_val=None)
        self.ops[eng].append((waits, fn, comp))
        self._commit(comp, reads, writes)
        return comp

    def dma(self, eng, fn, semname, reads=(), writes=()):
        key = self.dsem(semname)
        deps = self._deps(list(reads), list(writes))
        self.dma_count[key] += 16
        comp = (key, self.dma_count[key])
        waits = self._filter_waits(eng, deps, own=None, own_val=None)
        self.ops[eng].append((waits, fn, comp))
        self._commit(comp, list(reads), list(writes))
        return comp

    def _filter_waits(self, eng, deps, own, own_val):
        need = {}
        for (k, v) in deps:
            if need.get(k, -1) < v:
                need[k] = v
        out = []
        wd = self.waited[eng]
        for k, v in need.items():
            if wd.get(k, -1) >= v:
                continue
            wd[k] = v
            out.append((k, v))
        return out

    def wait_all_final(self, eng, comps):
        waits = self._filter_waits(eng, comps, None, None)
        self.ops[eng].append((waits, None, None))

    def emit(self):
        nc = self.nc
        sems = self.sems
        ops = self.ops
        with nc.Block() as block:
            def mk(e):
                def body(engine):
                    for (waits, fn, comp) in ops[e]:
                        for (k, v) in waits:
                            engine.wait_ge(sems[k], v)
                        if fn is None:
                            continue
                        ins = fn(engine)
                        if comp[0][0] == "eng":
                            ins.then_inc(sems[comp[0]], 1)
                        else:
                            ins.then_inc(sems[comp[0]], 16)
                return body
            block.tensor(mk("pe"))
            block.vector(mk("dve"))
            block.scalar(mk("act"))
            block.gpsimd(mk("pool"))
            block.sync(mk("sp"))


D = 1024
NCTX = 256
NLAT = 8192
NSEQ = NCTX + NLAT
EPS = 1e-6
BLK = 256
DEBUG_NBLK = 0


def col_layout(v):
    v = np.asarray(v, np.float32)
    return np.ascontiguousarray(v.reshape(-1, 128).T)


class PsumPool:
    def __init__(self, S, names, shape=(128, 512), dt=F32):
        self.tiles = [S.psum(nm, list(shape), dt) for nm in names]
        self.i = 0

    def next(self):
        t = self.tiles[self.i % len(self.tiles)]
        self.i += 1
        return t


def load_const(S, dram_ap, tile, name, eng="sp"):
    S.dma(eng, lambda e: e.dma_start(out=tile[:], in_=dram_ap), name, writes=[tile.tok()])


def norm_fm(S, X, n, gs, sh, mi, XN, W):
    sq, rstd, sd, tmpn, ones32, pN = W["sq"], W["rstd"], W["sd"], W["tmpn"], W["ones32"], W["pool"].next()
    S.op("act", lambda e: e.activation(out=sq[:, :, :n], in_=X[:, :, :n], func=AF.Square),
         reads=[X.tok()], writes=[sq.tok()])
    for c in range(8):
        S.op("pe", lambda e, c=c: e.matmul(pN[:, :n], lhsT=ones32[:, :], rhs=sq[:, c, :n], start=(c == 0), stop=(c == 7)),
             reads=[ones32.tok(), sq.tok()], writes=[pN.tok()])
    S.op("act", lambda e: e.activation(out=sd[:, :n], in_=pN[:, :n], func=AF.Sqrt, bias=W["epsc"][:, 0:1], scale=1.0 / D),
         reads=[pN.tok(), W["epsc"].tok()], writes=[sd.tok()])
    S.op("dve", lambda e: e.reciprocal(out=rstd[:, :n], in_=sd[:, :n]), reads=[sd.tok()], writes=[rstd.tok()])
    S.op("pool", lambda e: e.tensor_tensor(out=tmpn[:, :, :n], in0=X[:, :, :n],
                                           in1=rstd[:, :n].unsqueeze(1).to_broadcast([128, 8, n]), op=ALU.mult),
         reads=[X.tok(), rstd.tok()], writes=[tmpn.tok()])
    for c in range(8):
        S.op("act", lambda e, c=c: e.activation(out=XN[:, c, :n], in_=tmpn[:, c, :n], func=AF.Identity,
                                                bias=sh[:, mi, c:c + 1], scale=gs[:, mi, c:c + 1]),
             reads=[tmpn.tok(), gs.tok(), sh.tok()], writes=[XN.tok()])


def norm_work(S, pool, ones32, epsc):
    tmpn = S.sbuf("n_tmp", [128, 8, BLK], F32)
    return dict(sq=tmpn, rstd=S.sbuf("n_rstd", [128, BLK], F32),
                sd=S.sbuf("n_sd", [128, BLK], F32), tmpn=tmpn,
                ones32=ones32, pool=pool, epsc=epsc)


def make_gs(S, modv, gs, nsets):
    for i in range(nsets):
        S.op("dve", lambda e, i=i: e.scalar_tensor_tensor(out=gs[:, i, :], in0=modv[:, 1 + 2 * i, :], scalar=1.0,
                                                          in1=modv[:, 0, :], op0=ALU.add, op1=ALU.mult),
             reads=[modv.tok()], writes=[gs.tok()])


def load_cast_weight(S, w_dram, ncols, wb, st, stage_name, chunk=BLK, kchunks=8):
    wv = w_dram.rearrange("(c p) n -> p c n", p=128)
    i = 0
    for c0 in range(0, ncols, chunk):
        cw = min(chunk, ncols - c0)
        s = st[i % 2]
        S.dma("sp", lambda e, s=s, c0=c0, cw=cw: e.dma_start(out=s[:, :, :cw], in_=wv[:, :, c0:c0 + cw]),
              f"{stage_name}{i % 2}", writes=[s.tok()])
        eng = "dve" if i % 2 == 0 else "pool"
        S.op(eng, lambda e, s=s, c0=c0, cw=cw: e.tensor_copy(out=wb[:, :, c0:c0 + cw], in_=s[:, :, :cw]),
             reads=[s.tok()], writes=[wb.tok()])
        i += 1


P1_WCOLS = 1552


def seq_blocks():
    blocks = [(0, NCTX, 1)]
    for i in range(NLAT // BLK):
        blocks.append((NCTX + i * BLK, BLK, 0))
    if DEBUG_NBLK:
        blocks = blocks[:DEBUG_NBLK]
    return blocks


def build_p1():
    nc = bass.Bass("TRN2", target_bir_lowering=False)
    dt = lambda name, shape, kind="ExternalInput", d=F32: nc.dram_tensor(name, list(shape), d, kind=kind).ap()
    xT = dt("xT", [D, NSEQ])
    w1 = dt("w1", [D, P1_WCOLS])
    modv_d = dt("modv", [128, 5, 8])
    wa2_d = dt("wa2", [16, 256])
    ba_d = dt("ba", [64, 4])
    lruc_d = dt("lruc", [128, 4, 9])
    wbd_d = dt("wbd", [128, 2, 4, 128])
    ones_d = dt("ones", [128, 128])
    mask_d = dt("mask4", [64, 256])
    ident_d = dt("ident", [64, 64])
    rmask_d = dt("rmask", [128, BLK])
    og = dt("og", [NSEQ, 512], kind="ExternalOutput")
    hlT = dt("hlT", [512, NSEQ], kind="ExternalOutput")
    with ExitStack() as st:
        S = Sched(nc, st)
        pool = PsumPool(S, ["pp0", "pp1", "pp2"])
        pAT = S.psum("pAT", [128, 512], F32)
        pO = S.psum("pO", [128, 512], F32)
        pU = S.psum("pU", [128, 512], F32)
        pTR = S.psum("pTR", [64, 4, 256], BF16)
        ones32 = S.sbuf("ones32", [128, 128], F32); load_const(S, ones_d, ones32, "c_ones")
        mask4 = S.sbuf("mask4", [64, 256], F32); load_const(S, mask_d, mask4, "c_mask")
        ident32 = S.sbuf("ident32", [64, 64], F32); load_const(S, ident_d, ident32, "c_ident")
        rmask = S.sbuf("rmask", [128, BLK], F32); load_const(S, rmask_d, rmask, "c_rmask")
        modv = S.sbuf("modv", [128, 5, 8], F32); load_const(S, modv_d, modv, "c_modv")
        wa2 = S.sbuf("wa2", [16, 256], F32); load_const(S, wa2_d, wa2, "c_wa2")
        ba = S.sbuf("ba", [64, 4], F32); load_const(S, ba_d, ba, "c_ba")
        lruc = S.sbuf("lruc", [128, 4, 9], F32); load_const(S, lruc_d, lruc, "c_lruc")
        wbd32 = S.sbuf("wbd32", [128, 2, 4, 128], F32); load_const(S, wbd_d, wbd32, "c_wbd")
        identb = S.sbuf("identb", [64, 64], BF16)
        S.op("dve", lambda e: e.tensor_copy(out=identb[:], in_=ident32[:]), reads=[ident32.tok()], writes=[identb.tok()])
        wbdb = S.sbuf("wbdb", [128, 2, 4, 128], BF16)
        S.op("dve", lambda e: e.tensor_copy(out=wbdb[:], in_=wbd32[:]), reads=[wbd32.tok()], writes=[wbdb.tok()])
        nba = S.sbuf("nba", [64, 4], F32)
        S.op("dve", lambda e: e.tensor_scalar(out=nba[:], in0=ba[:], scalar1=-1.0, scalar2=None, op0=ALU.mult),
             reads=[ba.tok()], writes=[nba.tok()])
        epsc = S.sbuf("epsc", [128, 1], F32)
        S.op("dve", lambda e: e.memset(epsc[:], EPS), writes=[epsc.tok()])
        onec = S.sbuf("onec", [128, 1], F32)
        S.op("dve", lambda e: e.memset(onec[:], 1.0), writes=[onec.tok()])
        gs = S.sbuf("gs", [128, 2, 8], F32)
        make_gs(S, modv, gs, 2)
        sh = S.sbuf("sh", [128, 2, 8], F32)
        for i in range(2):
            S.op("dve", lambda e, i=i: e.tensor_copy(out=sh[:, i, :], in_=modv[:, 2 + 2 * i, :]), reads=[modv.tok()], writes=[sh.tok()])
        clam = S.sbuf("clam", [128, 4], F32)
        ctmp = S.sbuf("ctmp", [128, 4], F32)
        S.op("act", lambda e: e.activation(out=ctmp[:], in_=lruc[:, :, 8], func=AF.Exp, scale=-1.0), reads=[lruc.tok()], writes=[ctmp.tok()])
        S.op("act", lambda e: e.activation(out=ctmp[:], in_=ctmp[:], func=AF.Ln, bias=onec[:, 0:1], scale=1.0),
             reads=[ctmp.tok(), onec.tok()], writes=[ctmp.tok()])
        S.op("dve", lambda e: e.tensor_scalar(out=clam[:], in0=ctmp[:], scalar1=-8.0, scalar2=None, op0=ALU.mult),
             reads=[ctmp.tok()], writes=[clam.tok()])
        wb = S.sbuf("wb", [128, 8, P1_WCOLS], BF16)
        X = [S.sbuf(f"X{i}", [128, 8, BLK], F32) for i in range(2)]
        load_cast_weight(S, w1, P1_WCOLS, wb, X, "X")
        NW = norm_work(S, pool, ones32, epsc)
        XN = [S.sbuf(f"XN{i}", [128, 8, BLK], BF16) for i in range(2)]
        q32 = S.sbuf("q32", [64, 4, BLK], F32); k32 = S.sbuf("k32", [64, 4, BLK], F32)
        lr32 = S.sbuf("lr32", [16, BLK], F32)
        e1 = S.sbuf("e1", [64, 4, BLK], F32); sp = e1; csp = S.sbuf("csp", [64, 4, BLK], F32)
        eb = S.sbuf("eb", [64, 4, BLK], F32); enb = S.sbuf("enb", [64, 4, BLK], F32)
        dec = [S.sbuf(f"dec{i}", [64, 4, 4], F32) for i in range(2)]
        qt = [S.sbuf(f"qt{i}", [64, 4, BLK], BF16) for i in range(2)]
        kt = [S.sbuf(f"kt{i}", [64, 4, BLK], BF16) for i in range(2)]
        kh = [S.sbuf(f"kh{i}", [64, 4, BLK], BF16) for i in range(2)]
        vtok = [S.sbuf(f"vtok{i}", [64, 4, 512], BF16) for i in range(2)]
        khtok = [S.sbuf(f"khtok{i}", [64, 4, 256], BF16) for i in range(2)]
        xr = S.sbuf("xr", [128, 4, BLK], F32); xb = S.sbuf("xb", [128, 4, BLK], F32); xbb = S.sbuf("xbb", [128, 4, BLK], BF16)
        rr = S.sbuf("rr", [128, 4, BLK], F32); ii = S.sbuf("ii", [128, 4, BLK], F32)
        aa = S.sbuf("aa", [128, 4, BLK], F32); a2 = rr; uu = ii
        hl = [S.sbuf(f"hl{i}", [128, 4, BLK], F32) for i in range(2)]
        ost = [S.sbuf(f"ost{i}", [64, 4, 512], F32) for i in range(2)]
        ATs = S.sbuf("ATs", [64, 256], BF16)
        S32 = S.sbuf("S32", [64, 512], F32); Sb = S.sbuf("Sb", [64, 512], BF16)
        S.op("dve", lambda e: e.memset(S32[:], 0.0), writes=[S32.tok()])
        S.op("pool", lambda e: e.memset(Sb[:], 0.0), writes=[Sb.tok()])
        xTv = xT.rearrange("(c p) n -> p c n", p=128)
        hlv = hlT.rearrange("(c p) n -> p c n", p=128)
        outs = []
        prev_hl = None
        def do_block(bi, t0, n, mi, prev_hl):
            b2 = bi % 2
            nch = n // 64
            seg = 256 if mi == 1 else 64
            Xb, XNb = X[b2], XN[b2]
            S.dma("sp", lambda e, Xb=Xb, t0=t0, n=n: e.dma_start(out=Xb[:, :, :n], in_=xTv[:, :, t0:t0 + n]), f"X{b2}", writes=[Xb.tok()])
            norm_fm(S, Xb, n, gs, sh, mi, XNb, NW)
            for h in range(4):
                p = pool.next()
                for c in range(8):
                    S.op("pe", lambda e, p=p, c=c, h=h: e.matmul(p[0:64, :n], lhsT=wb[:, c, h * 64:(h + 1) * 64], rhs=XNb[:, c, :n], start=(c == 0), stop=(c == 7)),
                         reads=[wb.tok(), XNb.tok()], writes=[p.tok()])
                S.op("dve", lambda e, p=p, h=h: e.tensor_scalar(out=q32[:, h, :n], in0=p[0:64, :n], scalar1=0.125, scalar2=None, op0=ALU.mult),
                     reads=[p.tok()], writes=[q32.tok()])
            for h in range(4):
                p = pool.next()
                for c in range(8):
                    S.op("pe", lambda e, p=p, c=c, h=h: e.matmul(p[0:64, :n], lhsT=wb[:, c, 256 + h * 64:256 + (h + 1) * 64], rhs=XNb[:, c, :n], start=(c == 0), stop=(c == 7)),
                         reads=[wb.tok(), XNb.tok()], writes=[p.tok()])
                S.op("act", lambda e, p=p, h=h: e.activation(out=k32[:, h, :n], in_=p[0:64, :n], func=AF.Copy),
                     reads=[p.tok()], writes=[k32.tok()])
            p = pool.next()
            for c in range(8):
                S.op("pe", lambda e, p=p, c=c: e.matmul(p[0:16, :n], lhsT=wb[:, c, 1024:1040], rhs=XNb[:, c, :n], start=(c == 0), stop=(c == 7)),
                     reads=[wb.tok(), XNb.tok()], writes=[p.tok()])
            S.op("dve", lambda e, p=p: e.tensor_copy(out=lr32[:, :n], in_=p[0:16, :n]), reads=[p.tok()], writes=[lr32.tok()])
            for h in range(4):
                p = pool.next()
                S.op("pe", lambda e, p=p, h=h: e.matmul(p[0:64, :n], lhsT=wa2[:, h * 64:(h + 1) * 64], rhs=lr32[:, :n], start=True, stop=True),
                     reads=[wa2.tok(), lr32.tok()], writes=[p.tok()])
                S.op("act", lambda e, p=p, h=h: e.activation(out=e1[:, h, :n], in_=p[0:64, :n], func=AF.Exp, bias=nba[:, h:h + 1], scale=-1.0),
                     reads=[p.tok(), nba.tok()], writes=[e1.tok()])
            S.op("act", lambda e: e.activation(out=sp[:, :, :n], in_=e1[:, :, :n], func=AF.Ln, bias=onec[0:64, 0:1], scale=1.0),
                 reads=[e1.tok(), onec.tok()], writes=[sp.tok()])
            for h in range(4):
                S.op("dve", lambda e, h=h: e.tensor_tensor_scan(out=csp[:, h, :n], data0=rmask[0:64, :n], data1=sp[:, h, :n], initial=0.0, op0=ALU.mult, op1=ALU.add),
                     reads=[rmask.tok(), sp.tok()], writes=[csp.tok()])
            S.op("act", lambda e: e.activation(out=eb[:, :, :n], in_=csp[:, :, :n], func=AF.Exp, scale=-1.0 / 16), reads=[csp.tok()], writes=[eb.tok()])
            S.op("act", lambda e: e.activation(out=enb[:, :, :n], in_=csp[:, :, :n], func=AF.Exp, scale=1.0 / 16), reads=[csp.tok()], writes=[enb.tok()])
            decb = dec[b2]
            for h in range(4):
                S.op("dve", lambda e, h=h: e.tensor_copy(out=decb[:, h, :nch], in_=eb[:, h, :n].rearrange("p (c t) -> p c t", t=64)[:, :, 63]),
                     reads=[eb.tok()], writes=[decb.tok()])
            qtb, ktb, khb = qt[b2], kt[b2], kh[b2]
            S.op("dve", lambda e: e.tensor_tensor(out=qtb[:, :, :n], in0=q32[:, :, :n], in1=eb[:, :, :n], op=ALU.mult), reads=[q32.tok(), eb.tok()], writes=[qtb.tok()])
            S.op("pool", lambda e: e.tensor_tensor(out=ktb[:, :, :n], in0=k32[:, :, :n], in1=enb[:, :, :n], op=ALU.mult), reads=[k32.tok(), enb.tok()], writes=[ktb.tok()])
            for h in range(4):
                S.op("dve", lambda e, h=h: e.tensor_tensor(out=khb[:, h, :n].rearrange("p (c t) -> p c t", t=64),
                                                            in0=ktb[:, h, :n].rearrange("p (c t) -> p c t", t=64),
                                                            in1=decb[:, h, :nch].unsqueeze(2).to_broadcast([64, nch, 64]), op=ALU.mult),
                     reads=[ktb.tok(), decb.tok()], writes=[khb.tok()])
            vb = vtok[b2]
            for c in range(nch):
                p = pool.next()
                for kc in range(8):
                    S.op("pe", lambda e, p=p, kc=kc, c=c: e.matmul(p[0:64, :], lhsT=XNb[:, kc, c * 64:(c + 1) * 64], rhs=wb[:, kc, 1040:1552], start=(kc == 0), stop=(kc == 7)),
                         reads=[wb.tok(), XNb.tok()], writes=[p.tok()])
                S.op("act", lambda e, p=p, c=c: e.activation(out=vb[:, c, :], in_=p[0:64, :], func=AF.Copy), reads=[p.tok()], writes=[vb.tok(c)])
            khtb = khtok[b2]
            for c in range(nch):
                r = c % 4
                for h in range(4):
                    S.op("pe", lambda e, r=r, h=h, c=c: e.transpose(out=pTR[:, r, h * 64:(h + 1) * 64], in_=khb[:, h, c * 64:(c + 1) * 64], identity=identb[:]),
                         reads=[khb.tok(), identb.tok()], writes=[pTR.tok()])
                S.op("dve", lambda e, r=r, c=c: e.tensor_copy(out=khtb[:, c, :], in_=pTR[:, r, :]), reads=[pTR.tok()], writes=[khtb.tok(c)])
            for c4 in range(4):
                p = pool.next()
                for kc in range(8):
                    S.op("pe", lambda e, p=p, kc=kc, c4=c4: e.matmul(p[:, :n], lhsT=wb[:, kc, 512 + c4 * 128:512 + (c4 + 1) * 128], rhs=XNb[:, kc, :n], start=(kc == 0), stop=(kc == 7)),
                         reads=[wb.tok(), XNb.tok()], writes=[p.tok()])
                S.op("act", lambda e, p=p, c4=c4: e.activation(out=xr[:, c4, :n], in_=p[:, :n], func=AF.Copy), reads=[p.tok()], writes=[xr.tok()])
            for c4 in range(4):
                S.op("pool", lambda e, c4=c4: e.tensor_scalar(out=xb[:, c4, :n], in0=xr[:, c4, :n], scalar1=lruc[:, c4, 2:3], scalar2=lruc[:, c4, 5:6], op0=ALU.mult, op1=ALU.add),
                     reads=[xr.tok(), lruc.tok()], writes=[xb.tok()])
                for off in (-2, -1, 1, 2):
                    j = off + 2
                    xrv = xr[:, c4, :n].rearrange("p (s t) -> p s t", t=seg)
                    xbv = xb[:, c4, :n].rearrange("p (s t) -> p s t", t=seg)
                    if off < 0:
                        o_sl, i_sl = xbv[:, :, -off:seg], xrv[:, :, 0:seg + off]
                    else:
                        o_sl, i_sl = xbv[:, :, 0:seg - off], xrv[:, :, off:seg]
                    S.op("dve", lambda e, o_sl=o_sl, i_sl=i_sl, c4=c4, j=j: e.scalar_tensor_tensor(out=o_sl, in0=i_sl, scalar=lruc[:, c4, j:j + 1], in1=o_sl, op0=ALU.mult, op1=ALU.add),
                         reads=[xr.tok(), lruc.tok(), xb.tok()], writes=[xb.tok()])
            S.op("pool", lambda e: e.tensor_copy(out=xbb[:, :, :n], in_=xb[:, :, :n]), reads=[xb.tok()], writes=[xbb.tok()])
            for c4 in range(4):
                for (j, dst, bcol) in ((0, rr, 6), (1, ii, 7)):
                    p = pool.next()
                    S.op("pe", lambda e, p=p, j=j, c4=c4: e.matmul(p[:, :n], lhsT=wbdb[:, j, c4, :], rhs=xbb[:, c4, :n], start=True, stop=True),
                         reads=[wbdb.tok(), xbb.tok()], writes=[p.tok()])
                    S.op("act", lambda e, p=p, dst=dst, c4=c4, bcol=bcol: e.activation(out=dst[:, c4, :n], in_=p[:, :n], func=AF.Sigmoid, bias=lruc[:, c4, bcol:bcol + 1], scale=1.0),
                         reads=[p.tok(), lruc.tok()], writes=[dst.tok()])
            for c4 in range(4):
                S.op("act", lambda e, c4=c4: e.activation(out=aa[:, c4, :n], in_=rr[:, c4, :n], func=AF.Exp, scale=clam[:, c4:c4 + 1]),
                     reads=[rr.tok(), clam.tok()], writes=[aa.tok()])
            S.op("pool", lambda e: e.tensor_tensor(out=a2[:, :, :n], in0=aa[:, :, :n], in1=aa[:, :, :n], op=ALU.mult), reads=[aa.tok()], writes=[a2.tok()])
            S.op("act", lambda e: e.activation(out=a2[:, :, :n], in_=a2[:, :, :n], func=AF.Sqrt, bias=onec[:, 0:1], scale=-1.0),
                 reads=[a2.tok(), onec.tok()], writes=[a2.tok()])
            S.op("pool", lambda e: e.tensor_tensor(out=uu[:, :, :n], in0=a2[:, :, :n], in1=ii[:, :, :n], op=ALU.mult), reads=[a2.tok(), ii.tok()], writes=[uu.tok()])
            S.op("pool", lambda e: e.tensor_tensor(out=uu[:, :, :n], in0=uu[:, :, :n], in1=xb[:, :, :n], op=ALU.mult), reads=[uu.tok(), xb.tok()], writes=[uu.tok()])
            hlb = hl[b2]
            for c4 in range(4):
                if prev_hl is None:
                    init, rd = 0.0, []
                else:
                    ph, pn = prev_hl
                    init, rd = ph[:, c4, pn - 1:pn], [ph.tok()]
                S.op("dve", lambda e, c4=c4, init=init: e.tensor_tensor_scan(out=hlb[:, c4, :n], data0=aa[:, c4, :n], data1=uu[:, c4, :n], initial=init, op0=ALU.mult, op1=ALU.add),
                     reads=[aa.tok(), uu.tok()] + rd, writes=[hlb.tok()])
            prev_hl = (hlb, n)
            outs.append(S.dma("sp", lambda e, hlb=hlb, t0=t0, n=n: e.dma_start(out=hlv[:, :, t0:t0 + n], in_=hlb[:, :, :n]), f"hl{b2}", reads=[hlb.tok()]))
            return prev_hl

        def do_chunks(bi, t0, n, mi):
            b2 = bi % 2
            nch = n // 64
            qtb, ktb, khb, vb, khtb, decb = qt[b2], kt[b2], kh[b2], vtok[b2], khtok[b2], dec[b2]
            ob = ost[b2]
            for c in range(nch):
                cs = slice(c * 64, (c + 1) * 64)
                for h in range(4):
                    S.op("pe", lambda e, h=h, cs=cs: e.matmul(pAT[0:64, h * 64:(h + 1) * 64], lhsT=ktb[:, h, cs], rhs=qtb[:, h, cs], start=True, stop=True),
                         reads=[ktb.tok(), qtb.tok()], writes=[pAT.tok()])
                S.op("dve", lambda e: e.tensor_tensor(out=ATs[:], in0=pAT[0:64, 0:256], in1=mask4[:], op=ALU.mult), reads=[pAT.tok(), mask4.tok()], writes=[ATs.tok()])
                for h in range(4):
                    hv = slice(h * 128, (h + 1) * 128)
                    S.op("pe", lambda e, h=h, hv=hv, c=c: e.matmul(pO[0:64, hv], lhsT=ATs[:, h * 64:(h + 1) * 64], rhs=vb[:, c, hv], start=True, stop=False),
                         reads=[ATs.tok(), vb.tok(c)], writes=[pO.tok()])
                    S.op("pe", lambda e, h=h, hv=hv, cs=cs: e.matmul(pO[0:64, hv], lhsT=qtb[:, h, cs], rhs=Sb[:, hv], start=False, stop=True),
                         reads=[qtb.tok(), Sb.tok()], writes=[pO.tok()])
                S.op("act", lambda e, c=c: e.activation(out=ob[:, c, :], in_=pO[0:64, :], func=AF.Copy), reads=[pO.tok()], writes=[ob.tok()])
                for h in range(4):
                    hv = slice(h * 128, (h + 1) * 128)
                    S.op("pe", lambda e, h=h, hv=hv, c=c: e.matmul(pU[0:64, hv], lhsT=khtb[:, c, h * 64:(h + 1) * 64], rhs=vb[:, c, hv], start=True, stop=True),
                         reads=[khtb.tok(c), vb.tok(c)], writes=[pU.tok()])
                for h in range(4):
                    hv = slice(h * 128, (h + 1) * 128)
                    S.op("dve", lambda e, h=h, hv=hv, c=c: e.scalar_tensor_tensor(out=S32[:, hv], in0=S32[:, hv], scalar=decb[:, h, c:c + 1], in1=pU[0:64, hv], op0=ALU.mult, op1=ALU.add),
                         reads=[S32.tok(), decb.tok(), pU.tok()], writes=[S32.tok()])
                S.op("pool", lambda e: e.tensor_copy(out=Sb[:], in_=S32[:]), reads=[S32.tok()], writes=[Sb.tok()])
            outs.append(S.dma("sp", lambda e, ob=ob, t0=t0, n=n, nch=nch: e.dma_start(out=og[t0:t0 + n, :].rearrange("(c s) v -> s c v", s=64), in_=ob[:, :nch, :]),
                              f"ost{b2}", reads=[ob.tok()]))

        blks = seq_blocks()
        prev_hl = do_block(0, *blks[0], prev_hl)
        for bi in range(len(blks)):
            if bi + 1 < len(blks):
                prev_hl = do_block(bi + 1, *blks[bi + 1], prev_hl)
            do_chunks(bi, *blks[bi])
        S.wait_all_final("sp", outs)
        S.emit()
    return nc


def mods_split(m):
    names = ["sh1", "sc1", "g1", "sh2", "sc2", "g2"]
    return {nm: m[..., i * D:(i + 1) * D] for i, nm in enumerate(names)}


def consts_p1():
    s = np.arange(64)[:, None]
    t = np.arange(64)[None, :]
    m = (s <= t).astype(np.float32)
    rm = np.ones((128, BLK), np.float32)
    rm[:, ::64] = 0.0
    return dict(ones=np.ones((128, 128), np.float32), mask4=np.ascontiguousarray(np.tile(m, (1, 4))),
                ident=np.eye(64, dtype=np.float32), rmask=rm)


def seq_T(ctx_b, x_b, d):
    if d == 1:
        ctx_b, x_b = ctx_b[::-1], x_b[::-1]
    return np.ascontiguousarray(np.concatenate([ctx_b, x_b], 0).T)


def unseq(a, d):
    c, l = a[:NCTX], a[NCTX:]
    if d == 1:
        c, l = c[::-1], l[::-1]
    return c, l


def prep_p1(inp, mods0, cmods0, b, d):
    w_in = inp["ab_w_in"][0]
    lr = w_in[:, 1536:1552] if d == 0 else w_in[:, 1552:1568]
    w1 = np.ascontiguousarray(np.concatenate([w_in[:, 0:512], w_in[:, 2080:2592], lr, w_in[:, 512:1024]], 1))
    ml, mc = mods_split(mods0[b]), mods_split(cmods0)
    modv = np.stack([col_layout(inp["norm1_g"][0]), col_layout(ml["sc1"]), col_layout(ml["sh1"]),
                     col_layout(mc["sc1"]), col_layout(mc["sh1"])], 1)
    cw = inp["rg_conv_w"][0]
    z = np.zeros_like(cw[0])
    taps = [cw[0], cw[1], cw[2], cw[3], z] if d == 0 else [z, cw[3], cw[2], cw[1], cw[0]]
    vecs = taps + [inp["rg_conv_b"][0], inp["rg_br"][0, d], inp["rg_bi"][0, d], inp["rg_lambda"][0, d]]
    lruc = np.stack([col_layout(v) for v in vecs], 2)
    wbd = np.zeros((128, 2, 4, 128), np.float32)
    for j, W in enumerate([inp["rg_wr"][0, d], inp["rg_wi"][0, d]]):
        for c4 in range(4):
            wbd[0:64, j, c4, 0:64] = W[2 * c4]
            wbd[64:128, j, c4, 64:128] = W[2 * c4 + 1]
    m = dict(xT=seq_T(inp["ctx"][b], inp["x"][b], d), w1=w1, modv=np.ascontiguousarray(modv),
             wa2=np.ascontiguousarray(inp["gla_wa2"][0, d]), ba=np.ascontiguousarray(inp["gla_ba"][0, d].reshape(4, 64).T),
             lruc=np.ascontiguousarray(lruc), wbd=wbd)
    m.update(consts_p1())
    return m


NTOK = 4096 + 128


def tok_blocks():
    blocks = [(i * BLK, BLK, 0) for i in range(4096 // BLK)]
    blocks.append((4096, 128, 1))
    if DEBUG_NBLK:
        blocks = blocks[:DEBUG_NBLK - 1] + blocks[-1:]
    return blocks


def router_softmax(S, T2, n, t0, rw, probs_d, pool, W, outs, tag):
    for tt in range(n // 128):
        p = pool.next()
        for kc in range(8):
            S.op("pe", lambda e, p=p, kc=kc, tt=tt: e.matmul(p[:, 0:16], lhsT=T2[:, kc, tt * 128:(tt + 1) * 128], rhs=rw[:, kc, :], start=(kc == 0), stop=(kc == 7)),
                 reads=[T2.tok(), rw.tok()], writes=[p.tok()])
        mx, ex, ssum, pr = W["mx"], W["ex"], W["ssum"], W["pr"][tt % 2]
        S.op("dve", lambda e, p=p: e.tensor_reduce(out=mx[:], in_=p[:, 0:16], axis=AX.X, op=ALU.max, negate=True), reads=[p.tok()], writes=[mx.tok()])
        S.op("act", lambda e, p=p: e.activation(out=ex[:], in_=p[:, 0:16], func=AF.Exp, bias=mx[:, 0:1], scale=1.0, accum_out=ssum[:, 0:1]),
             reads=[p.tok(), mx.tok()], writes=[ex.tok(), ssum.tok()])
        S.op("dve", lambda e: e.reciprocal(out=ssum[:], in_=ssum[:]), reads=[ssum.tok()], writes=[ssum.tok()])
        S.op("dve", lambda e, pr=pr: e.tensor_scalar(out=pr[:], in0=ex[:], scalar1=ssum[:, 0:1], scalar2=None, op0=ALU.mult),
             reads=[ex.tok(), ssum.tok()], writes=[pr.tok()])
        r0 = t0 + tt * 128
        outs.append(S.dma("sp", lambda e, pr=pr, r0=r0: e.dma_start(out=probs_d[r0:r0 + 128, :], in_=pr[:]), f"{tag}pr{tt % 2}", reads=[pr.tok()]))


def router_work(S):
    return dict(mx=S.sbuf("r_mx", [128, 1], F32), ex=S.sbuf("r_ex", [128, 16], F32), ssum=S.sbuf("r_ss", [128, 1], F32),
                pr=[S.sbuf(f"r_pr{i}", [128, 16], F32) for i in range(2)])


def build_p2():
    nc = bass.Bass("TRN2", target_bir_lowering=False)
    dt = lambda name, shape, kind="ExternalInput", d=F32: nc.dram_tensor(name, list(shape), d, kind=kind).ap()
    xT = dt("xT", [D, NTOK])
    ogf_d, ogb_d = dt("ogf", [512, NTOK]), dt("ogb", [512, NTOK])
    hlf_d, hlb_d = dt("hlf", [512, NTOK]), dt("hlb", [512, NTOK])
    w2 = dt("w2", [D, 1024]); wout = dt("wout", [1024, D]); rw_d = dt("rw", [128, 8, 16])
    modv_d = dt("modv", [128, 5, 8]); modv2_d = dt("modv2", [128, 5, 8]); g1_d = dt("g1v", [128, 2, 8]); gng_d = dt("gng", [128, 4])
    ones_d = dt("ones", [128, 128])
    xmT = dt("xmT", [D, NTOK], kind="ExternalOutput")
    t2T = dt("t2T", [D, NTOK], kind="ExternalOutput")
    probs_d = dt("probs", [NTOK, 16], kind="ExternalOutput")
    with ExitStack() as st:
        S = Sched(nc, st)
        pool = PsumPool(S, ["pp0", "pp1", "pp2", "pp3", "pp4", "pp5"])
        ones32 = S.sbuf("ones32", [128, 128], F32); load_const(S, ones_d, ones32, "c_ones")
        modv = S.sbuf("modv", [128, 5, 8], F32); load_const(S, modv_d, modv, "c_modv")
        modv2 = S.sbuf("modv2", [128, 5, 8], F32); load_const(S, modv2_d, modv2, "c_modv2")
        g1v = S.sbuf("g1v", [128, 2, 8], F32); load_const(S, g1_d, g1v, "c_g1")
        gng = S.sbuf("gng", [128, 4], F32); load_const(S, gng_d, gng, "c_gng")
        rw = S.sbuf("rw", [128, 8, 16], F32); load_const(S, rw_d, rw, "c_rw")
        epsc = S.sbuf("epsc", [128, 1], F32)
        S.op("dve", lambda e: e.memset(epsc[:], EPS), writes=[epsc.tok()])
        gs1 = S.sbuf("gs1", [128, 2, 8], F32); make_gs(S, modv, gs1, 2)
        gs2 = S.sbuf("gs2", [128, 2, 8], F32); make_gs(S, modv2, gs2, 2)
        sh1 = S.sbuf("sh1", [128, 2, 8], F32); sh2 = S.sbuf("sh2", [128, 2, 8], F32)
        for i in range(2):
            S.op("dve", lambda e, i=i: e.tensor_copy(out=sh1[:, i, :], in_=modv[:, 2 + 2 * i, :]), reads=[modv.tok()], writes=[sh1.tok()])
            S.op("dve", lambda e, i=i: e.tensor_copy(out=sh2[:, i, :], in_=modv2[:, 2 + 2 * i, :]), reads=[modv2.tok()], writes=[sh2.tok()])
        X = [S.sbuf(f"X{i}", [128, 8, BLK], F32) for i in range(2)]
        w2b = S.sbuf("w2b", [128, 8, 1024], BF16); load_cast_weight(S, w2, 1024, w2b, X, "X")
        woutb = S.sbuf("woutb", [128, 8, 1024], BF16); load_cast_weight(S, wout, 1024, woutb, X, "X")
        NW = norm_work(S, pool, ones32, epsc)
        RW = router_work(S)
        XN = [S.sbuf(f"XN{i}", [128, 8, BLK], BF16) for i in range(2)]
        sg = S.sbuf("sg", [128, 4, BLK], F32); rgx = S.sbuf("rgx", [128, 4, BLK], F32)
        gt1 = S.sbuf("gt1", [128, 4, BLK], F32); gt2 = S.sbuf("gt2", [128, 4, BLK], F32)
        OG = [[S.sbuf(f"OG{j}{i}", [128, 4, BLK], F32) for i in range(2)] for j in range(2)]
        HL = [[S.sbuf(f"HL{j}{i}", [128, 4, BLK], F32) for i in range(2)] for j in range(2)]
        osum = S.sbuf("osum", [128, 4, BLK], F32); sqo = S.sbuf("sqo", [128, 4, BLK], F32)
        hrs = S.sbuf("hrs", [128, BLK], F32); hsd = S.sbuf("hsd", [128, BLK], F32); ht = S.sbuf("ht", [128, BLK], F32)
        mg = S.sbuf("mg", [128, 4, BLK], BF16); ml = S.sbuf("ml", [128, 4, BLK], BF16); hsum = S.sbuf("hsum", [128, 4, BLK], F32)
        XM = [S.sbuf(f"XM{i}", [128, 8, BLK], F32) for i in range(2)]
        T2 = [S.sbuf(f"T2{i}", [128, 8, BLK], F32) for i in range(2)]
        xTv = xT.rearrange("(c p) n -> p c n", p=128)
        v4 = lambda a: a.rearrange("(c p) n -> p c n", p=128)
        outs = []

        def do_block(bi, t0, n, mi):
            b2 = bi % 2
            Xb, XNb, XMb, T2b = X[b2], XN[b2], XM[b2], T2[b2]
            S.dma("sp", lambda e: e.dma_start(out=Xb[:, :, :n], in_=xTv[:, :, t0:t0 + n]), f"X{b2}", writes=[Xb.tok()])
            ogs, hls = [OG[0][b2], OG[1][b2]], [HL[0][b2], HL[1][b2]]
            for j, (src, dst) in enumerate([(ogf_d, ogs[0]), (ogb_d, ogs[1]), (hlf_d, hls[0]), (hlb_d, hls[1])]):
                S.dma("sp", lambda e, src=src, dst=dst: e.dma_start(out=dst[:, :, :n], in_=v4(src)[:, :, t0:t0 + n]), f"in{j}{b2}", writes=[dst.tok()])
            norm_fm(S, Xb, n, gs1, sh1, mi, XNb, NW)
            for c4 in range(4):
                p = pool.next()
                for kc in range(8):
                    S.op("pe", lambda e, p=p, kc=kc, c4=c4: e.matmul(p[:, :n], lhsT=w2b[:, kc, c4 * 128:(c4 + 1) * 128], rhs=XNb[:, kc, :n], start=(kc == 0), stop=(kc == 7)),
                         reads=[w2b.tok(), XNb.tok()], writes=[p.tok()])
                S.op("act", lambda e, p=p, c4=c4: e.activation(out=sg[:, c4, :n], in_=p[:, :n], func=AF.Silu), reads=[p.tok()], writes=[sg.tok()])
            for c4 in range(4):
                p = pool.next()
                for kc in range(8):
                    S.op("pe", lambda e, p=p, kc=kc, c4=c4: e.matmul(p[:, :n], lhsT=w2b[:, kc, 512 + c4 * 128:512 + (c4 + 1) * 128], rhs=XNb[:, kc, :n], start=(kc == 0), stop=(kc == 7)),
                         reads=[w2b.tok(), XNb.tok()], writes=[p.tok()])
                S.op("act", lambda e, p=p, c4=c4: e.activation(out=rgx[:, c4, :n], in_=p[:, :n], func=AF.Copy), reads=[p.tok()], writes=[rgx.tok()])
            S.op("pool", lambda e: e.tensor_tensor(out=gt1[:, :, :n], in0=rgx[:, :, :n], in1=rgx[:, :, :n], op=ALU.mult), reads=[rgx.tok()], writes=[gt1.tok()])
            S.op("pool", lambda e: e.tensor_scalar(out=gt1[:, :, :n], in0=gt1[:, :, :n], scalar1=0.044715, scalar2=1.0, op0=ALU.mult, op1=ALU.add), reads=[gt1.tok()], writes=[gt1.tok()])
            S.op("pool", lambda e: e.tensor_tensor(out=gt1[:, :, :n], in0=gt1[:, :, :n], in1=rgx[:, :, :n], op=ALU.mult), reads=[gt1.tok(), rgx.tok()], writes=[gt1.tok()])
            S.op("act", lambda e: e.activation(out=gt2[:, :, :n], in_=gt1[:, :, :n], func=AF.Sigmoid, scale=1.5957691216), reads=[gt1.tok()], writes=[gt2.tok()])
            S.op("pool", lambda e: e.tensor_tensor(out=gt2[:, :, :n], in0=gt2[:, :, :n], in1=rgx[:, :, :n], op=ALU.mult), reads=[gt2.tok(), rgx.tok()], writes=[gt2.tok()])
            S.op("pool", lambda e: e.tensor_tensor(out=osum[:, :, :n], in0=ogs[0][:, :, :n], in1=ogs[1][:, :, :n], op=ALU.add), reads=[ogs[0].tok(), ogs[1].tok()], writes=[osum.tok()])
            S.op("act", lambda e: e.activation(out=sqo[:, :, :n], in_=osum[:, :, :n], func=AF.Square), reads=[osum.tok()], writes=[sqo.tok()])
            for c4 in range(4):
                p = pool.next()
                S.op("pe", lambda e, p=p, c4=c4: e.matmul(p[:, :n], lhsT=ones32[:, :], rhs=sqo[:, c4, :n], start=True, stop=True), reads=[ones32.tok(), sqo.tok()], writes=[p.tok()])
                S.op("act", lambda e, p=p: e.activation(out=hsd[:, :n], in_=p[:, :n], func=AF.Sqrt, bias=epsc[:, 0:1], scale=1.0 / 128), reads=[p.tok(), epsc.tok()], writes=[hsd.tok()])
                S.op("dve", lambda e: e.reciprocal(out=hrs[:, :n], in_=hsd[:, :n]), reads=[hsd.tok()], writes=[hrs.tok()])
                S.op("dve", lambda e, c4=c4: e.tensor_tensor(out=ht[:, :n], in0=osum[:, c4, :n], in1=hrs[:, :n], op=ALU.mult), reads=[osum.tok(), hrs.tok()], writes=[ht.tok()])
                S.op("dve", lambda e, c4=c4: e.scalar_tensor_tensor(out=mg[:, c4, :n], in0=ht[:, :n], scalar=gng[:, c4:c4 + 1], in1=sg[:, c4, :n], op0=ALU.mult, op1=ALU.mult),
                     reads=[ht.tok(), gng.tok(), sg.tok()], writes=[mg.tok()])
            S.op("pool", lambda e: e.tensor_tensor(out=hsum[:, :, :n], in0=hls[0][:, :, :n], in1=hls[1][:, :, :n], op=ALU.add), reads=[hls[0].tok(), hls[1].tok()], writes=[hsum.tok()])
            S.op("pool", lambda e: e.tensor_tensor(out=ml[:, :, :n], in0=hsum[:, :, :n], in1=gt2[:, :, :n], op=ALU.mult), reads=[hsum.tok(), gt2.tok()], writes=[ml.tok()])
            for oc in range(8):
                p = pool.next()
                for kc in range(8):
                    src = mg if kc < 4 else ml
                    S.op("pe", lambda e, p=p, kc=kc, oc=oc, src=src: e.matmul(p[:, :n], lhsT=woutb[:, kc, oc * 128:(oc + 1) * 128], rhs=src[:, kc % 4, :n], start=(kc == 0), stop=(kc == 7)),
                         reads=[woutb.tok(), src.tok()], writes=[p.tok()])
                S.op("dve", lambda e, p=p, oc=oc: e.scalar_tensor_tensor(out=XMb[:, oc, :n], in0=p[:, :n], scalar=g1v[:, mi, oc:oc + 1], in1=Xb[:, oc, :n], op0=ALU.mult, op1=ALU.add),
                     reads=[p.tok(), g1v.tok(), Xb.tok()], writes=[XMb.tok()])
            outs.append(S.dma("sp", lambda e: e.dma_start(out=v4(xmT)[:, :, t0:t0 + n], in_=XMb[:, :, :n]), f"XM{b2}", reads=[XMb.tok()]))
            norm_fm(S, XMb, n, gs2, sh2, mi, T2b, NW)
            outs.append(S.dma("sp", lambda e: e.dma_start(out=v4(t2T)[:, :, t0:t0 + n], in_=T2b[:, :, :n]), f"T2{b2}", reads=[T2b.tok()]))
            router_softmax(S, T2b, n, t0, rw, probs_d, pool, RW, outs, "p2")

        for bi, (t0, n, mi) in enumerate(tok_blocks()):
            do_block(bi, t0, n, mi)
        S.wait_all_final("sp", outs)
        S.emit()
    return nc


def half_tokens(lat_b, ctx_b, half):
    return np.concatenate([lat_b[half * 4096:(half + 1) * 4096], ctx_b[half * 128:(half + 1) * 128]], 0)


def modv_pack(norm_g, ml, mc, sc, sh):
    return np.ascontiguousarray(np.stack([col_layout(norm_g), col_layout(ml[sc]), col_layout(ml[sh]), col_layout(mc[sc]), col_layout(mc[sh])], 1))


def prep_p2(inp, mods0, cmods0, b, half, og_f, og_b, hl_f, hl_b):
    w_in = inp["ab_w_in"][0]
    ml, mc = mods_split(mods0[b]), mods_split(cmods0)
    T = lambda lat, ctx: np.ascontiguousarray(half_tokens(lat, ctx, half).T)
    return dict(xT=T(inp["x"][b], inp["ctx"][b]),
                ogf=T(og_f["lat"], og_f["ctx"]), ogb=T(og_b["lat"], og_b["ctx"]),
                hlf=T(hl_f["lat"], hl_f["ctx"]), hlb=T(hl_b["lat"], hl_b["ctx"]),
                w2=np.ascontiguousarray(np.concatenate([w_in[:, 1024:1536], w_in[:, 1568:2080]], 1)),
                wout=np.ascontiguousarray(inp["ab_w_out"][0]),
                rw=np.ascontiguousarray(inp["router_w"][0].reshape(8, 128, 16).transpose(1, 0, 2)),
                modv=modv_pack(inp["norm1_g"][0], ml, mc, "sc1", "sh1"), modv2=modv_pack(inp["norm2_g"][0], ml, mc, "sc2", "sh2"),
                g1v=np.ascontiguousarray(np.stack([col_layout(ml["g1"]), col_layout(mc["g1"])], 1)),
                gng=col_layout(inp["gla_norm_g"][0]), ones=np.ones((128, 128), np.float32))


def build_p0():
    nc = bass.Bass("TRN2", target_bir_lowering=False)
    dt = lambda name, shape, kind="ExternalInput", d=F32: nc.dram_tensor(name, list(shape), d, kind=kind).ap()
    cT = dt("cT", [128, 8, 5])
    mw = dt("mw", [2, D, 768])
    mb = dt("mb", [2, 5, 768])
    out = dt("mods", [2, 5, 768], kind="ExternalOutput")
    with ExitStack() as st:
        S = Sched(nc, st)
        pool = PsumPool(S, ["pp0", "pp1"])
        c32 = S.sbuf("c32", [128, 8, 5], F32); load_const(S, cT, c32, "c_c")
        sc = S.sbuf("sc", [128, 8, 5], F32)
        S.op("act", lambda e: e.activation(out=sc[:], in_=c32[:], func=AF.Silu), reads=[c32.tok()], writes=[sc.tok()])
        outs = []
        for l in range(2):
            w = S.sbuf(f"w{l}", [128, 8, 768], F32)
            S.dma("sp", lambda e, w=w, l=l: e.dma_start(out=w[:], in_=mw[l].rearrange("(c p) n -> p c n", p=128)), f"w{l}", writes=[w.tok()])
            b = S.sbuf(f"b{l}", [5, 768], F32)
            S.dma("sp", lambda e, b=b, l=l: e.dma_start(out=b[:], in_=mb[l]), f"b{l}", writes=[b.tok()])
            o = S.sbuf(f"o{l}", [5, 768], F32)
            for j in range(2):
                p = pool.next()
                for kc in range(8):
                    S.op("pe", lambda e, p=p, kc=kc, j=j, w=w: e.matmul(p[0:5, 0:384], lhsT=sc[:, kc, :], rhs=w[:, kc, j * 384:(j + 1) * 384], start=(kc == 0), stop=(kc == 7)),
                         reads=[sc.tok(), w.tok()], writes=[p.tok()])
                S.op("dve", lambda e, p=p, j=j, o=o, b=b: e.tensor_tensor(out=o[:, j * 384:(j + 1) * 384], in0=p[0:5, 0:384], in1=b[:, j * 384:(j + 1) * 384], op=ALU.add),
                     reads=[p.tok(), b.tok()], writes=[o.tok()])
            outs.append(S.dma("sp", lambda e, o=o, l=l: e.dma_start(out=out[l], in_=o[:]), f"o{l}", reads=[o.tok()]))
        S.wait_all_final("sp", outs)
        S.emit()
    return nc


def run_p0(inp):
    cv = np.concatenate([inp["c"], inp["c_ctx"][None]], 0)
    cT = np.ascontiguousarray(cv.T.reshape(8, 128, 5).transpose(1, 0, 2))
    maps = []
    for i in range(8):
        sl = slice(i * 768, (i + 1) * 768)
        maps.append(dict(cT=cT, mw=np.ascontiguousarray(inp["mod_w"][:, :, sl]),
                         mb=np.ascontiguousarray(np.broadcast_to(inp["mod_b"][:, None, sl], (2, 5, 768)))))
    res = run_bass_kernel_spmd(build_p0(), maps, core_ids=list(range(8)))
    mods = np.concatenate([r["mods"] for r in res.results], 2)
    return mods


NITER = 26
PASSES = [[(i * BLK, BLK, 0) for i in range(4 * j, 4 * j + 4)] for j in range(4)]
PASSES[3] = PASSES[3] + [(4096, 128, 1)]
PASS_W = 1152


def build_p4(final):
    nc = bass.Bass("TRN2", target_bir_lowering=False)
    dt = lambda name, shape, kind="ExternalInput", d=F32: nc.dram_tensor(name, list(shape), d, kind=kind).ap()
    t2T = dt("t2T", [D, NTOK]); xmT = dt("xmT", [D, NTOK])
    pl_d = dt("pl", [128, 1024]); pc_d = dt("pc", [128, 32])
    po_d = dt("po", [16, NTOK])
    G_d = dt("G", [128, 128]); sel_d = dt("sel", [128, 16]); selE_d = dt("selE", [16, 16, 128]); kv_d = dt("kv", [128, 2])
    g2_d = dt("g2v", [128, 2, 8]); ones_d = dt("ones", [128, 128]); modf_d = dt("modf", [128, 3, 8])
    wg_d = dt("wg", [16, D, 1024], d=BF16); wu_d = dt("wu", [16, D, 1024], d=BF16); wd_d = dt("wd", [16, 1024, D], d=BF16)
    xoT = dt("xoT", [D, NTOK], kind="ExternalOutput")
    with ExitStack() as st:
        S = Sched(nc, st)
        pool = PsumPool(S, ["pp0", "pp1", "pp2", "pp3", "pp4", "pp5", "pp6"])
        pS = S.psum("pS", [128, 512], F32)
        PL = S.sbuf("PL", [128, 1024], F32); load_const(S, pl_d, PL, "c_pl")
        PC = S.sbuf("PC", [128, 32], F32); load_const(S, pc_d, PC, "c_pc")
        PO = S.sbuf("PO", [16, NTOK], F32); load_const(S, po_d, PO, "c_po")
        G = S.sbuf("G", [128, 128], F32); load_const(S, G_d, G, "c_G")
        sel = S.sbuf("sel", [128, 16], F32); load_const(S, sel_d, sel, "c_sel")
        selE = S.sbuf("selE", [16, 16, 128], F32); load_const(S, selE_d, selE, "c_selE")
        kv = S.sbuf("kv", [128, 2], F32); load_const(S, kv_d, kv, "c_kv")
        g2v = S.sbuf("g2v", [128, 2, 8], F32); load_const(S, g2_d, g2v, "c_g2")
        ones32 = S.sbuf("ones32", [128, 128], F32); load_const(S, ones_d, ones32, "c_ones")
        lo = S.sbuf("lo", [128, 2], F32); mid = S.sbuf("mid", [128, 2], F32); cnt = S.sbuf("cnt", [128, 2], F32)
        selm = S.sbuf("selm", [128, 2], F32)
        yacc = S.sbuf("yacc", [128, 8, PASS_W], F32)
        junk = Tile(yacc[:, 0, 0:1024], "junk")
        S.op("dve", lambda e: e.memset(lo[:], 0.0), writes=[lo.tok()])
        for it in range(NITER):
            hk = 2.0 ** -(it + 1)
            S.op("dve", lambda e, hk=hk: e.tensor_scalar(out=mid[:], in0=lo[:], scalar1=hk, scalar2=None, op0=ALU.add), reads=[lo.tok()], writes=[mid.tok()])
            S.op("dve", lambda e: e.tensor_scalar(out=junk[:, :], in0=PL[:, :], scalar1=mid[:, 0:1], scalar2=None, op0=ALU.is_ge, op1=ALU.add, accum_out=cnt[:, 0:1]),
                 reads=[PL.tok(), mid.tok()], writes=[junk.tok(), cnt.tok()])
            S.op("dve", lambda e: e.tensor_scalar(out=junk[:, 0:32], in0=PC[:, :], scalar1=mid[:, 1:2], scalar2=None, op0=ALU.is_ge, op1=ALU.add, accum_out=cnt[:, 1:2]),
                 reads=[PC.tok(), mid.tok()], writes=[junk.tok(), cnt.tok()])
            S.op("pe", lambda e: e.matmul(pS[:, 0:2], lhsT=G[:, :], rhs=cnt[:, :], start=True, stop=True), reads=[G.tok(), cnt.tok()], writes=[pS.tok()])
            S.op("dve", lambda e: e.tensor_tensor(out=selm[:], in0=pS[:, 0:2], in1=kv[:], op=ALU.is_ge), reads=[pS.tok(), kv.tok()], writes=[selm.tok()])
            S.op("dve", lambda e, hk=hk: e.scalar_tensor_tensor(out=lo[:], in0=selm[:], scalar=hk, in1=lo[:], op0=ALU.mult, op1=ALU.add), reads=[selm.tok(), lo.tok()], writes=[lo.tok()])
        thr = S.sbuf("thr", [16, 2], F32)
        S.op("pe", lambda e: e.matmul(pS[0:16, 0:2], lhsT=sel[:, :], rhs=lo[:, :], start=True, stop=True), reads=[sel.tok(), lo.tok()], writes=[pS.tok()])
        S.op("dve", lambda e: e.tensor_copy(out=thr[:], in_=pS[0:16, 0:2]), reads=[pS.tok()], writes=[thr.tok()])
        gateT = PO
        S.op("dve", lambda e: e.scalar_tensor_tensor(out=gateT[:, 0:4096], in0=PO[:, 0:4096], scalar=thr[:, 0:1], in1=PO[:, 0:4096], op0=ALU.is_ge, op1=ALU.mult),
             reads=[PO.tok(), thr.tok()], writes=[gateT.tok()])
        S.op("dve", lambda e: e.scalar_tensor_tensor(out=gateT[:, 4096:NTOK], in0=PO[:, 4096:NTOK], scalar=thr[:, 1:2], in1=PO[:, 4096:NTOK], op0=ALU.is_ge, op1=ALU.mult),
             reads=[PO.tok(), thr.tok()], writes=[gateT.tok()])
        WSET = [[S.sbuf(f"w{nm}{i}", [128, 8, 1024], BF16) for nm in ("g", "u", "d")] for i in range(2)]
        t2b = S.sbuf("t2b", [128, 8, PASS_W], BF16)
        gbc = S.sbuf("gbc", [128, BLK], F32)
        sgt = [S.sbuf(f"sgt{i}", [128, BLK], F32) for i in range(2)]
        hh = [S.sbuf(f"hh{i}", [128, BLK], F32) for i in range(2)]
        hgb = S.sbuf("hgb", [128, 8, BLK], BF16)
        XO = [S.sbuf("XO0", [128, 8, BLK], F32)] * 2
        stg = XO
        v4 = lambda a: a.rearrange("(c p) n -> p c n", p=128)
        outs = []
        if final:
            modf = S.sbuf("modf", [128, 3, 8], F32); load_const(S, modf_d, modf, "c_modf")
            epsc = S.sbuf("epsc", [128, 1], F32)
            S.op("dve", lambda e: e.memset(epsc[:], EPS), writes=[epsc.tok()])
            gsf = S.sbuf("gsf", [128, 1, 8], F32); make_gs(S, modf, gsf, 1)
            shf = S.sbuf("shf", [128, 1, 8], F32)
            S.op("dve", lambda e: e.tensor_copy(out=shf[:, 0, :], in_=modf[:, 2, :]), reads=[modf.tok()], writes=[shf.tok()])
            NW = norm_work(S, pool, ones32, epsc)
            XF = XO
        stgi = [0]

        def load_w(src, dst, nm):
            wv = src.rearrange("(c p) n -> p c n", p=128)
            for hh_ in range(2):
                S.dma("sp", lambda e, hh_=hh_: e.dma_start(out=dst[:, 4 * hh_:4 * hh_ + 4, :], in_=wv[:, 4 * hh_:4 * hh_ + 4, :]), nm, writes=[dst.tok()])

        def expert_block(e_i, off, t0, n):
            wgb, wub, wdb = WSET[e_i % 2]
            p = pool.next()
            S.op("pe", lambda e: e.matmul(p[:, :n], lhsT=selE[:, e_i, :], rhs=gateT[:, t0:t0 + n], start=True, stop=True), reads=[selE.tok(), gateT.tok()], writes=[p.tok()])
            S.op("act", lambda e: e.activation(out=gbc[:, :n], in_=p[:, :n], func=AF.Copy), reads=[p.tok()], writes=[gbc.tok()])
            for fc in range(8):
                pg, pu = pool.next(), pool.next()
                for kc in range(8):
                    S.op("pe", lambda e, kc=kc, fc=fc, pg=pg: e.matmul(pg[:, :n], lhsT=wgb[:, kc, fc * 128:(fc + 1) * 128], rhs=t2b[:, kc, off:off + n], start=(kc == 0), stop=(kc == 7)),
                         reads=[wgb.tok(), t2b.tok()], writes=[pg.tok()])
                for kc in range(8):
                    S.op("pe", lambda e, kc=kc, fc=fc, pu=pu: e.matmul(pu[:, :n], lhsT=wub[:, kc, fc * 128:(fc + 1) * 128], rhs=t2b[:, kc, off:off + n], start=(kc == 0), stop=(kc == 7)),
                         reads=[wub.tok(), t2b.tok()], writes=[pu.tok()])
                sg_, hh_ = sgt[fc % 2], hh[fc % 2]
                S.op("act", lambda e, sg_=sg_, pg=pg: e.activation(out=sg_[:, :n], in_=pg[:, :n], func=AF.Silu), reads=[pg.tok()], writes=[sg_.tok()])
                S.op("dve", lambda e, hh_=hh_, pu=pu, sg_=sg_: e.tensor_tensor(out=hh_[:, :n], in0=pu[:, :n], in1=sg_[:, :n], op=ALU.mult), reads=[pu.tok(), sg_.tok()], writes=[hh_.tok()])
                S.op("pool", lambda e, fc=fc, hh_=hh_: e.tensor_tensor(out=hgb[:, fc, :n], in0=hh_[:, :n], in1=gbc[:, :n], op=ALU.mult), reads=[hh_.tok(), gbc.tok()], writes=[hgb.tok()])
            for dc in range(8):
                py = pool.next()
                for fc in range(8):
                    S.op("pe", lambda e, fc=fc, dc=dc, py=py: e.matmul(py[:, :n], lhsT=wdb[:, fc, dc * 128:(dc + 1) * 128], rhs=hgb[:, fc, :n], start=(fc == 0), stop=(fc == 7)),
                         reads=[wdb.tok(), hgb.tok()], writes=[py.tok()])
                if e_i == 0:
                    S.op("dve", lambda e, dc=dc, py=py: e.tensor_copy(out=yacc[:, dc, off:off + n], in_=py[:, :n]), reads=[py.tok()], writes=[yacc.tok((off, dc)), junk.tok()])
                else:
                    S.op("dve", lambda e, dc=dc, py=py: e.tensor_tensor(out=yacc[:, dc, off:off + n], in0=py[:, :n], in1=yacc[:, dc, off:off + n], op=ALU.add),
                         reads=[py.tok(), yacc.tok((off, dc))], writes=[yacc.tok((off, dc))])

        def finish_block(bi, off, t0, n, mi):
            XOb = XO[bi % 2]
            S.dma("sp", lambda e: e.dma_start(out=XOb[:, :, :n], in_=v4(xmT)[:, :, t0:t0 + n]), "XO0", writes=[XOb.tok()])
            for dc in range(8):
                S.op("dve", lambda e, dc=dc: e.scalar_tensor_tensor(out=XOb[:, dc, :n], in0=yacc[:, dc, off:off + n], scalar=g2v[:, mi, dc:dc + 1], in1=XOb[:, dc, :n], op0=ALU.mult, op1=ALU.add),
                     reads=[yacc.tok((off, dc)), g2v.tok(), XOb.tok()], writes=[XOb.tok()])
            if final:
                XFb = XF[bi % 2]
                norm_fm(S, XOb, n, gsf, shf, 0, XFb, NW)
                outs.append(S.dma("sp", lambda e: e.dma_start(out=v4(xoT)[:, :, t0:t0 + n], in_=XFb[:, :, :n]), "XFo", reads=[XFb.tok()]))
            else:
                outs.append(S.dma("sp", lambda e: e.dma_start(out=v4(xoT)[:, :, t0:t0 + n], in_=XOb[:, :, :n]), "XOo", reads=[XOb.tok()]))

        npass = 1 if DEBUG_NBLK else 4
        nexp = DEBUG_NBLK if DEBUG_NBLK else 16
        for ps_i in range(npass):
            blocks = PASSES[ps_i]
            if DEBUG_NBLK:
                blocks = blocks[:1]
            offs = []
            off = 0
            for (t0, n, mi) in blocks:
                offs.append(off)
                s_ = stg[0]
                def ld(s_=s_, t0=t0, n=n, off=off):
                    S.dma("sp", lambda e: e.dma_start(out=s_[:, :, :n], in_=v4(t2T)[:, :, t0:t0 + n]), "XO0", writes=[s_.tok()])
                    S.op("pool", lambda e: e.tensor_copy(out=t2b[:, :, off:off + n], in_=s_[:, :, :n]), reads=[s_.tok()], writes=[t2b.tok()])
                ld()
                stgi[0] += 1
                off += n
            for e_i in range(nexp):
                ws = WSET[e_i % 2]
                load_w(wg_d[e_i], ws[0], f"wg{e_i % 2}"); load_w(wu_d[e_i], ws[1], f"wu{e_i % 2}"); load_w(wd_d[e_i], ws[2], f"wd{e_i % 2}")
                for (t0, n, mi), off in zip(blocks, offs):
                    expert_block(e_i, off, t0, n)
            for bi, ((t0, n, mi), off) in enumerate(zip(blocks, offs)):
                finish_block(bi, off, t0, n, mi)
        S.wait_all_final("sp", outs)
        S.emit()
    return nc


def consts_p4():
    p = np.arange(128)
    G = (p[:, None] // 8 == p[None, :] // 8).astype(np.float32)
    sel = np.zeros((128, 16), np.float32); sel[np.arange(16) * 8, np.arange(16)] = 1.0
    selE = np.zeros((16, 16, 128), np.float32)
    for e in range(16):
        selE[e, e, :] = 1.0
    kv = np.zeros((128, 2), np.float32); kv[:, 0] = 1024.0; kv[:, 1] = 32.0
    return dict(G=G, sel=sel, selE=selE, kv=kv, ones=np.ones((128, 128), np.float32))


def prep_p4(inp, wq_l, mods_l, cmods_l, b, half, t2T, xmT, probs_lat_b, probs_ctx_b, probs_own):
    ml, mc = mods_split(mods_l[b]), mods_split(cmods_l)
    m = dict(t2T=t2T, xmT=xmT,
             pl=np.ascontiguousarray(probs_lat_b.T.reshape(128, 1024)), pc=np.ascontiguousarray(probs_ctx_b.T.reshape(128, 32)),
             po=np.ascontiguousarray(probs_own.T),
             g2v=np.ascontiguousarray(np.stack([col_layout(ml["g2"]), col_layout(mc["g2"])], 1)),
             modf=np.ascontiguousarray(np.stack([col_layout(inp["final_g"]), np.zeros((128, 8), np.float32), np.zeros((128, 8), np.float32)], 1)),
             wg=wq_l["exp_w_gate"], wu=wq_l["exp_w_up"], wd=wq_l["exp_w_down"])
    m.update(consts_p4())
    return m


KSCALE = 512 ** -0.5


def build_p5():
    nc = bass.Bass("TRN2", target_bir_lowering=False)
    dt = lambda name, shape, kind="ExternalInput", d=F32: nc.dram_tensor(name, list(shape), d, kind=kind).ap()
    xT = dt("xT", [D, NSEQ])
    wup = dt("wup", [D, 2048])
    modv_d = dt("modv", [128, 5, 8])
    mcv_d = dt("mcv", [128, 16, 6])
    wbd_d = dt("wbd", [3, 128, 16, 128])
    wgt_d = dt("wgt", [128, 48, 8])
    bg_d = dt("bg", [1, 8])
    ones_d = dt("ones", [128, 128]); mask_d = dt("mask4", [64, 256])
    hout = dt("hout", [NSEQ, 2048], kind="ExternalOutput")
    xcT = dt("xcT", [2048, NSEQ], kind="ExternalOutput")
    with ExitStack() as st:
        S = Sched(nc, st)
        pool = PsumPool(S, ["pp0", "pp1", "pp2", "pp3", "pp4"])
        pAT = S.psum("pAT", [128, 512], F32)
        psm = PsumPool(S, ["psm0", "psm1"])
        ones32 = S.sbuf("ones32", [128, 128], F32); load_const(S, ones_d, ones32, "c_ones")
        mask4 = S.sbuf("mask4", [64, 256], F32); load_const(S, mask_d, mask4, "c_mask")
        modv = S.sbuf("modv", [128, 5, 8], F32); load_const(S, modv_d, modv, "c_modv")
        mcv = S.sbuf("mcv", [128, 16, 6], F32); load_const(S, mcv_d, mcv, "c_mcv")
        bgb = S.sbuf("bgb", [64, 8], F32); load_const(S, bg_d.partition_broadcast(64), bgb, "c_bg")
        epsc = S.sbuf("epsc", [128, 1], F32)
        S.op("dve", lambda e: e.memset(epsc[:], EPS), writes=[epsc.tok()])
        onec = S.sbuf("onec", [128, 1], F32)
        S.op("dve", lambda e: e.memset(onec[:], 1.0), writes=[onec.tok()])
        gs = S.sbuf("gs", [128, 2, 8], F32); make_gs(S, modv, gs, 2)
        sh = S.sbuf("sh", [128, 2, 8], F32)
        for i in range(2):
            S.op("dve", lambda e, i=i: e.tensor_copy(out=sh[:, i, :], in_=modv[:, 2 + 2 * i, :]), reads=[modv.tok()], writes=[sh.tok()])
        X0 = S.sbuf("X0", [128, 8, BLK], F32)
        X = [X0, X0]
        wupb = S.sbuf("wupb", [128, 8, 2048], BF16); load_cast_weight(S, wup, 2048, wupb, X, "X")
        wbdb = []
        for j in range(3):
            stg_ = X0
            S.dma("sp", lambda e, stg_=stg_, j=j: e.dma_start(out=stg_[:, :, :].rearrange("p a b -> p (a b)"), in_=wbd_d[j].rearrange("p a b -> p (a b)")), "X0", writes=[stg_.tok()])
            wt = S.sbuf(f"wbd{j}", [128, 16, 128], BF16)
            S.op("dve", lambda e, stg_=stg_, wt=wt: e.tensor_copy(out=wt[:, :, :].rearrange("p a b -> p (a b)"), in_=stg_[:, :, :].rearrange("p a b -> p (a b)")), reads=[stg_.tok()], writes=[wt.tok()])
            wbdb.append(wt)
        wg32 = S.sbuf("wg32", [128, 48, 8], F32); load_const(S, wgt_d, wg32, "c_wgt")
        wgb = S.sbuf("wgb", [128, 48, 8], BF16)
        S.op("dve", lambda e: e.tensor_copy(out=wgb[:], in_=wg32[:]), reads=[wg32.tok()], writes=[wgb.tok()])
        NW = norm_work(S, pool, ones32, epsc)
        XN = S.sbuf("XN", [128, 8, BLK], BF16)
        XMt = S.sbuf("XMt", [128, 8, BLK], F32); XCt = S.sbuf("XCt", [128, 8, BLK], F32)
        XMb = S.sbuf("XMb", [128, 16, BLK], BF16); XCb = S.sbuf("XCb", [128, 16, BLK], BF16)
        QTs = [S.sbuf(f"QT{i}", [128, 16, BLK], BF16) for i in range(2)]
        KTf = S.sbuf("KTf", [128, 16, BLK], BF16); VT = S.sbuf("VT", [128, 16, BLK], BF16)
        C32 = S.sbuf("C32", [128, 16, 516], F32); Cb = S.sbuf("Cb", [128, 16, 516], BF16)
        S.op("dve", lambda e: e.memset(C32[:], 0.0), writes=[C32.tok()])
        S.op("pool", lambda e: e.memset(Cb[:], 0.0), writes=[Cb.tok()])
        HO = Tile(NW["tmpn"][0:64, :, :].rearrange("p a b -> p (a b)"), "HO")
        HO.toks = NW["tmpn"].toks
        AT_ = []
        for i in range(2):
            AT_.append(dict(VV=S.sbuf(f"VV{i}", [64, 4, 516], BF16), KTk=S.sbuf(f"KTk{i}", [64, 2048], BF16),
                            gt=S.sbuf(f"gt{i}", [64, 8], F32), sp=S.sbuf(f"sp{i}", [64, 4], F32), csp=S.sbuf(f"csp{i}", [64, 4], F32),
                            eb=S.sbuf(f"eb{i}", [64, 4], F32), ev=S.sbuf(f"ev{i}", [64, 4], F32), dl=S.sbuf(f"dl{i}", [128, 4], F32),
                            ATs=S.sbuf(f"ATs{i}", [64, 256], BF16), dlk=S.sbuf(f"dlk{i}", [64, 4], F32)))
        dd = S.sbuf("dd", [64, 4], F32); scl = S.sbuf("scl", [64, 4], F32)
        xTv = xT.rearrange("(c p) n -> p c n", p=128)
        xcv = xcT.rearrange("(c p) n -> p c n", p=128)
        outs = []

        def features(bi, t0, n, mi):
            Xb = X0
            QT = QTs[bi % 2]
            seg = 256 if mi == 1 else 64
            S.dma("sp", lambda e: e.dma_start(out=Xb[:, :, :n], in_=xTv[:, :, t0:t0 + n]), "X0", writes=[Xb.tok()])
            norm_fm(S, Xb, n, gs, sh, mi, XN, NW)
            for c in range(16):
                c8 = c % 8
                p = pool.next()
                for kc in range(8):
                    S.op("pe", lambda e, p=p, kc=kc, c=c: e.matmul(p[:, :n], lhsT=wupb[:, kc, c * 128:(c + 1) * 128], rhs=XN[:, kc, :n], start=(kc == 0), stop=(kc == 7)),
                         reads=[wupb.tok(), XN.tok()], writes=[p.tok()])
                S.op("act", lambda e, p=p, c8=c8: e.activation(out=XMt[:, c8, :n], in_=p[:, :n], func=AF.Copy), reads=[p.tok()], writes=[XMt.tok(c8)])
                S.op("pool", lambda e, c=c, c8=c8: e.tensor_scalar(out=XCt[:, c8, :n], in0=XMt[:, c8, :n], scalar1=mcv[:, c, 2:3], scalar2=mcv[:, c, 5:6], op0=ALU.mult, op1=ALU.add),
                     reads=[XMt.tok(c8), mcv.tok()], writes=[XCt.tok(c8)])
                for off in (-2, -1, 1, 2):
                    j = off + 2
                    xrv = XMt[:, c8, :n].rearrange("p (s t) -> p s t", t=seg)
                    xbv = XCt[:, c8, :n].rearrange("p (s t) -> p s t", t=seg)
                    if off < 0:
                        o_sl, i_sl = xbv[:, :, -off:seg], xrv[:, :, 0:seg + off]
                    else:
                        o_sl, i_sl = xbv[:, :, 0:seg - off], xrv[:, :, off:seg]
                    S.op("dve", lambda e, o_sl=o_sl, i_sl=i_sl, c=c, j=j: e.scalar_tensor_tensor(out=o_sl, in0=i_sl, scalar=mcv[:, c, j:j + 1], in1=o_sl, op0=ALU.mult, op1=ALU.add),
                         reads=[XMt.tok(c8), mcv.tok(), XCt.tok(c8)], writes=[XCt.tok(c8)])
                S.op("act", lambda e, c8=c8: e.activation(out=XCt[:, c8, :n], in_=XCt[:, c8, :n], func=AF.Silu), reads=[XCt.tok(c8)], writes=[XCt.tok(c8)])
                S.op("pool", lambda e, c=c, c8=c8: e.tensor_copy(out=XCb[:, c, :n], in_=XCt[:, c8, :n]), reads=[XCt.tok(c8)], writes=[XCb.tok(c)])
                S.op("pool", lambda e, c=c, c8=c8: e.tensor_copy(out=XMb[:, c, :n], in_=XMt[:, c8, :n]), reads=[XMt.tok(c8)], writes=[XMb.tok(c)])
                if c8 == 7:
                    hc = c - 7
                    outs.append(S.dma("sp", lambda e, hc=hc: e.dma_start(out=xcv[:, hc:hc + 8, t0:t0 + n], in_=XCt[:, :, :n]), "XCt", reads=[XCt.tok(k) for k in range(8)]))
            for c in range(16):
                for (j, src, dst, scale) in ((0, XCb, QT, 1.0), (1, XCb, KTf, KSCALE), (2, XMb, VT, 1.0)):
                    p = pool.next()
                    S.op("pe", lambda e, p=p, j=j, c=c, src=src: e.matmul(p[:, :n], lhsT=wbdb[j][:, c, :], rhs=src[:, c, :n], start=True, stop=True),
                         reads=[wbdb[j].tok(), src.tok(c)], writes=[p.tok()])
                    if j == 1:
                        S.op("dve", lambda e, p=p, c=c, dst=dst, scale=scale: e.tensor_scalar(out=dst[:, c, :n], in0=p[:, :n], scalar1=scale, scalar2=None, op0=ALU.mult),
                             reads=[p.tok()], writes=[dst.tok(c)])
                    else:
                        S.op("act", lambda e, p=p, c=c, dst=dst: e.activation(out=dst[:, c, :n], in_=p[:, :n], func=AF.Copy), reads=[p.tok()], writes=[dst.tok(c)])

        def stage_a(g, bi, t0, cc):
            A = AT_[g % 2]
            QT = QTs[bi % 2]
            VV, KTk, gt, sp_, csp, eb, ev, dl, ATs = A["VV"], A["KTk"], A["gt"], A["sp"], A["csp"], A["eb"], A["ev"], A["dl"], A["ATs"]
            cs = slice(cc * 64, (cc + 1) * 64)
            pg = psm.next()
            k = 0
            for src in (QT, KTf, VT):
                for c in range(16):
                    S.op("pe", lambda e, k=k, c=c, src=src: e.matmul(pg[0:64, 0:8], lhsT=src[:, c, cs], rhs=wgb[:, k, :], start=(k == 0), stop=(k == 47)),
                         reads=[src.tok(c), wgb.tok()], writes=[pg.tok()])
                    k += 1
            S.op("dve", lambda e: e.tensor_tensor(out=gt[:], in0=pg[0:64, 0:8], in1=bgb[:], op=ALU.add), reads=[pg.tok(), bgb.tok()], writes=[gt.tok()])
            S.op("act", lambda e: e.activation(out=sp_[:], in_=gt[:, 4:8], func=AF.Exp, scale=-1.0), reads=[gt.tok()], writes=[sp_.tok()])
            S.op("act", lambda e: e.activation(out=sp_[:], in_=sp_[:], func=AF.Ln, bias=onec[0:64, 0:1], scale=1.0), reads=[sp_.tok(), onec.tok()], writes=[sp_.tok()])
            pb = psm.next()
            S.op("pe", lambda e: e.matmul(pb[0:64, 0:4], lhsT=mask4[:, 0:64], rhs=sp_[:, :], start=True, stop=True), reads=[mask4.tok(), sp_.tok()], writes=[pb.tok()])
            S.op("pe", lambda e: e.matmul(pb[:, 8:12], lhsT=ones32[0:64, :], rhs=sp_[:, :], start=True, stop=True), reads=[ones32.tok(), sp_.tok()], writes=[pb.tok()])
            S.op("dve", lambda e: e.tensor_copy(out=csp[:], in_=pb[0:64, 0:4]), reads=[pb.tok()], writes=[csp.tok()])
            S.op("act", lambda e: e.activation(out=eb[:], in_=pb[0:64, 0:4], func=AF.Exp, scale=-1.0), reads=[pb.tok()], writes=[eb.tok()])
            S.op("act", lambda e: e.activation(out=dl[:], in_=pb[:, 8:12], func=AF.Exp, scale=-1.0), reads=[pb.tok()], writes=[dl.tok()])
            S.op("dve", lambda e: e.tensor_tensor(out=ev[:], in0=gt[:, 0:4], in1=csp[:], op=ALU.add), reads=[gt.tok(), csp.tok()], writes=[ev.tok()])
            S.op("act", lambda e: e.activation(out=ev[:], in_=ev[:], func=AF.Exp), reads=[ev.tok()], writes=[ev.tok()])
            dlk = A["dlk"]
            S.op("dve", lambda e: e.tensor_scalar(out=dlk[:], in0=dl[0:64, :], scalar1=KSCALE, scalar2=None, op0=ALU.mult), reads=[dl.tok()], writes=[dlk.tok()])
            for h in range(4):
                p = pool.next()
                for j in range(4):
                    c = 4 * h + j
                    S.op("pe", lambda e, p=p, j=j, c=c: e.matmul(p[0:64, j * 128:(j + 1) * 128], lhsT=XMb[:, c, cs], rhs=wbdb[2][:, c, :], start=True, stop=True),
                         reads=[XMb.tok(c), wbdb[2].tok()], writes=[p.tok()])
                S.op("dve", lambda e, p=p, h=h: e.tensor_scalar(out=VV[:, h, 0:512], in0=p[0:64, :], scalar1=ev[:, h:h + 1], scalar2=None, op0=ALU.mult),
                     reads=[p.tok(), ev.tok()], writes=[VV.tok(h)])
                S.op("pool", lambda e, h=h: e.tensor_copy(out=VV[:, h, 512:513], in_=ev[:, h:h + 1]), reads=[ev.tok()], writes=[VV.tok(h)])
            for h in range(4):
                p = pool.next()
                for j in range(4):
                    c = 4 * h + j
                    S.op("pe", lambda e, p=p, j=j, c=c: e.matmul(p[0:64, j * 128:(j + 1) * 128], lhsT=XCb[:, c, cs], rhs=wbdb[1][:, c, :], start=True, stop=True),
                         reads=[XCb.tok(c), wbdb[1].tok()], writes=[p.tok()])
                S.op("act", lambda e, p=p, h=h: e.activation(out=KTk[:, h * 512:(h + 1) * 512], in_=p[0:64, :], func=AF.Copy, scale=dlk[:, h:h + 1]),
                     reads=[p.tok(), dlk.tok()], writes=[KTk.tok(h)])
            for h in range(4):
                for j in range(4):
                    c = 4 * h + j
                    S.op("pe", lambda e, h=h, j=j, c=c: e.matmul(pAT[0:64, h * 64:(h + 1) * 64], lhsT=KTf[:, c, cs], rhs=QT[:, c, cs], start=(j == 0), stop=(j == 3)),
                         reads=[KTf.tok(c), QT.tok(c)], writes=[pAT.tok()])
            S.op("dve", lambda e: e.tensor_tensor(out=ATs[:], in0=pAT[0:64, 0:256], in1=mask4[:], op=ALU.mult), reads=[pAT.tok(), mask4.tok()], writes=[ATs.tok()])

        def stage_b(g, bi, t0, cc):
            A = AT_[g % 2]
            QT = QTs[bi % 2]
            VV, KTk, eb, dl, ATs = A["VV"], A["KTk"], A["eb"], A["dl"], A["ATs"]
            cs = slice(cc * 64, (cc + 1) * 64)
            pd = psm.next()
            for h in range(4):
                S.op("pe", lambda e, h=h: e.matmul(pd[0:64, h:h + 1], lhsT=ATs[:, h * 64:(h + 1) * 64], rhs=VV[:, h, 512:513], start=True, stop=False),
                     reads=[ATs.tok(), VV.tok(h)], writes=[pd.tok()])
                for j in range(4):
                    c = 4 * h + j
                    S.op("pe", lambda e, h=h, j=j, c=c: e.matmul(pd[0:64, h:h + 1], lhsT=QT[:, c, cs], rhs=Cb[:, c, 512:513], start=False, stop=(j == 3)),
                         reads=[QT.tok(c), Cb.tok(c)], writes=[pd.tok()])
            S.op("dve", lambda e: e.tensor_tensor(out=dd[:], in0=pd[0:64, 0:4], in1=eb[:], op=ALU.mult), reads=[pd.tok(), eb.tok()], writes=[dd.tok()])
            S.op("dve", lambda e: e.tensor_scalar(out=scl[:], in0=dd[:], scalar1=-1.0, scalar2=None, op0=ALU.mult), reads=[dd.tok()], writes=[scl.tok()])
            S.op("dve", lambda e: e.tensor_tensor(out=dd[:], in0=dd[:], in1=scl[:], op=ALU.max), reads=[dd.tok(), scl.tok()], writes=[dd.tok()])
            S.op("dve", lambda e: e.tensor_scalar(out=dd[:], in0=dd[:], scalar1=1.0, scalar2=None, op0=ALU.max), reads=[dd.tok()], writes=[dd.tok()])
            S.op("dve", lambda e: e.reciprocal(out=dd[:], in_=dd[:]), reads=[dd.tok()], writes=[dd.tok()])
            S.op("dve", lambda e: e.tensor_tensor(out=scl[:], in0=dd[:], in1=eb[:], op=ALU.mult), reads=[dd.tok(), eb.tok()], writes=[scl.tok()])
            for h in range(4):
                p = pool.next()
                S.op("pe", lambda e, p=p, h=h: e.matmul(p[0:64, :], lhsT=ATs[:, h * 64:(h + 1) * 64], rhs=VV[:, h, 0:512], start=True, stop=False),
                     reads=[ATs.tok(), VV.tok(h)], writes=[p.tok()])
                for j in range(4):
                    c = 4 * h + j
                    S.op("pe", lambda e, p=p, j=j, c=c: e.matmul(p[0:64, :], lhsT=QT[:, c, cs], rhs=Cb[:, c, 0:512], start=False, stop=(j == 3)),
                         reads=[QT.tok(c), Cb.tok(c)], writes=[p.tok()])
                if h % 2 == 0:
                    S.op("act", lambda e, p=p, h=h: e.activation(out=HO[:, h * 512:(h + 1) * 512], in_=p[0:64, :], func=AF.Copy, scale=scl[:, h:h + 1]),
                         reads=[p.tok(), scl.tok()], writes=[HO.tok()])
                else:
                    S.op("dve", lambda e, p=p, h=h: e.tensor_scalar(out=HO[:, h * 512:(h + 1) * 512], in0=p[0:64, :], scalar1=scl[:, h:h + 1], scalar2=None, op0=ALU.mult),
                         reads=[p.tok(), scl.tok()], writes=[HO.tok()])
            r0 = t0 + cc * 64
            outs.append(S.dma("sp", lambda e: e.dma_start(out=hout[r0:r0 + 64, :], in_=HO[:]), "HO", reads=[HO.tok()]))
            for h in range(4):
                for j in range(4):
                    c = 4 * h + j
                    p = pool.next()
                    S.op("pe", lambda e, p=p, h=h, c=c: e.matmul(p[:, :], lhsT=KTk[:, c * 128:(c + 1) * 128], rhs=VV[:, h, 0:512], start=True, stop=True),
                         reads=[KTk.tok(h), VV.tok(h)], writes=[p.tok()])
                    pn = psm.next()
                    S.op("pe", lambda e, pn=pn, h=h, c=c: e.matmul(pn[:, 0:1], lhsT=KTk[:, c * 128:(c + 1) * 128], rhs=VV[:, h, 512:513], start=True, stop=True),
                         reads=[KTk.tok(h), VV.tok(h)], writes=[pn.tok()])
                    S.op("dve", lambda e, p=p, h=h, c=c: e.scalar_tensor_tensor(out=C32[:, c, 0:512], in0=C32[:, c, 0:512], scalar=dl[:, h:h + 1], in1=p[:, :], op0=ALU.mult, op1=ALU.add),
                         reads=[C32.tok(c), dl.tok(), p.tok()], writes=[C32.tok(c)])
                    S.op("dve", lambda e, pn=pn, h=h, c=c: e.scalar_tensor_tensor(out=C32[:, c, 512:513], in0=C32[:, c, 512:513], scalar=dl[:, h:h + 1], in1=pn[:, 0:1], op0=ALU.mult, op1=ALU.add),
                         reads=[C32.tok(c), dl.tok(), pn.tok()], writes=[C32.tok(c)])
                    if c % 2 == 0:
                        S.op("act", lambda e, c=c: e.activation(out=Cb[:, c, 0:513], in_=C32[:, c, 0:513], func=AF.Copy), reads=[C32.tok(c)], writes=[Cb.tok(c)])
                    else:
                        S.op("pool", lambda e, c=c: e.tensor_copy(out=Cb[:, c, 0:513], in_=C32[:, c, 0:513]), reads=[C32.tok(c)], writes=[Cb.tok(c)])

        chunks = []
        for bi, (t0, n, mi) in enumerate(seq_blocks()):
            for cc in range(n // 64):
                chunks.append((bi, t0, n, mi, cc))
        features(*chunks[0][:4])
        stage_a(0, chunks[0][0], chunks[0][1], chunks[0][4])
        for g, (bi, t0, n, mi, cc) in enumerate(chunks):
            if g + 1 < len(chunks):
                nb, nt0, nn, nmi, ncc = chunks[g + 1]
                if ncc == 0:
                    features(nb, nt0, nn, nmi)
                stage_a(g + 1, nb, nt0, ncc)
            stage_b(g, bi, t0, cc)
        S.wait_all_final("sp", outs)
        S.emit()
    return nc


def bd_tiles(w):
    out = np.zeros((128, 16, 128), np.float32)
    for c in range(16):
        for j in range(32):
            out[4 * j:4 * j + 4, c, 4 * j:4 * j + 4] = w[c * 32 + j]
    return out


def prep_p5(inp, mods1, cmods1, b, d):
    ml, mc = mods_split(mods1[b]), mods_split(cmods1)
    cw = inp["m_conv_w"][0]
    z = np.zeros_like(cw[0])
    taps = [cw[0], cw[1], cw[2], cw[3], z] if d == 0 else [z, cw[3], cw[2], cw[1], cw[0]]
    mcv = np.stack([col_layout(v) for v in taps + [inp["m_conv_b"][0]]], 2)
    wgt = inp["m_w_gates"][0, d]
    m = dict(wup=np.ascontiguousarray(inp["m_w_up"][0][:, :2048]),
             modv=modv_pack(inp["norm1_g"][1], ml, mc, "sc1", "sh1"), mcv=np.ascontiguousarray(mcv),
             wbd=np.ascontiguousarray(np.stack([bd_tiles(inp["m_wq"][0]), bd_tiles(inp["m_wk"][0]), bd_tiles(inp["m_wv"][0])], 0)),
             wgt=np.ascontiguousarray(wgt.reshape(48, 128, 8).transpose(1, 0, 2)),
             bg=np.ascontiguousarray(inp["m_b_gates"][0, d][None, :]))
    c = consts_p1()
    m["ones"] = c["ones"]; m["mask4"] = c["mask4"]
    return m


def build_p6():
    nc = bass.Bass("TRN2", target_bir_lowering=False)
    dt = lambda name, shape, kind="ExternalInput", d=F32: nc.dram_tensor(name, list(shape), d, kind=kind).ap()
    xT = dt("xT", [D, NTOK])
    hf_d, hb_d, xc_d = dt("hf", [2048, NTOK]), dt("hb", [2048, NTOK]), dt("xc", [2048, NTOK])
    wz = dt("wz", [D, 2048]); wdn = dt("wdn", [2048, D]); rw_d = dt("rw", [128, 8, 16])
    modv_d = dt("modv", [128, 5, 8]); modv2_d = dt("modv2", [128, 5, 8]); g1_d = dt("g1v", [128, 2, 8])
    sk_d = dt("skip", [128, 16]); ng_d = dt("ng", [128, 16]); ones_d = dt("ones", [128, 128])
    xmT = dt("xmT", [D, NTOK], kind="ExternalOutput")
    t2T = dt("t2T", [D, NTOK], kind="ExternalOutput")
    probs_d = dt("probs", [NTOK, 16], kind="ExternalOutput")
    with ExitStack() as st:
        S = Sched(nc, st)
        pool = PsumPool(S, ["pp0", "pp1", "pp2", "pp3", "pp4", "pp5"])
        ones32 = S.sbuf("ones32", [128, 128], F32); load_const(S, ones_d, ones32, "c_ones")
        modv = S.sbuf("modv", [128, 5, 8], F32); load_const(S, modv_d, modv, "c_modv")
        modv2 = S.sbuf("modv2", [128, 5, 8], F32); load_const(S, modv2_d, modv2, "c_modv2")
        g1v = S.sbuf("g1v", [128, 2, 8], F32); load_const(S, g1_d, g1v, "c_g1")
        skp = S.sbuf("skp", [128, 16], F32); load_const(S, sk_d, skp, "c_sk")
        ng = S.sbuf("ng", [128, 16], F32); load_const(S, ng_d, ng, "c_ng")
        rw = S.sbuf("rw", [128, 8, 16], F32); load_const(S, rw_d, rw, "c_rw")
        epsc = S.sbuf("epsc", [128, 1], F32)
        S.op("dve", lambda e: e.memset(epsc[:], EPS), writes=[epsc.tok()])
        gs1 = S.sbuf("gs1", [128, 2, 8], F32); make_gs(S, modv, gs1, 2)
        gs2 = S.sbuf("gs2", [128, 2, 8], F32); make_gs(S, modv2, gs2, 2)
        sh1 = S.sbuf("sh1", [128, 2, 8], F32); sh2 = S.sbuf("sh2", [128, 2, 8], F32)
        for i in range(2):
            S.op("dve", lambda e, i=i: e.tensor_copy(out=sh1[:, i, :], in_=modv[:, 2 + 2 * i, :]), reads=[modv.tok()], writes=[sh1.tok()])
            S.op("dve", lambda e, i=i: e.tensor_copy(out=sh2[:, i, :], in_=modv2[:, 2 + 2 * i, :]), reads=[modv2.tok()], writes=[sh2.tok()])
        X = [S.sbuf(f"X{i}", [128, 8, BLK], F32) for i in range(2)]
        wzb = S.sbuf("wzb", [128, 8, 2048], BF16); load_cast_weight(S, wz, 2048, wzb, X, "X")
        wdnb = S.sbuf("wdnb", [128, 16, 1024], BF16)
        wdv = wdn.rearrange("(c p) n -> p c n", p=128)
        for i, c0 in enumerate(range(0, 16, 4)):
            for j, n0 in enumerate(range(0, 1024, 512)):
                s_ = X[(2 * i + j) % 2]
                S.dma("sp", lambda e, s_=s_, c0=c0, n0=n0: e.dma_start(out=s_[:, :, :].rearrange("p a (b c) -> p (a b) c", b=2)[:, 0:4, :].rearrange("p a c -> p a c"), in_=wdv[:, c0:c0 + 4, n0:n0 + 512]) if False else
                      e.dma_start(out=s_[:, 0:4, :], in_=wdv[:, c0:c0 + 4, n0:n0 + 256]), f"X{(2 * i + j) % 2}", writes=[s_.tok()])
                S.dma("sp", lambda e, s_=s_, c0=c0, n0=n0: e.dma_start(out=s_[:, 4:8, :], in_=wdv[:, c0:c0 + 4, n0 + 256:n0 + 512]), f"X{(2 * i + j) % 2}", writes=[s_.tok()])
                S.op("dve", lambda e, s_=s_, c0=c0, n0=n0: e.tensor_copy(out=wdnb[:, c0:c0 + 4, n0:n0 + 256], in_=s_[:, 0:4, :]), reads=[s_.tok()], writes=[wdnb.tok()])
                S.op("pool", lambda e, s_=s_, c0=c0, n0=n0: e.tensor_copy(out=wdnb[:, c0:c0 + 4, n0 + 256:n0 + 512], in_=s_[:, 4:8, :]), reads=[s_.tok()], writes=[wdnb.tok()])
        NW = norm_work(S, pool, ones32, epsc)
        RW = router_work(S)
        XN = S.sbuf("XN", [128, 8, BLK], BF16)
        sz = S.sbuf("sz", [128, 16, BLK], F32)
        HF = S.sbuf("HF", [128, 16, BLK], F32); HB = S.sbuf("HB", [128, 16, BLK], F32); XC = S.sbuf("XC", [128, 16, BLK], F32)
        mean = S.sbuf("mean", [128, BLK], F32); hsd = S.sbuf("hsd", [128, BLK], F32); hrs = S.sbuf("hrs", [128, BLK], F32)
        sqh = S.sbuf("sqh", [128, 4, BLK], F32); t1 = S.sbuf("t1", [128, BLK], F32)
        mrg = S.sbuf("mrg", [128, 16, BLK], BF16)
        XM = [S.sbuf(f"XM{i}", [128, 8, BLK], F32) for i in range(2)]
        T2 = S.sbuf("T2", [128, 8, BLK], F32)
        xTv = xT.rearrange("(c p) n -> p c n", p=128)
        v4 = lambda a: a.rearrange("(c p) n -> p c n", p=128)
        outs = []

        def do_block(bi, t0, n, mi):
            b2 = bi % 2
            Xb, XMb = X[b2], XM[b2]
            S.dma("sp", lambda e: e.dma_start(out=Xb[:, :, :n], in_=xTv[:, :, t0:t0 + n]), f"X{b2}", writes=[Xb.tok()])
            for (src, dst, nm) in ((hf_d, HF, "HF"), (hb_d, HB, "HB"), (xc_d, XC, "XC")):
                S.dma("sp", lambda e, src=src, dst=dst: e.dma_start(out=dst[:, :, :n], in_=v4(src)[:, :, t0:t0 + n]), nm, writes=[dst.tok()])
            norm_fm(S, Xb, n, gs1, sh1, mi, XN, NW)
            for c in range(16):
                p = pool.next()
                for kc in range(8):
                    S.op("pe", lambda e, p=p, kc=kc, c=c: e.matmul(p[:, :n], lhsT=wzb[:, kc, c * 128:(c + 1) * 128], rhs=XN[:, kc, :n], start=(kc == 0), stop=(kc == 7)),
                         reads=[wzb.tok(), XN.tok()], writes=[p.tok()])
                S.op("act", lambda e, p=p, c=c: e.activation(out=sz[:, c, :n], in_=p[:, :n], func=AF.Silu), reads=[p.tok()], writes=[sz.tok()])
            S.op("pool", lambda e: e.tensor_tensor(out=HF[:, :, :n], in0=HF[:, :, :n], in1=HB[:, :, :n], op=ALU.add), reads=[HF.tok(), HB.tok()], writes=[HF.tok()])
            for c in range(16):
                S.op("pool", lambda e, c=c: e.tensor_scalar(out=XC[:, c, :n], in0=XC[:, c, :n], scalar1=skp[:, c:c + 1], scalar2=None, op0=ALU.mult), reads=[XC.tok(), skp.tok()], writes=[XC.tok()])
            for h in range(4):
                hs = slice(4 * h, 4 * h + 4)
                p = pool.next()
                for j in range(4):
                    S.op("pe", lambda e, p=p, j=j, h=h: e.matmul(p[:, :n], lhsT=ones32[:, :], rhs=HF[:, 4 * h + j, :n], start=(j == 0), stop=(j == 3)), reads=[ones32.tok(), HF.tok()], writes=[p.tok()])
                S.op("act", lambda e, p=p: e.activation(out=mean[:, :n], in_=p[:, :n], func=AF.Copy, scale=1.0 / 512), reads=[p.tok()], writes=[mean.tok()])
                for j in range(4):
                    S.op("dve", lambda e, j=j, h=h: e.tensor_tensor(out=HF[:, 4 * h + j, :n], in0=HF[:, 4 * h + j, :n], in1=mean[:, :n], op=ALU.subtract), reads=[HF.tok(), mean.tok()], writes=[HF.tok()])
                S.op("act", lambda e, hs=hs: e.activation(out=sqh[:, :, :n], in_=HF[:, hs, :n], func=AF.Square), reads=[HF.tok()], writes=[sqh.tok()])
                p = pool.next()
                for j in range(4):
                    S.op("pe", lambda e, p=p, j=j: e.matmul(p[:, :n], lhsT=ones32[:, :], rhs=sqh[:, j, :n], start=(j == 0), stop=(j == 3)), reads=[ones32.tok(), sqh.tok()], writes=[p.tok()])
                S.op("act", lambda e, p=p: e.activation(out=hsd[:, :n], in_=p[:, :n], func=AF.Sqrt, bias=epsc[:, 0:1], scale=1.0 / 512), reads=[p.tok(), epsc.tok()], writes=[hsd.tok()])
                S.op("dve", lambda e: e.reciprocal(out=hrs[:, :n], in_=hsd[:, :n]), reads=[hsd.tok()], writes=[hrs.tok()])
                for j in range(4):
                    c = 4 * h + j
                    S.op("dve", lambda e, c=c: e.tensor_tensor(out=t1[:, :n], in0=HF[:, c, :n], in1=hrs[:, :n], op=ALU.mult), reads=[HF.tok(), hrs.tok()], writes=[t1.tok()])
                    S.op("dve", lambda e, c=c: e.scalar_tensor_tensor(out=t1[:, :n], in0=t1[:, :n], scalar=ng[:, c:c + 1], in1=XC[:, c, :n], op0=ALU.mult, op1=ALU.add),
                         reads=[t1.tok(), ng.tok(), XC.tok()], writes=[t1.tok()])
                    S.op("pool", lambda e, c=c: e.tensor_tensor(out=mrg[:, c, :n], in0=t1[:, :n], in1=sz[:, c, :n], op=ALU.mult), reads=[t1.tok(), sz.tok()], writes=[mrg.tok()])
            for oc in range(8):
                p = pool.next()
                for c in range(16):
                    S.op("pe", lambda e, p=p, c=c, oc=oc: e.matmul(p[:, :n], lhsT=wdnb[:, c, oc * 128:(oc + 1) * 128], rhs=mrg[:, c, :n], start=(c == 0), stop=(c == 15)),
                         reads=[wdnb.tok(), mrg.tok()], writes=[p.tok()])
                S.op("dve", lambda e, p=p, oc=oc: e.scalar_tensor_tensor(out=XMb[:, oc, :n], in0=p[:, :n], scalar=g1v[:, mi, oc:oc + 1], in1=Xb[:, oc, :n], op0=ALU.mult, op1=ALU.add),
                     reads=[p.tok(), g1v.tok(), Xb.tok()], writes=[XMb.tok()])
            outs.append(S.dma("sp", lambda e: e.dma_start(out=v4(xmT)[:, :, t0:t0 + n], in_=XMb[:, :, :n]), f"XM{b2}", reads=[XMb.tok()]))
            norm_fm(S, XMb, n, gs2, sh2, mi, T2, NW)
            outs.append(S.dma("sp", lambda e: e.dma_start(out=v4(t2T)[:, :, t0:t0 + n], in_=T2[:, :, :n]), "T2", reads=[T2.tok()]))
            router_softmax(S, T2, n, t0, rw, probs_d, pool, RW, outs, "p6")

        for bi, (t0, n, mi) in enumerate(tok_blocks()):
            do_block(bi, t0, n, mi)
        S.wait_all_final("sp", outs)
        S.emit()
    return nc


def prep_p6(inp, mods1, cmods1, b, half, x_lat, x_ctx, hf, hb, xc):
    ml, mc = mods_split(mods1[b]), mods_split(cmods1)
    T = lambda lat, ctx: np.ascontiguousarray(half_tokens(lat, ctx, half).T)
    return dict(xT=T(x_lat, x_ctx), hf=T(hf["lat"], hf["ctx"]), hb=T(hb["lat"], hb["ctx"]), xc=T(xc["lat"], xc["ctx"]),
                wz=np.ascontiguousarray(inp["m_w_up"][0][:, 2048:]), wdn=np.ascontiguousarray(inp["m_w_down"][0]),
                rw=np.ascontiguousarray(inp["router_w"][1].reshape(8, 128, 16).transpose(1, 0, 2)),
                modv=modv_pack(inp["norm1_g"][1], ml, mc, "sc1", "sh1"), modv2=modv_pack(inp["norm2_g"][1], ml, mc, "sc2", "sh2"),
                g1v=np.ascontiguousarray(np.stack([col_layout(ml["g1"]), col_layout(mc["g1"])], 1)),
                skip=col_layout(inp["m_skip"][0]), ng=col_layout(inp["m_norm_g"][0]), ones=np.ones((128, 128), np.float32))


def build_pw():
    nc = bass.Bass("TRN2", target_bir_lowering=False)
    src = nc.dram_tensor("wsrc", [12, D, 1024], F32, kind="ExternalInput").ap()
    dst = nc.dram_tensor("wdst", [12, D, 1024], BF16, kind="ExternalOutput").ap()
    with ExitStack() as st:
        S = Sched(nc, st)
        stg = [S.sbuf(f"stg{i}", [128, 8, 512], F32) for i in range(3)]
        ob = [S.sbuf(f"ob{i}", [128, 8, 512], BF16) for i in range(3)]
        outs = []
        k = 0
        for m in range(12):
            sv = src[m].rearrange("(c p) n -> p c n", p=128)
            dv = dst[m].rearrange("(c p) n -> p c n", p=128)
            for c0 in range(0, 1024, 512):
                s_, o_ = stg[k % 3], ob[k % 3]
                S.dma("sp", lambda e, s_=s_, sv=sv, c0=c0: e.dma_start(out=s_[:], in_=sv[:, :, c0:c0 + 512]), f"stg{k % 3}", writes=[s_.tok()])
                eng = ("dve", "act", "pool")[k % 3]
                if eng == "act":
                    S.op("act", lambda e, s_=s_, o_=o_: e.activation(out=o_[:], in_=s_[:], func=AF.Copy), reads=[s_.tok()], writes=[o_.tok()])
                else:
                    S.op(eng, lambda e, s_=s_, o_=o_: e.tensor_copy(out=o_[:], in_=s_[:]), reads=[s_.tok()], writes=[o_.tok()])
                outs.append(S.dma("sp", lambda e, o_=o_, dv=dv, c0=c0: e.dma_start(out=dv[:, :, c0:c0 + 512], in_=o_[:]), f"ob{k % 3}", reads=[o_.tok()]))
                k += 1
        S.wait_all_final("sp", outs)
        S.emit()
    return nc


def run_pw(inp):
    names = ["exp_w_gate", "exp_w_up", "exp_w_down"]
    maps = []
    for i in range(8):
        mats = [inp[nm][l, 2 * i + j] for l in range(2) for j in range(2) for nm in names]
        maps.append(dict(wsrc=np.ascontiguousarray(np.stack(mats, 0))))
    res = _run(build_pw(), maps)
    out = [dict(), dict()]
    for l in range(2):
        for mi, nm in enumerate(names):
            out[l][nm] = np.ascontiguousarray(np.concatenate(
                [np.stack([res[i]["wdst"][(l * 2 + j) * 3 + mi] for j in range(2)], 0) for i in range(8)], 0))
    return out


def _run(nc, maps):
    return run_bass_kernel_spmd(nc, maps, core_ids=list(range(len(maps)))).results


def _moe_layer(inp, wq_l, mods_l, cmods_l, res_merge, final):
    maps = []
    for b in range(4):
        pr0, pr1 = res_merge[2 * b]["probs"], res_merge[2 * b + 1]["probs"]
        pl = np.concatenate([pr0[:4096], pr1[:4096]], 0)
        pc = np.concatenate([pr0[4096:], pr1[4096:]], 0)
        for half in range(2):
            r = res_merge[2 * b + half]
            maps.append(prep_p4(inp, wq_l, mods_l, cmods_l, b, half, np.ascontiguousarray(r["t2T"]), np.ascontiguousarray(r["xmT"]), pl, pc, r["probs"]))
    res = _run(build_p4(final), maps)
    return [np.ascontiguousarray(r["xoT"].T) for r in res]


def kernel(**inp):
    inp = {k: np.asarray(v) for k, v in inp.items()}
    mods = run_p0(inp)
    wq = run_pw(inp)
    mods0, cmods0, mods1, cmods1 = mods[0, :4], mods[0, 4], mods[1, :4], mods[1, 4]
    maps = [prep_p1(inp, mods0, cmods0, b, d) for b in range(4) for d in range(2)]
    r1 = _run(build_p1(), maps)
    del maps
    maps = []
    for b in range(4):
        og, hl = [], []
        for d in range(2):
            r = r1[2 * b + d]
            c, l = unseq(r["og"], d); og.append(dict(ctx=c, lat=l))
            c, l = unseq(r["hlT"].T, d); hl.append(dict(ctx=c, lat=l))
        for half in range(2):
            maps.append(prep_p2(inp, mods0, cmods0, b, half, og[0], og[1], hl[0], hl[1]))
    r2 = _run(build_p2(), maps)
    del maps, r1
    xo = _moe_layer(inp, wq[0], mods0, cmods0, r2, False)
    del r2
    x1 = [np.concatenate([xo[2 * b][:4096], xo[2 * b + 1][:4096]], 0) for b in range(4)]
    c1 = [np.concatenate([xo[2 * b][4096:], xo[2 * b + 1][4096:]], 0) for b in range(4)]
    maps = []
    for b in range(4):
        for d in range(2):
            m = prep_p5(inp, mods1, cmods1, b, d)
            m["xT"] = seq_T(c1[b], x1[b], d)
            maps.append(m)
    r5 = _run(build_p5(), maps)
    del maps
    maps = []
    for b in range(4):
        c, l = unseq(r5[2 * b]["hout"], 0); hf = dict(ctx=c, lat=l)
        c, l = unseq(r5[2 * b + 1]["hout"], 1); hb = dict(ctx=c, lat=l)
        c, l = unseq(r5[2 * b]["xcT"].T, 0); xc = dict(ctx=c, lat=l)
        for half in range(2):
            maps.append(prep_p6(inp, mods1, cmods1, b, half, x1[b], c1[b], hf, hb, xc))
    r6 = _run(build_p6(), maps)
    del maps, r5
    xo = _moe_layer(inp, wq[1], mods1, cmods1, r6, True)
    out = np.stack([np.concatenate([xo[2 * b][:4096], xo[2 * b + 1][:4096]], 0) for b in range(4)], 0)
    return np.ascontiguousarray(out.astype(np.float32))
```

```python
import numpy as np
from contextlib import ExitStack
import concourse.bass as bass
import concourse.mybir as mybir
from concourse.bass_utils import run_bass_kernel_spmd

F32 = mybir.dt.float32
BF16 = mybir.dt.bfloat16
I32 = mybir.dt.int32
U32 = mybir.dt.uint32
AF = mybir.ActivationFunctionType
ALU = mybir.AluOpType
AX = mybir.AxisListType


class Tok:
    __slots__ = ("w", "r", "name")

    def __init__(self, name=""):
        self.w = None
        self.r = []
        self.name = name


class Tile:
    def __init__(self, t, name):
        self.t = t
        self.name = name
        self.toks = {}

    def tok(self, key=None):
        k = self.toks.get(key)
        if k is None:
            k = self.toks[key] = Tok(f"{self.name}:{key}")
        return k

    def __getitem__(self, idx):
        return self.t[idx]


class Sched:
    ENGS = ("pe", "dve", "act", "pool", "sp")
    EPOCH = 1500
    NO_SELF_WAIT = ("pe",)

    def __init__(self, nc, stack):
        self.nc = nc
        self.stack = stack
        self.ops = {e: [] for e in self.ops_engines()}
        self.count = {e: 0 for e in self.ops_engines()}
        self.sems = {}
        self.dma_count = {}
        self.waited = {e: {} for e in self.ops_engines()}
        self.n_sem = 0
        self.final = []

    def ops_engines(self):
        return self.ENGS

    def sbuf(self, name, shape, dt):
        t = self.stack.enter_context(self.nc.sbuf_tensor("sb_" + name, list(shape), dt))
        return Tile(t, name)

    def psum(self, name, shape, dt):
        t = self.stack.enter_context(self.nc.psum_tensor("ps_" + name, list(shape), dt))
        return Tile(t, name)

    def dsem(self, name):
        key = ("dma", name)
        if key not in self.sems:
            self.sems[key] = self.stack.enter_context(self.nc.semaphore(f"d_{name}"))
            self.dma_count[key] = 0
        return key

    def _deps(self, reads, writes):
        deps = []
        for t in reads:
            if t.w is not None:
                deps.append(t.w)
        for t in writes:
            if t.w is not None:
                deps.append(t.w)
            deps.extend(t.r)
        return deps

    def _commit(self, comp, reads, writes):
        for t in writes:
            t.w = comp
            t.r = []
        for t in reads:
            if t not in writes:
                t.r.append(comp)

    def op(self, eng, fn, reads=(), writes=()):
        reads = [r for r in reads]
        writes = [w for w in writes]
        deps = self._deps(reads, writes)
        self.count[eng] += 1
        ep = (self.count[eng] - 1) // self.EPOCH
        key = ("eng", eng, ep)
        if key not in self.sems:
            self.sems[key] = self.stack.enter_context(self.nc.semaphore(f"s_{eng}_{ep}"))
        comp = (key, self.count[eng] - ep * self.EPOCH)
        if eng in self.NO_SELF_WAIT:
            deps = [d for d in deps if not (d[0][0] == "eng" and d[0][1] == eng)]
        waits = self._filter_waits(eng, deps, own=None, own_val=None)
        self.ops[eng].append((waits, fn, comp))
        self._commit(comp, reads, writes)
        return comp

    def dma(self, eng, fn, semname, reads=(), writes=()):
        key = self.dsem(semname)
        deps = self._deps(list(reads), list(writes))
        self.dma_count[key] += 16
        comp = (key, self.dma_count[key])
        waits = self._filter_waits(eng, deps, own=None, own_val=None)
        self.ops[eng].append((waits, fn, comp))
        self._commit(comp, list(reads), list(writes))
        return comp

    def _filter_waits(self, eng, deps, own, own_val):
        need = {}
        for (k, v) in deps:
            if need.get(k, -1) < v:
                need[k] = v
        out = []
        wd = self.waited[eng]
        for k, v in need.items():
            if wd.get(k, -1) >= v:
                continue
            wd[k] = v
            out.append((k, v))
        return out

    def wait_all_final(self, eng, comps):
        waits = self._filter_waits(eng, comps, None, None)
        self.ops[eng].append((waits, None, None))

    def emit(self):
        nc = self.nc
        sems = self.sems
        ops = self.ops
        with nc.Block() as block:
            def mk(e):
                def body(engine):
                    for (waits, fn, comp) in ops[e]:
                        for (k, v) in waits:
                            engine.wait_ge(sems[k], v)
                        if fn is None:
                            continue
                        ins = fn(engine)
                        if comp[0][0] == "eng":
                            ins.then_inc(sems[comp[0]], 1)
                        else:
                            ins.then_inc(sems[comp[0]], 16)
                return body
            block.tensor(mk("pe"))
            block.vector(mk("dve"))
            block.scalar(mk("act"))
            block.gpsimd(mk("pool"))
            block.sync(mk("sp"))


D = 1024
NCTX = 256
NLAT = 8192
NSEQ = NCTX + NLAT
EPS = 1e-6
BLK = 256
DEBUG_NBLK = 0


def col_layout(v):
    v = np.asarray(v, np.float32)
    return np.ascontiguousarray(v.reshape(-1, 128).T)


class PsumPool:
    def __init__(self, S, names, shape=(128, 512), dt=F32):
        self.tiles = [S.psum(nm, list(shape), dt) for nm in names]
        self.i = 0

    def next(self):
        t = self.tiles[self.i % len(self.tiles)]
        self.i += 1
        return t


def load_const(S, dram_ap, tile, name, eng="sp"):
    S.dma(eng, lambda e: e.dma_start(out=tile[:], in_=dram_ap), name, writes=[tile.tok()])


def norm_fm(S, X, n, gs, sh, mi, XN, W):
    sq, rstd, sd, tmpn, ones32, pN = W["sq"], W["rstd"], W["sd"], W["tmpn"], W["ones32"], W["pool"].next()
    S.op("act", lambda e: e.activation(out=sq[:, :, :n], in_=X[:, :, :n], func=AF.Square),
         reads=[X.tok()], writes=[sq.tok()])
    for c in range(8):
        S.op("pe", lambda e, c=c: e.matmul(pN[:, :n], lhsT=ones32[:, :], rhs=sq[:, c, :n], start=(c == 0), stop=(c == 7)),
             reads=[ones32.tok(), sq.tok()], writes=[pN.tok()])
    S.op("act", lambda e: e.activation(out=sd[:, :n], in_=pN[:, :n], func=AF.Sqrt, bias=W["epsc"][:, 0:1], scale=1.0 / D),
         reads=[pN.tok(), W["epsc"].tok()], writes=[sd.tok()])
    S.op("dve", lambda e: e.reciprocal(out=rstd[:, :n], in_=sd[:, :n]), reads=[sd.tok()], writes=[rstd.tok()])
    S.op("pool", lambda e: e.tensor_tensor(out=tmpn[:, :, :n], in0=X[:, :, :n],
                                           in1=rstd[:, :n].unsqueeze(1).to_broadcast([128, 8, n]), op=ALU.mult),
         reads=[X.tok(), rstd.tok()], writes=[tmpn.tok()])
    for c in range(8):
        S.op("act", lambda e, c=c: e.activation(out=XN[:, c, :n], in_=tmpn[:, c, :n], func=AF.Identity,
                                                bias=sh[:, mi, c:c + 1], scale=gs[:, mi, c:c + 1]),
             reads=[tmpn.tok(), gs.tok(), sh.tok()], writes=[XN.tok()])


def norm_work(S, pool, ones32, epsc):
    tmpn = S.sbuf("n_tmp", [128, 8, BLK], F32)
    return dict(sq=tmpn, rstd=S.sbuf("n_rstd", [128, BLK], F32),
                sd=S.sbuf("n_sd", [128, BLK], F32), tmpn=tmpn,
                ones32=ones32, pool=pool, epsc=epsc)


def make_gs(S, modv, gs, nsets):
    for i in range(nsets):
        S.op("dve", lambda e, i=i: e.scalar_tensor_tensor(out=gs[:, i, :], in0=modv[:, 1 + 2 * i, :], scalar=1.0,
                                                          in1=modv[:, 0, :], op0=ALU.add, op1=ALU.mult),
             reads=[modv.tok()], writes=[gs.tok()])


def load_cast_weight(S, w_dram, ncols, wb, st, stage_name, chunk=BLK, kchunks=8):
    wv = w_dram.rearrange("(c p) n -> p c n", p=128)
    i = 0
    for c0 in range(0, ncols, chunk):
        cw = min(chunk, ncols - c0)
        s = st[i % 2]
        S.dma("sp", lambda e, s=s, c0=c0, cw=cw: e.dma_start(out=s[:, :, :cw], in_=wv[:, :, c0:c0 + cw]),
              f"{stage_name}{i % 2}", writes=[s.tok()])
        eng = "dve" if i % 2 == 0 else "pool"
        S.op(eng, lambda e, s=s, c0=c0, cw=cw: e.tensor_copy(out=wb[:, :, c0:c0 + cw], in_=s[:, :, :cw]),
             reads=[s.tok()], writes=[wb.tok()])
        i += 1


P1_WCOLS = 1552


def seq_blocks():
    blocks = [(0, NCTX, 1)]
    for i in range(NLAT // BLK):
        blocks.append((NCTX + i * BLK, BLK, 0))
    if DEBUG_NBLK:
        blocks = blocks[:DEBUG_NBLK]
    return blocks


def build_p1():
    nc = bass.Bass("TRN2", target_bir_lowering=False)
    dt = lambda name, shape, kind="ExternalInput", d=F32: nc.dram_tensor(name, list(shape), d, kind=kind).ap()
    xT = dt("xT", [D, NSEQ])
    w1 = dt("w1", [D, P1_WCOLS])
    modv_d = dt("modv", [128, 5, 8])
    wa2_d = dt("wa2", [16, 256])
    ba_d = dt("ba", [64, 4])
    lruc_d = dt("lruc", [128, 4, 9])
    wbd_d = dt("wbd", [128, 2, 4, 128])
    ones_d = dt("ones", [128, 128])
    mask_d = dt("mask4", [64, 256])
    ident_d = dt("ident", [64, 64])
    rmask_d = dt("rmask", [128, BLK])
    og = dt("og", [NSEQ, 512], kind="ExternalOutput")
    hlT = dt("hlT", [512, NSEQ], kind="ExternalOutput")
    with ExitStack() as st:
        S = Sched(nc, st)
        pool = PsumPool(S, ["pp0", "pp1", "pp2"])
        pAT = S.psum("pAT", [128, 512], F32)
        pO = S.psum("pO", [128, 512], F32)
        pU = S.psum("pU", [128, 512], F32)
        pTR = S.psum("pTR", [64, 4, 256], BF16)
        ones32 = S.sbuf("ones32", [128, 128], F32); load_const(S, ones_d, ones32, "c_ones")
        mask4 = S.sbuf("mask4", [64, 256], F32); load_const(S, mask_d, mask4, "c_mask")
        ident32 = S.sbuf("ident32", [64, 64], F32); load_const(S, ident_d, ident32, "c_ident")
        rmask = S.sbuf("rmask", [128, BLK], F32); load_const(S, rmask_d, rmask, "c_rmask")
        modv = S.sbuf("modv", [128, 5, 8], F32); load_const(S, modv_d, modv, "c_modv")
        wa2 = S.sbuf("wa2", [16, 256], F32); load_const(S, wa2_d, wa2, "c_wa2")
        ba = S.sbuf("ba", [64, 4], F32); load_const(S, ba_d, ba, "c_ba")
        lruc = S.sbuf("lruc", [128, 4, 9], F32); load_const(S, lruc_d, lruc, "c_lruc")
        wbd32 = S.sbuf("wbd32", [128, 2, 4, 128], F32); load_const(S, wbd_d, wbd32, "c_wbd")
        identb = S.sbuf("identb", [64, 64], BF16)
        S.op("dve", lambda e: e.tensor_copy(out=identb[:], in_=ident32[:]), reads=[ident32.tok()], writes=[identb.tok()])
        wbdb = S.sbuf("wbdb", [128, 2, 4, 128], BF16)
        S.op("dve", lambda e: e.tensor_copy(out=wbdb[:], in_=wbd32[:]), reads=[wbd32.tok()], writes=[wbdb.tok()])
        nba = S.sbuf("nba", [64, 4], F32)
        S.op("dve", lambda e: e.tensor_scalar(out=nba[:], in0=ba[:], scalar1=-1.0, scalar2=None, op0=ALU.mult),
             reads=[ba.tok()], writes=[nba.tok()])
        epsc = S.sbuf("epsc", [128, 1], F32)
        S.op("dve", lambda e: e.memset(epsc[:], EPS), writes=[epsc.tok()])
        onec = S.sbuf("onec", [128, 1], F32)
        S.op("dve", lambda e: e.memset(onec[:], 1.0), writes=[onec.tok()])
        gs = S.sbuf("gs", [128, 2, 8], F32)
        make_gs(S, modv, gs, 2)
        sh = S.sbuf("sh", [128, 2, 8], F32)
        for i in range(2):
            S.op("dve", lambda e, i=i: e.tensor_copy(out=sh[:, i, :], in_=modv[:, 2 + 2 * i, :]), reads=[modv.tok()], writes=[sh.tok()])
        clam = S.sbuf("clam", [128, 4], F32)
        ctmp = S.sbuf("ctmp", [128, 4], F32)
        S.op("act", lambda e: e.activation(out=ctmp[:], in_=lruc[:, :, 8], func=AF.Exp, scale=-1.0), reads=[lruc.tok()], writes=[ctmp.tok()])
        S.op("act", lambda e: e.activation(out=ctmp[:], in_=ctmp[:], func=AF.Ln, bias=onec[:, 0:1], scale=1.0),
             reads=[ctmp.tok(), onec.tok()], writes=[ctmp.tok()])
        S.op("dve", lambda e: e.tensor_scalar(out=clam[:], in0=ctmp[:], scalar1=-8.0, scalar2=None, op0=ALU.mult),
             reads=[ctmp.tok()], writes=[clam.tok()])
        wb = S.sbuf("wb", [128, 8, P1_WCOLS], BF16)
        X = [S.sbuf(f"X{i}", [128, 8, BLK], F32) for i in range(2)]
        load_cast_weight(S, w1, P1_WCOLS, wb, X, "X")
        NW = norm_work(S, pool, ones32, epsc)
        XN = [S.sbuf(f"XN{i}", [128, 8, BLK], BF16) for i in range(2)]
        q32 = S.sbuf("q32", [64, 4, BLK], F32); k32 = S.sbuf("k32", [64, 4, BLK], F32)
        lr32 = S.sbuf("lr32", [16, BLK], F32)
        e1 = S.sbuf("e1", [64, 4, BLK], F32); sp = e1; csp = S.sbuf("csp", [64, 4, BLK], F32)
        eb = S.sbuf("eb", [64, 4, BLK], F32); enb = S.sbuf("enb", [64, 4, BLK], F32)
        dec = [S.sbuf(f"dec{i}", [64, 4, 4], F32) for i in range(2)]
        qt = [S.sbuf(f"qt{i}", [64, 4, BLK], BF16) for i in range(2)]
        kt = [S.sbuf(f"kt{i}", [64, 4, BLK], BF16) for i in range(2)]
        kh = [S.sbuf(f"kh{i}", [64, 4, BLK], BF16) for i in range(2)]
        vtok = [S.sbuf(f"vtok{i}", [64, 4, 512], BF16) for i in range(2)]
        khtok = [S.sbuf(f"khtok{i}", [64, 4, 256], BF16) for i in range(2)]
        xr = S.sbuf("xr", [128, 4, BLK], F32); xb = S.sbuf("xb", [128, 4, BLK], F32); xbb = S.sbuf("xbb", [128, 4, BLK], BF16)
        rr = S.sbuf("rr", [128, 4, BLK], F32); ii = S.sbuf("ii", [128, 4, BLK], F32)
        aa = S.sbuf("aa", [128, 4, BLK], F32); a2 = rr; uu = ii
        hl = [S.sbuf(f"hl{i}", [128, 4, BLK], F32) for i in range(2)]
        ost = [S.sbuf(f"ost{i}", [64, 4, 512], F32) for i in range(2)]
        ATs = S.sbuf("ATs", [64, 256], BF16)
        S32 = S.sbuf("S32", [64, 512], F32); Sb = S.sbuf("Sb", [64, 512], BF16)
        S.op("dve", lambda e: e.memset(S32[:], 0.0), writes=[S32.tok()])
        S.op("pool", lambda e: e.memset(Sb[:], 0.0), writes=[Sb.tok()])
        xTv = xT.rearrange("(c p) n -> p c n", p=128)
        hlv = hlT.rearrange("(c p) n -> p c n", p=128)
        outs = []
        prev_hl = None
        def do_block(bi, t0, n, mi, prev_hl):
            b2 = bi % 2
            nch = n // 64
            seg = 256 if mi == 1 else 64
            Xb, XNb = X[b2], XN[b2]
            S.dma("sp", lambda e, Xb=Xb, t0=t0, n=n: e.dma_start(out=Xb[:, :, :n], in_=xTv[:, :, t0:t0 + n]), f"X{b2}", writes=[Xb.tok()])
            norm_fm(S, Xb, n, gs, sh, mi, XNb, NW)
            for h in range(4):
                p = pool.next()
                for c in range(8):
                    S.op("pe", lambda e, p=p, c=c, h=h: e.matmul(p[0:64, :n], lhsT=wb[:, c, h * 64:(h + 1) * 64], rhs=XNb[:, c, :n], start=(c == 0), stop=(c == 7)),
                         reads=[wb.tok(), XNb.tok()], writes=[p.tok()])
                S.op("dve", lambda e, p=p, h=h: e.tensor_scalar(out=q32[:, h, :n], in0=p[0:64, :n], scalar1=0.125, scalar2=None, op0=ALU.mult),
                     reads=[p.tok()], writes=[q32.tok()])
            for h in range(4):
                p = pool.next()
                for c in range(8):
                    S.op("pe", lambda e, p=p, c=c, h=h: e.matmul(p[0:64, :n], lhsT=wb[:, c, 256 + h * 64:256 + (h + 1) * 64], rhs=XNb[:, c, :n], start=(c == 0), stop=(c == 7)),
                         reads=[wb.tok(), XNb.tok()], writes=[p.tok()])
                S.op("act", lambda e, p=p, h=h: e.activation(out=k32[:, h, :n], in_=p[0:64, :n], func=AF.Copy),
                     reads=[p.tok()], writes=[k32.tok()])
            p = pool.next()
            for c in range(8):
                S.op("pe", lambda e, p=p, c=c: e.matmul(p[0:16, :n], lhsT=wb[:, c, 1024:1040], rhs=XNb[:, c, :n], start=(c == 0), stop=(c == 7)),
                     reads=[wb.tok(), XNb.tok()], writes=[p.tok()])
            S.op("dve", lambda e, p=p: e.tensor_copy(out=lr32[:, :n], in_=p[0:16, :n]), reads=[p.tok()], writes=[lr32.tok()])
            for h in range(4):
                p = pool.next()
                S.op("pe", lambda e, p=p, h=h: e.matmul(p[0:64, :n], lhsT=wa2[:, h * 64:(h + 1) * 64], rhs=lr32[:, :n], start=True, stop=True),
                     reads=[wa2.tok(), lr32.tok()], writes=[p.tok()])
                S.op("act", lambda e, p=p, h=h: e.activation(out=e1[:, h, :n], in_=p[0:64, :n], func=AF.Exp, bias=nba[:, h:h + 1], scale=-1.0),
                     reads=[p.tok(), nba.tok()], writes=[e1.tok()])
            S.op("act", lambda e: e.activation(out=sp[:, :, :n], in_=e1[:, :, :n], func=AF.Ln, bias=onec[0:64, 0:1], scale=1.0),
                 reads=[e1.tok(), onec.tok()], writes=[sp.tok()])
            for h in range(4):
                S.op("dve", lambda e, h=h: e.tensor_tensor_scan(out=csp[:, h, :n], data0=rmask[0:64, :n], data1=sp[:, h, :n], initial=0.0, op0=ALU.mult, op1=ALU.add),
                     reads=[rmask.tok(), sp.tok()], writes=[csp.tok()])
            S.op("act", lambda e: e.activation(out=eb[:, :, :n], in_=csp[:, :, :n], func=AF.Exp, scale=-1.0 / 16), reads=[csp.tok()], writes=[eb.tok()])
            S.op("act", lambda e: e.activation(out=enb[:, :, :n], in_=csp[:, :, :n], func=AF.Exp, scale=1.0 / 16), reads=[csp.tok()], writes=[enb.tok()])
            decb = dec[b2]
            for h in range(4):
                S.op("dve", lambda e, h=h: e.tensor_copy(out=decb[:, h, :nch], in_=eb[:, h, :n].rearrange("p (c t) -> p c t", t=64)[:, :, 63]),
                     reads=[eb.tok()], writes=[decb.tok()])
            qtb, ktb, khb = qt[b2], kt[b2], kh[b2]
            S.op("dve", lambda e: e.tensor_tensor(out=qtb[:, :, :n], in0=q32[:, :, :n], in1=eb[:, :, :n], op=ALU.mult), reads=[q32.tok(), eb.tok()], writes=[qtb.tok()])
            S.op("pool", lambda e: e.tensor_tensor(out=ktb[:, :, :n], in0=k32[:, :, :n], in1=enb[:, :, :n], op=ALU.mult), reads=[k32.tok(), enb.tok()], writes=[ktb.tok()])
            for h in range(4):
                S.op("dve", lambda e, h=h: e.tensor_tensor(out=khb[:, h, :n].rearrange("p (c t) -> p c t", t=64),
                                                            in0=ktb[:, h, :n].rearrange("p (c t) -> p c t", t=64),
                                                            in1=decb[:, h, :nch].unsqueeze(2).to_broadcast([64, nch, 64]), op=ALU.mult),
                     reads=[ktb.tok(), decb.tok()], writes=[khb.tok()])
            vb = vtok[b2]
            for c in range(nch):
                p = pool.next()
                for kc in range(8):
                    S.op("pe", lambda e, p=p, kc=kc, c=c: e.matmul(p[0:64, :], lhsT=XNb[:, kc, c * 64:(c + 1) * 64], rhs=wb[:, kc, 1040:1552], start=(kc == 0), stop=(kc == 7)),
                         reads=[wb.tok(), XNb.tok()], writes=[p.tok()])
                S.op("act", lambda e, p=p, c=c: e.activation(out=vb[:, c, :], in_=p[0:64, :], func=AF.Copy), reads=[p.tok()], writes=[vb.tok(c)])
            khtb = khtok[b2]
            for c in range(nch):
                r = c % 4
                for h in range(4):
                    S.op("pe", lambda e, r=r, h=h, c=c: e.transpose(out=pTR[:, r, h * 64:(h + 1) * 64], in_=khb[:, h, c * 64:(c + 1) * 64], identity=identb[:]),
                         reads=[khb.tok(), identb.tok()], writes=[pTR.tok()])
                S.op("dve", lambda e, r=r, c=c: e.tensor_copy(out=khtb[:, c, :], in_=pTR[:, r, :]), reads=[pTR.tok()], writes=[khtb.tok(c)])
            for c4 in range(4):
                p = pool.next()
                for kc in range(8):
                    S.op("pe", lambda e, p=p, kc=kc, c4=c4: e.matmul(p[:, :n], lhsT=wb[:, kc, 512 + c4 * 128:512 + (c4 + 1) * 128], rhs=XNb[:, kc, :n], start=(kc == 0), stop=(kc == 7)),
                         reads=[wb.tok(), XNb.tok()], writes=[p.tok()])
                S.op("act", lambda e, p=p, c4=c4: e.activation(out=xr[:, c4, :n], in_=p[:, :n], func=AF.Copy), reads=[p.tok()], writes=[xr.tok()])
            for c4 in range(4):
                S.op("pool", lambda e, c4=c4: e.tensor_scalar(out=xb[:, c4, :n], in0=xr[:, c4, :n], scalar1=lruc[:, c4, 2:3], scalar2=lruc[:, c4, 5:6], op0=ALU.mult, op1=ALU.add),
                     reads=[xr.tok(), lruc.tok()], writes=[xb.tok()])
                for off in (-2, -1, 1, 2):
                    j = off + 2
                    xrv = xr[:, c4, :n].rearrange("p (s t) -> p s t", t=seg)
                    xbv = xb[:, c4, :n].rearrange("p (s t) -> p s t", t=seg)
                    if off < 0:
                        o_sl, i_sl = xbv[:, :, -off:seg], xrv[:, :, 0:seg + off]
                    else:
                        o_sl, i_sl = xbv[:, :, 0:seg - off], xrv[:, :, off:seg]
                    S.op("dve", lambda e, o_sl=o_sl, i_sl=i_sl, c4=c4, j=j: e.scalar_tensor_tensor(out=o_sl, in0=i_sl, scalar=lruc[:, c4, j:j + 1], in1=o_sl, op0=ALU.mult, op1=ALU.add),
                         reads=[xr.tok(), lruc.tok(), xb.tok()], writes=[xb.tok()])
            S.op("pool", lambda e: e.tensor_copy(out=xbb[:, :, :n], in_=xb[:, :, :n]), reads=[xb.tok()], writes=[xbb.tok()])
            for c4 in range(4):
                for (j, dst, bcol) in ((0, rr, 6), (1, ii, 7)):
                    p = pool.next()
                    S.op("pe", lambda e, p=p, j=j, c4=c4: e.matmul(p[:, :n], lhsT=wbdb[:, j, c4, :], rhs=xbb[:, c4, :n], start=True, stop=True),
                         reads=[wbdb.tok(), xbb.tok()], writes=[p.tok()])
                    S.op("act", lambda e, p=p, dst=dst, c4=c4, bcol=bcol: e.activation(out=dst[:, c4, :n], in_=p[:, :n], func=AF.Sigmoid, bias=lruc[:, c4, bcol:bcol + 1], scale=1.0),
                         reads=[p.tok(), lruc.tok()], writes=[dst.tok()])
            for c4 in range(4):
                S.op("act", lambda e, c4=c4: e.activation(out=aa[:, c4, :n], in_=rr[:, c4, :n], func=AF.Exp, scale=clam[:, c4:c4 + 1]),
                     reads=[rr.tok(), clam.tok()], writes=[aa.tok()])
            S.op("pool", lambda e: e.tensor_tensor(out=a2[:, :, :n], in0=aa[:, :, :n], in1=aa[:, :, :n], op=ALU.mult), reads=[aa.tok()], writes=[a2.tok()])
            S.op("act", lambda e: e.activation(out=a2[:, :, :n], in_=a2[:, :, :n], func=AF.Sqrt, bias=onec[:, 0:1], scale=-1.0),
                 reads=[a2.tok(), onec.tok()], writes=[a2.tok()])
            S.op("pool", lambda e: e.tensor_tensor(out=uu[:, :, :n], in0=a2[:, :, :n], in1=ii[:, :, :n], op=ALU.mult), reads=[a2.tok(), ii.tok()], writes=[uu.tok()])
            S.op("pool", lambda e: e.tensor_tensor(out=uu[:, :, :n], in0=uu[:, :, :n], in1=xb[:, :, :n], op=ALU.mult), reads=[uu.tok(), xb.tok()], writes=[uu.tok()])
            hlb = hl[b2]
            for c4 in range(4):
                if prev_hl is None:
                    init, rd = 0.0, []
                else:
                    ph, pn = prev_hl
                    init, rd = ph[:, c4, pn - 1:pn], [ph.tok()]
                S.op("dve", lambda e, c4=c4, init=init: e.tensor_tensor_scan(out=hlb[:, c4, :n], data0=aa[:, c4, :n], data1=uu[:, c4, :n], initial=init, op0=ALU.mult, op1=ALU.add),
                     reads=[aa.tok(), uu.tok()] + rd, writes=[hlb.tok()])
            prev_hl = (hlb, n)
            outs.append(S.dma("sp", lambda e, hlb=hlb, t0=t0, n=n: e.dma_start(out=hlv[:, :, t0:t0 + n], in_=hlb[:, :, :n]), f"hl{b2}", reads=[hlb.tok()]))
            return prev_hl

        def do_chunks(bi, t0, n, mi):
            b2 = bi % 2
            nch = n // 64
            qtb, ktb, khb, vb, khtb, decb = qt[b2], kt[b2], kh[b2], vtok[b2], khtok[b2], dec[b2]
            ob = ost[b2]
            for c in range(nch):
                cs = slice(c * 64, (c + 1) * 64)
                for h in range(4):
                    S.op("pe", lambda e, h=h, cs=cs: e.matmul(pAT[0:64, h * 64:(h + 1) * 64], lhsT=ktb[:, h, cs], rhs=qtb[:, h, cs], start=True, stop=True),
                         reads=[ktb.tok(), qtb.tok()], writes=[pAT.tok()])
                S.op("dve", lambda e: e.tensor_tensor(out=ATs[:], in0=pAT[0:64, 0:256], in1=mask4[:], op=ALU.mult), reads=[pAT.tok(), mask4.tok()], writes=[ATs.tok()])
                for h in range(4):
                    hv = slice(h * 128, (h + 1) * 128)
                    S.op("pe", lambda e, h=h, hv=hv, c=c: e.matmul(pO[0:64, hv], lhsT=ATs[:, h * 64:(h + 1) * 64], rhs=vb[:, c, hv], start=True, stop=False),
                         reads=[ATs.tok(), vb.tok(c)], writes=[pO.tok()])
                    S.op("pe", lambda e, h=h, hv=hv, cs=cs: e.matmul(pO[0:64, hv], lhsT=qtb[:, h, cs], rhs=Sb[:, hv], start=False, stop=True),
                         reads=[qtb.tok(), Sb.tok()], writes=[pO.tok()])
                S.op("act", lambda e, c=c: e.activation(out=ob[:, c, :], in_=pO[0:64, :], func=AF.Copy), reads=[pO.tok()], writes=[ob.tok()])
                for h in range(4):
                    hv = slice(h * 128, (h + 1) * 128)
                    S.op("pe", lambda e, h=h, hv=hv, c=c: e.matmul(pU[0:64, hv], lhsT=khtb[:, c, h * 64:(h + 1) * 64], rhs=vb[:, c, hv], start=True, stop=True),
                         reads=[khtb.tok(c), vb.tok(c)], writes=[pU.tok()])
                for h in range(4):
                    hv = slice(h * 128, (h + 1) * 128)
                    S.op("dve", lambda e, h=h, hv=hv, c=c: e.scalar_tensor_tensor(out=S32[:, hv], in0=S32[:, hv], scalar=decb[:, h, c:c + 1], in1=pU[0:64, hv], op0=ALU.mult, op1=ALU.add),
                         reads=[S32.tok(), decb.tok(), pU.tok()], writes=[S32.tok()])
                S.op("pool", lambda e: e.tensor_copy(out=Sb[:], in_=S32[:]), reads=[S32.tok()], writes=[Sb.tok()])
            outs.append(S.dma("sp", lambda e, ob=ob, t0=t0, n=n, nch=nch: e.dma_start(out=og[t0:t0 + n, :].rearrange("(c s) v -> s c v", s=64), in_=ob[:, :nch, :]),
                              f"ost{b2}", reads=[ob.tok()]))

        blks = seq_blocks()
        prev_hl = do_block(0, *blks[0], prev_hl)
        for bi in range(len(blks)):
            if bi + 1 < len(blks):
                prev_hl = do_block(bi + 1, *blks[bi + 1], prev_hl)
            do_chunks(bi, *blks[bi])
        S.wait_all_final("sp", outs)
        S.emit()
    return nc


def mods_split(m):
    names = ["sh1", "sc1", "g1", "sh2", "sc2", "g2"]
    return {nm: m[..., i * D:(i + 1) * D] for i, nm in enumerate(names)}


def consts_p1():
    s = np.arange(64)[:, None]
    t = np.arange(64)[None, :]
    m = (s <= t).astype(np.float32)
    rm = np.ones((128, BLK), np.float32)
    rm[:, ::64] = 0.0
    return dict(ones=np.ones((128, 128), np.float32), mask4=np.ascontiguousarray(np.tile(m, (1, 4))),
                ident=np.eye(64, dtype=np.float32), rmask=rm)


def seq_T(ctx_b, x_b, d):
    if d == 1:
        ctx_b, x_b = ctx_b[::-1], x_b[::-1]
    return np.ascontiguousarray(np.concatenate([ctx_b, x_b], 0).T)


def unseq(a, d):
    c, l = a[:NCTX], a[NCTX:]
    if d == 1:
        c, l = c[::-1], l[::-1]
    return c, l


def prep_p1(inp, mods0, cmods0, b, d):
    w_in = inp["ab_w_in"][0]
    lr = w_in[:, 1536:1552] if d == 0 else w_in[:, 1552:1568]
    w1 = np.ascontiguousarray(np.concatenate([w_in[:, 0:512], w_in[:, 2080:2592], lr, w_in[:, 512:1024]], 1))
    ml, mc = mods_split(mods0[b]), mods_split(cmods0)
    modv = np.stack([col_layout(inp["norm1_g"][0]), col_layout(ml["sc1"]), col_layout(ml["sh1"]),
                     col_layout(mc["sc1"]), col_layout(mc["sh1"])], 1)
    cw = inp["rg_conv_w"][0]
    z = np.zeros_like(cw[0])
    taps = [cw[0], cw[1], cw[2], cw[3], z] if d == 0 else [z, cw[3], cw[2], cw[1], cw[0]]
    vecs = taps + [inp["rg_conv_b"][0], inp["rg_br"][0, d], inp["rg_bi"][0, d], inp["rg_lambda"][0, d]]
    lruc = np.stack([col_layout(v) for v in vecs], 2)
    wbd = np.zeros((128, 2, 4, 128), np.float32)
    for j, W in enumerate([inp["rg_wr"][0, d], inp["rg_wi"][0, d]]):
        for c4 in range(4):
            wbd[0:64, j, c4, 0:64] = W[2 * c4]
            wbd[64:128, j, c4, 64:128] = W[2 * c4 + 1]
    m = dict(xT=seq_T(inp["ctx"][b], inp["x"][b], d), w1=w1, modv=np.ascontiguousarray(modv),
             wa2=np.ascontiguousarray(inp["gla_wa2"][0, d]), ba=np.ascontiguousarray(inp["gla_ba"][0, d].reshape(4, 64).T),
             lruc=np.ascontiguousarray(lruc), wbd=wbd)
    m.update(consts_p1())
    return m


NTOK = 4096 + 128


def tok_blocks():
    blocks = [(i * BLK, BLK, 0) for i in range(4096 // BLK)]
    blocks.append((4096, 128, 1))
    if DEBUG_NBLK:
        blocks = blocks[:DEBUG_NBLK - 1] + blocks[-1:]
    return blocks


def router_softmax(S, T2, n, t0, rw, probs_d, pool, W, outs, tag):
    for tt in range(n // 128):
        p = pool.next()
        for kc in range(8):
            S.op("pe", lambda e, p=p, kc=kc, tt=tt: e.matmul(p[:, 0:16], lhsT=T2[:, kc, tt * 128:(tt + 1) * 128], rhs=rw[:, kc, :], start=(kc == 0), stop=(kc == 7)),
                 reads=[T2.tok(), rw.tok()], writes=[p.tok()])
        mx, ex, ssum, pr = W["mx"], W["ex"], W["ssum"], W["pr"][tt % 2]
        S.op("dve", lambda e, p=p: e.tensor_reduce(out=mx[:], in_=p[:, 0:16], axis=AX.X, op=ALU.max, negate=True), reads=[p.tok()], writes=[mx.tok()])
        S.op("act", lambda e, p=p: e.activation(out=ex[:], in_=p[:, 0:16], func=AF.Exp, bias=mx[:, 0:1], scale=1.0, accum_out=ssum[:, 0:1]),
             reads=[p.tok(), mx.tok()], writes=[ex.tok(), ssum.tok()])
        S.op("dve", lambda e: e.reciprocal(out=ssum[:], in_=ssum[:]), reads=[ssum.tok()], writes=[ssum.tok()])
        S.op("dve", lambda e, pr=pr: e.tensor_scalar(out=pr[:], in0=ex[:], scalar1=ssum[:, 0:1], scalar2=None, op0=ALU.mult),
             reads=[ex.tok(), ssum.tok()], writes=[pr.tok()])
        r0 = t0 + tt * 128
        outs.append(S.dma("sp", lambda e, pr=pr, r0=r0: e.dma_start(out=probs_d[r0:r0 + 128, :], in_=pr[:]), f"{tag}pr{tt % 2}", reads=[pr.tok()]))


def router_work(S):
    return dict(mx=S.sbuf("r_mx", [128, 1], F32), ex=S.sbuf("r_ex", [128, 16], F32), ssum=S.sbuf("r_ss", [128, 1], F32),
                pr=[S.sbuf(f"r_pr{i}", [128, 16], F32) for i in range(2)])


def build_p2():
    nc = bass.Bass("TRN2", target_bir_lowering=False)
    dt = lambda name, shape, kind="ExternalInput", d=F32: nc.dram_tensor(name, list(shape), d, kind=kind).ap()
    xT = dt("xT", [D, NTOK])
    ogf_d, ogb_d = dt("ogf", [512, NTOK]), dt("ogb", [512, NTOK])
    hlf_d, hlb_d = dt("hlf", [512, NTOK]), dt("hlb", [512, NTOK])
    w2 = dt("w2", [D, 1024]); wout = dt("wout", [1024, D]); rw_d = dt("rw", [128, 8, 16])
    modv_d = dt("modv", [128, 5, 8]); modv2_d = dt("modv2", [128, 5, 8]); g1_d = dt("g1v", [128, 2, 8]); gng_d = dt("gng", [128, 4])
    ones_d = dt("ones", [128, 128])
    xmT = dt("xmT", [D, NTOK], kind="ExternalOutput")
    t2T = dt("t2T", [D, NTOK], kind="ExternalOutput")
    probs_d = dt("probs", [NTOK, 16], kind="ExternalOutput")
    with ExitStack() as st:
        S = Sched(nc, st)
        pool = PsumPool(S, ["pp0", "pp1", "pp2", "pp3", "pp4", "pp5"])
        ones32 = S.sbuf("ones32", [128, 128], F32); load_const(S, ones_d, ones32, "c_ones")
        modv = S.sbuf("modv", [128, 5, 8], F32); load_const(S, modv_d, modv, "c_modv")
        modv2 = S.sbuf("modv2", [128, 5, 8], F32); load_const(S, modv2_d, modv2, "c_modv2")
        g1v = S.sbuf("g1v", [128, 2, 8], F32); load_const(S, g1_d, g1v, "c_g1")
        gng = S.sbuf("gng", [128, 4], F32); load_const(S, gng_d, gng, "c_gng")
        rw = S.sbuf("rw", [128, 8, 16], F32); load_const(S, rw_d, rw, "c_rw")
        epsc = S.sbuf("epsc", [128, 1], F32)
        S.op("dve", lambda e: e.memset(epsc[:], EPS), writes=[epsc.tok()])
        gs1 = S.sbuf("gs1", [128, 2, 8], F32); make_gs(S, modv, gs1, 2)
        gs2 = S.sbuf("gs2", [128, 2, 8], F32); make_gs(S, modv2, gs2, 2)
        sh1 = S.sbuf("sh1", [128, 2, 8], F32); sh2 = S.sbuf("sh2", [128, 2, 8], F32)
        for i in range(2):
            S.op("dve", lambda e, i=i: e.tensor_copy(out=sh1[:, i, :], in_=modv[:, 2 + 2 * i, :]), reads=[modv.tok()], writes=[sh1.tok()])
            S.op("dve", lambda e, i=i: e.tensor_copy(out=sh2[:, i, :], in_=modv2[:, 2 + 2 * i, :]), reads=[modv2.tok()], writes=[sh2.tok()])
        X = [S.sbuf(f"X{i}", [128, 8, BLK], F32) for i in range(2)]
        w2b = S.sbuf("w2b", [128, 8, 1024], BF16); load_cast_weight(S, w2, 1024, w2b, X, "X")
        woutb = S.sbuf("woutb", [128, 8, 1024], BF16); load_cast_weight(S, wout, 1024, woutb, X, "X")
        NW = norm_work(S, pool, ones32, epsc)
        RW = router_work(S)
        XN = [S.sbuf(f"XN{i}", [128, 8, BLK], BF16) for i in range(2)]
        sg = S.sbuf("sg", [128, 4, BLK], F32); rgx = S.sbuf("rgx", [128, 4, BLK], F32)
        gt1 = S.sbuf("gt1", [128, 4, BLK], F32); gt2 = S.sbuf("gt2", [128, 4, BLK], F32)
        OG = [[S.sbuf(f"OG{j}{i}", [128, 4, BLK], F32) for i in range(2)] for j in range(2)]
        HL = [[S.sbuf(f"HL{j}{i}", [128, 4, BLK], F32) for i in range(2)] for j in range(2)]
        osum = S.sbuf("osum", [128, 4, BLK], F32); sqo = S.sbuf("sqo", [128, 4, BLK], F32)
        hrs = S.sbuf("hrs", [128, BLK], F32); hsd = S.sbuf("hsd", [128, BLK], F32); ht = S.sbuf("ht", [128, BLK], F32)
        mg = S.sbuf("mg", [128, 4, BLK], BF16); ml = S.sbuf("ml", [128, 4, BLK], BF16); hsum = S.sbuf("hsum", [128, 4, BLK], F32)
        XM = [S.sbuf(f"XM{i}", [128, 8, BLK], F32) for i in range(2)]
        T2 = [S.sbuf(f"T2{i}", [128, 8, BLK], F32) for i in range(2)]
        xTv = xT.rearrange("(c p) n -> p c n", p=128)
        v4 = lambda a: a.rearrange("(c p) n -> p c n", p=128)
        outs = []

        def do_block(bi, t0, n, mi):
            b2 = bi % 2
            Xb, XNb, XMb, T2b = X[b2], XN[b2], XM[b2], T2[b2]
            S.dma("sp", lambda e: e.dma_start(out=Xb[:, :, :n], in_=xTv[:, :, t0:t0 + n]), f"X{b2}", writes=[Xb.tok()])
            ogs, hls = [OG[0][b2], OG[1][b2]], [HL[0][b2], HL[1][b2]]
            for j, (src, dst) in enumerate([(ogf_d, ogs[0]), (ogb_d, ogs[1]), (hlf_d, hls[0]), (hlb_d, hls[1])]):
                S.dma("sp", lambda e, src=src, dst=dst: e.dma_start(out=dst[:, :, :n], in_=v4(src)[:, :, t0:t0 + n]), f"in{j}{b2}", writes=[dst.tok()])
            norm_fm(S, Xb, n, gs1, sh1, mi, XNb, NW)
            for c4 in range(4):
                p = pool.next()
                for kc in range(8):
                    S.op("pe", lambda e, p=p, kc=kc, c4=c4: e.matmul(p[:, :n], lhsT=w2b[:, kc, c4 * 128:(c4 + 1) * 128], rhs=XNb[:, kc, :n], start=(kc == 0), stop=(kc == 7)),
                         reads=[w2b.tok(), XNb.tok()], writes=[p.tok()])
                S.op("act", lambda e, p=p, c4=c4: e.activation(out=sg[:, c4, :n], in_=p[:, :n], func=AF.Silu), reads=[p.tok()], writes=[sg.tok()])
            for c4 in range(4):
                p = pool.next()
                for kc in range(8):
                    S.op("pe", lambda e, p=p, kc=kc, c4=c4: e.matmul(p[:, :n], lhsT=w2b[:, kc, 512 + c4 * 128:512 + (c4 + 1) * 128], rhs=XNb[:, kc, :n], start=(kc == 0), stop=(kc == 7)),
                         reads=[w2b.tok(), XNb.tok()], writes=[p.tok()])
                S.op("act", lambda e, p=p, c4=c4: e.activation(out=rgx[:, c4, :n], in_=p[:, :n], func=AF.Copy), reads=[p.tok()], writes=[rgx.tok()])
            S.op("pool", lambda e: e.tensor_tensor(out=gt1[:, :, :n], in0=rgx[:, :, :n], in1=rgx[:, :, :n], op=ALU.mult), reads=[rgx.tok()], writes=[gt1.tok()])
            S.op("pool", lambda e: e.tensor_scalar(out=gt1[:, :, :n], in0=gt1[:, :, :n], scalar1=0.044715, scalar2=1.0, op0=ALU.mult, op1=ALU.add), reads=[gt1.tok()], writes=[gt1.tok()])
            S.op("pool", lambda e: e.tensor_tensor(out=gt1[:, :, :n], in0=gt1[:, :, :n], in1=rgx[:, :, :n], op=ALU.mult), reads=[gt1.tok(), rgx.tok()], writes=[gt1.tok()])
            S.op("act", lambda e: e.activation(out=gt2[:, :, :n], in_=gt1[:, :, :n], func=AF.Sigmoid, scale=1.5957691216), reads=[gt1.tok()], writes=[gt2.tok()])
            S.op("pool", lambda e: e.tensor_tensor(out=gt2[:, :, :n], in0=gt2[:, :, :n], in1=rgx[:, :, :n], op=ALU.mult), reads=[gt2.tok(), rgx.tok()], writes=[gt2.tok()])
            S.op("pool", lambda e: e.tensor_tensor(out=osum[:, :, :n], in0=ogs[0][:, :, :n], in1=ogs[1][:, :, :n], op=ALU.add), reads=[ogs[0].tok(), ogs[1].tok()], writes=[osum.tok()])
            S.op("act", lambda e: e.activation(out=sqo[:, :, :n], in_=osum[:, :, :n], func=AF.Square), reads=[osum.tok()], writes=[sqo.tok()])
            for c4 in range(4):
                p = pool.next()
                S.op("pe", lambda e, p=p, c4=c4: e.matmul(p[:, :n], lhsT=ones32[:, :], rhs=sqo[:, c4, :n], start=True, stop=True), reads=[ones32.tok(), sqo.tok()], writes=[p.tok()])
                S.op("act", lambda e, p=p: e.activation(out=hsd[:, :n], in_=p[:, :n], func=AF.Sqrt, bias=epsc[:, 0:1], scale=1.0 / 128), reads=[p.tok(), epsc.tok()], writes=[hsd.tok()])
                S.op("dve", lambda e: e.reciprocal(out=hrs[:, :n], in_=hsd[:, :n]), reads=[hsd.tok()], writes=[hrs.tok()])
                S.op("dve", lambda e, c4=c4: e.tensor_tensor(out=ht[:, :n], in0=osum[:, c4, :n], in1=hrs[:, :n], op=ALU.mult), reads=[osum.tok(), hrs.tok()], writes=[ht.tok()])
                S.op("dve", lambda e, c4=c4: e.scalar_tensor_tensor(out=mg[:, c4, :n], in0=ht[:, :n], scalar=gng[:, c4:c4 + 1], in1=sg[:, c4, :n], op0=ALU.mult, op1=ALU.mult),
                     reads=[ht.tok(), gng.tok(), sg.tok()], writes=[mg.tok()])
            S.op("pool", lambda e: e.tensor_tensor(out=hsum[:, :, :n], in0=hls[0][:, :, :n], in1=hls[1][:, :, :n], op=ALU.add), reads=[hls[0].tok(), hls[1].tok()], writes=[hsum.tok()])
            S.op("pool", lambda e: e.tensor_tensor(out=ml[:, :, :n], in0=hsum[:, :, :n], in1=gt2[:, :, :n], op=ALU.mult), reads=[hsum.tok(), gt2.tok()], writes=[ml.tok()])
            for oc in range(8):
                p = pool.next()
                for kc in range(8):
                    src = mg if kc < 4 else ml
                    S.op("pe", lambda e, p=p, kc=kc, oc=oc, src=src: e.matmul(p[:, :n], lhsT=woutb[:, kc, oc * 128:(oc + 1) * 128], rhs=src[:, kc % 4, :n], start=(kc == 0), stop=(kc == 7)),
                         reads=[woutb.tok(), src.tok()], writes=[p.tok()])
                S.op("dve", lambda e, p=p, oc=oc: e.scalar_tensor_tensor(out=XMb[:, oc, :n], in0=p[:, :n], scalar=g1v[:, mi, oc:oc + 1], in1=Xb[:, oc, :n], op0=ALU.mult, op1=ALU.add),
                     reads=[p.tok(), g1v.tok(), Xb.tok()], writes=[XMb.tok()])
            outs.append(S.dma("sp", lambda e: e.dma_start(out=v4(xmT)[:, :, t0:t0 + n], in_=XMb[:, :, :n]), f"XM{b2}", reads=[XMb.tok()]))
            norm_fm(S, XMb, n, gs2, sh2, mi, T2b, NW)
            outs.append(S.dma("sp", lambda e: e.dma_start(out=v4(t2T)[:, :, t0:t0 + n], in_=T2b[:, :, :n]), f"T2{b2}", reads=[T2b.tok()]))
            router_softmax(S, T2b, n, t0, rw, probs_d, pool, RW, outs, "p2")

        for bi, (t0, n, mi) in enumerate(tok_blocks()):
            do_block(bi, t0, n, mi)
        S.wait_all_final("sp", outs)
        S.emit()
    return nc


def half_tokens(lat_b, ctx_b, half):
    return np.concatenate([lat_b[half * 4096:(half + 1) * 4096], ctx_b[half * 128:(half + 1) * 128]], 0)


def modv_pack(norm_g, ml, mc, sc, sh):
    return np.ascontiguousarray(np.stack([col_layout(norm_g), col_layout(ml[sc]), col_layout(ml[sh]), col_layout(mc[sc]), col_layout(mc[sh])], 1))


def prep_p2(inp, mods0, cmods0, b, half, og_f, og_b, hl_f, hl_b):
    w_in = inp["ab_w_in"][0]
    ml, mc = mods_split(mods0[b]), mods_split(cmods0)
    T = lambda lat, ctx: np.ascontiguousarray(half_tokens(lat, ctx, half).T)
    return dict(xT=T(inp["x"][b], inp["ctx"][b]),
                ogf=T(og_f["lat"], og_f["ctx"]), ogb=T(og_b["lat"], og_b["ctx"]),
                hlf=T(hl_f["lat"], hl_f["ctx"]), hlb=T(hl_b["lat"], hl_b["ctx"]),
                w2=np.ascontiguousarray(np.concatenate([w_in[:, 1024:1536], w_in[:, 1568:2080]], 1)),
                wout=np.ascontiguousarray(inp["ab_w_out"][0]),
                rw=np.ascontiguousarray(inp["router_w"][0].reshape(8, 128, 16).transpose(1, 0, 2)),
                modv=modv_pack(inp["norm1_g"][0], ml, mc, "sc1", "sh1"), modv2=modv_pack(inp["norm2_g"][0], ml, mc, "sc2", "sh2"),
                g1v=np.ascontiguousarray(np.stack([col_layout(ml["g1"]), col_layout(mc["g1"])], 1)),
                gng=col_layout(inp["gla_norm_g"][0]), ones=np.ones((128, 128), np.float32))


def build_p0():
    nc = bass.Bass("TRN2", target_bir_lowering=False)
    dt = lambda name, shape, kind="ExternalInput", d=F32: nc.dram_tensor(name, list(shape), d, kind=kind).ap()
    cT = dt("cT", [128, 8, 5])
    mw = dt("mw", [2, D, 768])
    mb = dt("mb", [2, 5, 768])
    out = dt("mods", [2, 5, 768], kind="ExternalOutput")
    with ExitStack() as st:
        S = Sched(nc, st)
        pool = PsumPool(S, ["pp0", "pp1"])
        c32 = S.sbuf("c32", [128, 8, 5], F32); load_const(S, cT, c32, "c_c")
        sc = S.sbuf("sc", [128, 8, 5], F32)
        S.op("act", lambda e: e.activation(out=sc[:], in_=c32[:], func=AF.Silu), reads=[c32.tok()], writes=[sc.tok()])
        outs = []
        for l in range(2):
            w = S.sbuf(f"w{l}", [128, 8, 768], F32)
            S.dma("sp", lambda e, w=w, l=l: e.dma_start(out=w[:], in_=mw[l].rearrange("(c p) n -> p c n", p=128)), f"w{l}", writes=[w.tok()])
            b = S.sbuf(f"b{l}", [5, 768], F32)
            S.dma("sp", lambda e, b=b, l=l: e.dma_start(out=b[:], in_=mb[l]), f"b{l}", writes=[b.tok()])
            o = S.sbuf(f"o{l}", [5, 768], F32)
            for j in range(2):
                p = pool.next()
                for kc in range(8):
                    S.op("pe", lambda e, p=p, kc=kc, j=j, w=w: e.matmul(p[0:5, 0:384], lhsT=sc[:, kc, :], rhs=w[:, kc, j * 384:(j + 1) * 384], start=(kc == 0), stop=(kc == 7)),
                         reads=[sc.tok(), w.tok()], writes=[p.tok()])
                S.op("dve", lambda e, p=p, j=j, o=o, b=b: e.tensor_tensor(out=o[:, j * 384:(j + 1) * 384], in0=p[0:5, 0:384], in1=b[:, j * 384:(j + 1) * 384], op=ALU.add),
                     reads=[p.tok(), b.tok()], writes=[o.tok()])
            outs.append(S.dma("sp", lambda e, o=o, l=l: e.dma_start(out=out[l], in_=o[:]), f"o{l}", reads=[o.tok()]))
        S.wait_all_final("sp", outs)
        S.emit()
    return nc


def run_p0(inp):
    cv = np.concatenate([inp["c"], inp["c_ctx"][None]], 0)
    cT = np.ascontiguousarray(cv.T.reshape(8, 128, 5).transpose(1, 0, 2))
    maps = []
    for i in range(8):
        sl = slice(i * 768, (i + 1) * 768)
        maps.append(dict(cT=cT, mw=np.ascontiguousarray(inp["mod_w"][:, :, sl]),
                         mb=np.ascontiguousarray(np.broadcast_to(inp["mod_b"][:, None, sl], (2, 5, 768)))))
    res = run_bass_kernel_spmd(build_p0(), maps, core_ids=list(range(8)))
    mods = np.concatenate([r["mods"] for r in res.results], 2)
    return mods


NITER = 26
PASSES = [[(i * BLK, BLK, 0) for i in range(4 * j, 4 * j + 4)] for j in range(4)]
PASSES[3] = PASSES[3] + [(4096, 128, 1)]
PASS_W = 1152


def build_p4(final):
    nc = bass.Bass("TRN2", target_bir_lowering=False)
    dt = lambda name, shape, kind="ExternalInput", d=F32: nc.dram_tensor(name, list(shape), d, kind=kind).ap()
    t2T = dt("t2T", [D, NTOK]); xmT = dt("xmT", [D, NTOK])
    pl_d = dt("pl", [128, 1024]); pc_d = dt("pc", [128, 32])
    po_d = dt("po", [16, NTOK])
    G_d = dt("G", [128, 128]); sel_d = dt("sel", [128, 16]); selE_d = dt("selE", [16, 16, 128]); kv_d = dt("kv", [128, 2])
    g2_d = dt("g2v", [128, 2, 8]); ones_d = dt("ones", [128, 128]); modf_d = dt("modf", [128, 3, 8])
    wg_d = dt("wg", [16, D, 1024], d=BF16); wu_d = dt("wu", [16, D, 1024], d=BF16); wd_d = dt("wd", [16, 1024, D], d=BF16)
    xoT = dt("xoT", [D, NTOK], kind="ExternalOutput")
    with ExitStack() as st:
        S = Sched(nc, st)
        pool = PsumPool(S, ["pp0", "pp1", "pp2", "pp3", "pp4", "pp5", "pp6"])
        pS = S.psum("pS", [128, 512], F32)
        PL = S.sbuf("PL", [128, 1024], F32); load_const(S, pl_d, PL, "c_pl")
        PC = S.sbuf("PC", [128, 32], F32); load_const(S, pc_d, PC, "c_pc")
        PO = S.sbuf("PO", [16, NTOK], F32); load_const(S, po_d, PO, "c_po")
        G = S.sbuf("G", [128, 128], F32); load_const(S, G_d, G, "c_G")
        sel = S.sbuf("sel", [128, 16], F32); load_const(S, sel_d, sel, "c_sel")
        selE = S.sbuf("selE", [16, 16, 128], F32); load_const(S, selE_d, selE, "c_selE")
        kv = S.sbuf("kv", [128, 2], F32); load_const(S, kv_d, kv, "c_kv")
        g2v = S.sbuf("g2v", [128, 2, 8], F32); load_const(S, g2_d, g2v, "c_g2")
        ones32 = S.sbuf("ones32", [128, 128], F32); load_const(S, ones_d, ones32, "c_ones")
        lo = S.sbuf("lo", [128, 2], F32); mid = S.sbuf("mid", [128, 2], F32); cnt = S.sbuf("cnt", [128, 2], F32)
        selm = S.sbuf("selm", [128, 2], F32)
        yacc = S.sbuf("yacc", [128, 8, PASS_W], F32)
        junk = Tile(yacc[:, 0, 0:1024], "junk")
        S.op("dve", lambda e: e.memset(lo[:], 0.0), writes=[lo.tok()])
        for it in range(NITER):
            hk = 2.0 ** -(it + 1)
            S.op("dve", lambda e, hk=hk: e.tensor_scalar(out=mid[:], in0=lo[:], scalar1=hk, scalar2=None, op0=ALU.add), reads=[lo.tok()], writes=[mid.tok()])
            S.op("dve", lambda e: e.tensor_scalar(out=junk[:, :], in0=PL[:, :], scalar1=mid[:, 0:1], scalar2=None, op0=ALU.is_ge, op1=ALU.add, accum_out=cnt[:, 0:1]),
                 reads=[PL.tok(), mid.tok()], writes=[junk.tok(), cnt.tok()])
            S.op("dve", lambda e: e.tensor_scalar(out=junk[:, 0:32], in0=PC[:, :], scalar1=mid[:, 1:2], scalar2=None, op0=ALU.is_ge, op1=ALU.add, accum_out=cnt[:, 1:2]),
                 reads=[PC.tok(), mid.tok()], writes=[junk.tok(), cnt.tok()])
            S.op("pe", lambda e: e.matmul(pS[:, 0:2], lhsT=G[:, :], rhs=cnt[:, :], start=True, stop=True), reads=[G.tok(), cnt.tok()], writes=[pS.tok()])
            S.op("dve", lambda e: e.tensor_tensor(out=selm[:], in0=pS[:, 0:2], in1=kv[:], op=ALU.is_ge), reads=[pS.tok(), kv.tok()], writes=[selm.tok()])
            S.op("dve", lambda e, hk=hk: e.scalar_tensor_tensor(out=lo[:], in0=selm[:], scalar=hk, in1=lo[:], op0=ALU.mult, op1=ALU.add), reads=[selm.tok(), lo.tok()], writes=[lo.tok()])
        thr = S.sbuf("thr", [16, 2], F32)
        S.op("pe", lambda e: e.matmul(pS[0:16, 0:2], lhsT=sel[:, :], rhs=lo[:, :], start=True, stop=True), reads=[sel.tok(), lo.tok()], writes=[pS.tok()])
        S.op("dve", lambda e: e.tensor_copy(out=thr[:], in_=pS[0:16, 0:2]), reads=[pS.tok()], writes=[thr.tok()])
        gateT = PO
        S.op("dve", lambda e: e.scalar_tensor_tensor(out=gateT[:, 0:4096], in0=PO[:, 0:4096], scalar=thr[:, 0:1], in1=PO[:, 0:4096], op0=ALU.is_ge, op1=ALU.mult),
             reads=[PO.tok(), thr.tok()], writes=[gateT.tok()])
        S.op("dve", lambda e: e.scalar_tensor_tensor(out=gateT[:, 4096:NTOK], in0=PO[:, 4096:NTOK], scalar=thr[:, 1:2], in1=PO[:, 4096:NTOK], op0=ALU.is_ge, op1=ALU.mult),
             reads=[PO.tok(), thr.tok()], writes=[gateT.tok()])
        WSET = [[S.sbuf(f"w{nm}{i}", [128, 8, 1024], BF16) for nm in ("g", "u", "d")] for i in range(2)]
        t2b = S.sbuf("t2b", [128, 8, PASS_W], BF16)
        gbc = S.sbuf("gbc", [128, BLK], F32)
        sgt = [S.sbuf(f"sgt{i}", [128, BLK], F32) for i in range(2)]
        hh = [S.sbuf(f"hh{i}", [128, BLK], F32) for i in range(2)]
        hgb = S.sbuf("hgb", [128, 8, BLK], BF16)
        XO = [S.sbuf("XO0", [128, 8, BLK], F32)] * 2
        stg = XO
        v4 = lambda a: a.rearrange("(c p) n -> p c n", p=128)
        outs = []
        if final:
            modf = S.sbuf("modf", [128, 3, 8], F32); load_const(S, modf_d, modf, "c_modf")
            epsc = S.sbuf("epsc", [128, 1], F32)
            S.op("dve", lambda e: e.memset(epsc[:], EPS), writes=[epsc.tok()])
            gsf = S.sbuf("gsf", [128, 1, 8], F32); make_gs(S, modf, gsf, 1)
            shf = S.sbuf("shf", [128, 1, 8], F32)
            S.op("dve", lambda e: e.tensor_copy(out=shf[:, 0, :], in_=modf[:, 2, :]), reads=[modf.tok()], writes=[shf.tok()])
            NW = norm_work(S, pool, ones32, epsc)
            XF = XO
        stgi = [0]

        def load_w(src, dst, nm):
            wv = src.rearrange("(c p) n -> p c n", p=128)
            for hh_ in range(2):
                S.dma("sp", lambda e, hh_=hh_: e.dma_start(out=dst[:, 4 * hh_:4 * hh_ + 4, :], in_=wv[:, 4 * hh_:4 * hh_ + 4, :]), nm, writes=[dst.tok()])

        def expert_block(e_i, off, t0, n):
            wgb, wub, wdb = WSET[e_i % 2]
            p = pool.next()
            S.op("pe", lambda e: e.matmul(p[:, :n], lhsT=selE[:, e_i, :], rhs=gateT[:, t0:t0 + n], start=True, stop=True), reads=[selE.tok(), gateT.tok()], writes=[p.tok()])
            S.op("act", lambda e: e.activation(out=gbc[:, :n], in_=p[:, :n], func=AF.Copy), reads=[p.tok()], writes=[gbc.tok()])
            for fc in range(8):
                pg, pu = pool.next(), pool.next()
                for kc in range(8):
                    S.op("pe", lambda e, kc=kc, fc=fc, pg=pg: e.matmul(pg[:, :n], lhsT=wgb[:, kc, fc * 128:(fc + 1) * 128], rhs=t2b[:, kc, off:off + n], start=(kc == 0), stop=(kc == 7)),
                         reads=[wgb.tok(), t2b.tok()], writes=[pg.tok()])
                for kc in range(8):
                    S.op("pe", lambda e, kc=kc, fc=fc, pu=pu: e.matmul(pu[:, :n], lhsT=wub[:, kc, fc * 128:(fc + 1) * 128], rhs=t2b[:, kc, off:off + n], start=(kc == 0), stop=(kc == 7)),
                         reads=[wub.tok(), t2b.tok()], writes=[pu.tok()])
                sg_, hh_ = sgt[fc % 2], hh[fc % 2]
                S.op("act", lambda e, sg_=sg_, pg=pg: e.activation(out=sg_[:, :n], in_=pg[:, :n], func=AF.Silu), reads=[pg.tok()], writes=[sg_.tok()])
                S.op("dve", lambda e, hh_=hh_, pu=pu, sg_=sg_: e.tensor_tensor(out=hh_[:, :n], in0=pu[:, :n], in1=sg_[:, :n], op=ALU.mult), reads=[pu.tok(), sg_.tok()], writes=[hh_.tok()])
                S.op("pool", lambda e, fc=fc, hh_=hh_: e.tensor_tensor(out=hgb[:, fc, :n], in0=hh_[:, :n], in1=gbc[:, :n], op=ALU.mult), reads=[hh_.tok(), gbc.tok()], writes=[hgb.tok()])
            for dc in range(8):
                py = pool.next()
                for fc in range(8):
                    S.op("pe", lambda e, fc=fc, dc=dc, py=py: e.matmul(py[:, :n], lhsT=wdb[:, fc, dc * 128:(dc + 1) * 128], rhs=hgb[:, fc, :n], start=(fc == 0), stop=(fc == 7)),
                         reads=[wdb.tok(), hgb.tok()], writes=[py.tok()])
                if e_i == 0:
                    S.op("dve", lambda e, dc=dc, py=py: e.tensor_copy(out=yacc[:, dc, off:off + n], in_=py[:, :n]), reads=[py.tok()], writes=[yacc.tok((off, dc)), junk.tok()])
                else:
                    S.op("dve", lambda e, dc=dc, py=py: e.tensor_tensor(out=yacc[:, dc, off:off + n], in0=py[:, :n], in1=yacc[:, dc, off:off + n], op=ALU.add),
                         reads=[py.tok(), yacc.tok((off, dc))], writes=[yacc.tok((off, dc))])

        def finish_block(bi, off, t0, n, mi):
            XOb = XO[bi % 2]
            S.dma("sp", lambda e: e.dma_start(out=XOb[:, :, :n], in_=v4(xmT)[:, :, t0:t0 + n]), "XO0", writes=[XOb.tok()])
            for dc in range(8):
                S.op("dve", lambda e, dc=dc: e.scalar_tensor_tensor(out=XOb[:, dc, :n], in0=yacc[:, dc, off:off + n], scalar=g2v[:, mi, dc:dc + 1], in1=XOb[:, dc, :n], op0=ALU.mult, op1=ALU.add),
                     reads=[yacc.tok((off, dc)), g2v.tok(), XOb.tok()], writes=[XOb.tok()])
            if final:
                XFb = XF[bi % 2]
                norm_fm(S, XOb, n, gsf, shf, 0, XFb, NW)
                outs.append(S.dma("sp", lambda e: e.dma_start(out=v4(xoT)[:, :, t0:t0 + n], in_=XFb[:, :, :n]), "XFo", reads=[XFb.tok()]))
            else:
                outs.append(S.dma("sp", lambda e: e.dma_start(out=v4(xoT)[:, :, t0:t0 + n], in_=XOb[:, :, :n]), "XOo", reads=[XOb.tok()]))

        npass = 1 if DEBUG_NBLK else 4
        nexp = DEBUG_NBLK if DEBUG_NBLK else 16
        for ps_i in range(npass):
            blocks = PASSES[ps_i]
            if DEBUG_NBLK:
                blocks = blocks[:1]
            offs = []
            off = 0
            for (t0, n, mi) in blocks:
                offs.append(off)
                s_ = stg[0]
                def ld(s_=s_, t0=t0, n=n, off=off):
                    S.dma("sp", lambda e: e.dma_start(out=s_[:, :, :n], in_=v4(t2T)[:, :, t0:t0 + n]), "XO0", writes=[s_.tok()])
                    S.op("pool", lambda e: e.tensor_copy(out=t2b[:, :, off:off + n], in_=s_[:, :, :n]), reads=[s_.tok()], writes=[t2b.tok()])
                ld()
                stgi[0] += 1
                off += n
            for e_i in range(nexp):
                ws = WSET[e_i % 2]
                load_w(wg_d[e_i], ws[0], f"wg{e_i % 2}"); load_w(wu_d[e_i], ws[1], f"wu{e_i % 2}"); load_w(wd_d[e_i], ws[2], f"wd{e_i % 2}")
                for (t0, n, mi), off in zip(blocks, offs):
                    expert_block(e_i, off, t0, n)
            for bi, ((t0, n, mi), off) in enumerate(zip(blocks, offs)):
                finish_block(bi, off, t0, n, mi)
        S.wait_all_final("sp", outs)
        S.emit()
    return nc


def consts_p4():
    p = np.arange(128)
    G = (p[:, None] // 8 == p[None, :] // 8).astype(np.float32)
    sel = np.zeros((128, 16), np.float32); sel[np.arange(16) * 8, np.arange(16)] = 1.0
    selE = np.zeros((16, 16, 128), np.float32)
    for e in range(16):
        selE[e, e, :] = 1.0
    kv = np.zeros((128, 2), np.float32); kv[:, 0] = 1024.0; kv[:, 1] = 32.0
    return dict(G=G, sel=sel, selE=selE, kv=kv, ones=np.ones((128, 128), np.float32))


def prep_p4(inp, wq_l, mods_l, cmods_l, b, half, t2T, xmT, probs_lat_b, probs_ctx_b, probs_own):
    ml, mc = mods_split(mods_l[b]), mods_split(cmods_l)
    m = dict(t2T=t2T, xmT=xmT,
             pl=np.ascontiguousarray(probs_lat_b.T.reshape(128, 1024)), pc=np.ascontiguousarray(probs_ctx_b.T.reshape(128, 32)),
             po=np.ascontiguousarray(probs_own.T),
             g2v=np.ascontiguousarray(np.stack([col_layout(ml["g2"]), col_layout(mc["g2"])], 1)),
             modf=np.ascontiguousarray(np.stack([col_layout(inp["final_g"]), np.zeros((128, 8), np.float32), np.zeros((128, 8), np.float32)], 1)),
             wg=wq_l["exp_w_gate"], wu=wq_l["exp_w_up"], wd=wq_l["exp_w_down"])
    m.update(consts_p4())
    return m


KSCALE = 512 ** -0.5


def build_p5():
    nc = bass.Bass("TRN2", target_bir_lowering=False)
    dt = lambda name, shape, kind="ExternalInput", d=F32: nc.dram_tensor(name, list(shape), d, kind=kind).ap()
    xT = dt("xT", [D, NSEQ])
    wup = dt("wup", [D, 2048])
    modv_d = dt("modv", [128, 5, 8])
    mcv_d = dt("mcv", [128, 16, 6])
    wbd_d = dt("wbd", [3, 128, 16, 128])
    wgt_d = dt("wgt", [128, 48, 8])
    bg_d = dt("bg", [1, 8])
    ones_d = dt("ones", [128, 128]); mask_d = dt("mask4", [64, 256])
    hout = dt("hout", [NSEQ, 2048], kind="ExternalOutput")
    xcT = dt("xcT", [2048, NSEQ], kind="ExternalOutput")
    with ExitStack() as st:
        S = Sched(nc, st)
        pool = PsumPool(S, ["pp0", "pp1", "pp2", "pp3", "pp4"])
        pAT = S.psum("pAT", [128, 512], F32)
        psm = PsumPool(S, ["psm0", "psm1"])
        ones32 = S.sbuf("ones32", [128, 128], F32); load_const(S, ones_d, ones32, "c_ones")
        mask4 = S.sbuf("mask4", [64, 256], F32); load_const(S, mask_d, mask4, "c_mask")
        modv = S.sbuf("modv", [128, 5, 8], F32); load_const(S, modv_d, modv, "c_modv")
        mcv = S.sbuf("mcv", [128, 16, 6], F32); load_const(S, mcv_d, mcv, "c_mcv")
        bgb = S.sbuf("bgb", [64, 8], F32); load_const(S, bg_d.partition_broadcast(64), bgb, "c_bg")
        epsc = S.sbuf("epsc", [128, 1], F32)
        S.op("dve", lambda e: e.memset(epsc[:], EPS), writes=[epsc.tok()])
        onec = S.sbuf("onec", [128, 1], F32)
        S.op("dve", lambda e: e.memset(onec[:], 1.0), writes=[onec.tok()])
        gs = S.sbuf("gs", [128, 2, 8], F32); make_gs(S, modv, gs, 2)
        sh = S.sbuf("sh", [128, 2, 8], F32)
        for i in range(2):
            S.op("dve", lambda e, i=i: e.tensor_copy(out=sh[:, i, :], in_=modv[:, 2 + 2 * i, :]), reads=[modv.tok()], writes=[sh.tok()])
        X0 = S.sbuf("X0", [128, 8, BLK], F32)
        X = [X0, X0]
        wupb = S.sbuf("wupb", [128, 8, 2048], BF16); load_cast_weight(S, wup, 2048, wupb, X, "X")
        wbdb = []
        for j in range(3):
            stg_ = X0
            S.dma("sp", lambda e, stg_=stg_, j=j: e.dma_start(out=stg_[:, :, :].rearrange("p a b -> p (a b)"), in_=wbd_d[j].rearrange("p a b -> p (a b)")), "X0", writes=[stg_.tok()])
            wt = S.sbuf(f"wbd{j}", [128, 16, 128], BF16)
            S.op("dve", lambda e, stg_=stg_, wt=wt: e.tensor_copy(out=wt[:, :, :].rearrange("p a b -> p (a b)"), in_=stg_[:, :, :].rearrange("p a b -> p (a b)")), reads=[stg_.tok()], writes=[wt.tok()])
            wbdb.append(wt)
        wg32 = S.sbuf("wg32", [128, 48, 8], F32); load_const(S, wgt_d, wg32, "c_wgt")
        wgb = S.sbuf("wgb", [128, 48, 8], BF16)
        S.op("dve", lambda e: e.tensor_copy(out=wgb[:], in_=wg32[:]), reads=[wg32.tok()], writes=[wgb.tok()])
        NW = norm_work(S, pool, ones32, epsc)
        XN = S.sbuf("XN", [128, 8, BLK], BF16)
        XMt = S.sbuf("XMt", [128, 8, BLK], F32); XCt = S.sbuf("XCt", [128, 8, BLK], F32)
        XMb = S.sbuf("XMb", [128, 16, BLK], BF16); XCb = S.sbuf("XCb", [128, 16, BLK], BF16)
        QTs = [S.sbuf(f"QT{i}", [128, 16, BLK], BF16) for i in range(2)]
        KTf = S.sbuf("KTf", [128, 16, BLK], BF16); VT = S.sbuf("VT", [128, 16, BLK], BF16)
        C32 = S.sbuf("C32", [128, 16, 516], F32); Cb = S.sbuf("Cb", [128, 16, 516], BF16)
        S.op("dve", lambda e: e.memset(C32[:], 0.0), writes=[C32.tok()])
        S.op("pool", lambda e: e.memset(Cb[:], 0.0), writes=[Cb.tok()])
        HO = Tile(NW["tmpn"][0:64, :, :].rearrange("p a b -> p (a b)"), "HO")
        HO.toks = NW["tmpn"].toks
        AT_ = []
        for i in range(2):
            AT_.append(dict(VV=S.sbuf(f"VV{i}", [64, 4, 516], BF16), KTk=S.sbuf(f"KTk{i}", [64, 2048], BF16),
                            gt=S.sbuf(f"gt{i}", [64, 8], F32), sp=S.sbuf(f"sp{i}", [64, 4], F32), csp=S.sbuf(f"csp{i}", [64, 4], F32),
                            eb=S.sbuf(f"eb{i}", [64, 4], F32), ev=S.sbuf(f"ev{i}", [64, 4], F32), dl=S.sbuf(f"dl{i}", [128, 4], F32),
                            ATs=S.sbuf(f"ATs{i}", [64, 256], BF16), dlk=S.sbuf(f"dlk{i}", [64, 4], F32)))
        dd = S.sbuf("dd", [64, 4], F32); scl = S.sbuf("scl", [64, 4], F32)
        xTv = xT.rearrange("(c p) n -> p c n", p=128)
        xcv = xcT.rearrange("(c p) n -> p c n", p=128)
        outs = []

        def features(bi, t0, n, mi):
            Xb = X0
            QT = QTs[bi % 2]
            seg = 256 if mi == 1 else 64
            S.dma("sp", lambda e: e.dma_start(out=Xb[:, :, :n], in_=xTv[:, :, t0:t0 + n]), "X0", writes=[Xb.tok()])
            norm_fm(S, Xb, n, gs, sh, mi, XN, NW)
            for c in range(16):
                c8 = c % 8
                p = pool.next()
                for kc in range(8):
                    S.op("pe", lambda e, p=p, kc=kc, c=c: e.matmul(p[:, :n], lhsT=wupb[:, kc, c * 128:(c + 1) * 128], rhs=XN[:, kc, :n], start=(kc == 0), stop=(kc == 7)),
                         reads=[wupb.tok(), XN.tok()], writes=[p.tok()])
                S.op("act", lambda e, p=p, c8=c8: e.activation(out=XMt[:, c8, :n], in_=p[:, :n], func=AF.Copy), reads=[p.tok()], writes=[XMt.tok(c8)])
                S.op("pool", lambda e, c=c, c8=c8: e.tensor_scalar(out=XCt[:, c8, :n], in0=XMt[:, c8, :n], scalar1=mcv[:, c, 2:3], scalar2=mcv[:, c, 5:6], op0=ALU.mult, op1=ALU.add),
                     reads=[XMt.tok(c8), mcv.tok()], writes=[XCt.tok(c8)])
                for off in (-2, -1, 1, 2):
                    j = off + 2
                    xrv = XMt[:, c8, :n].rearrange("p (s t) -> p s t", t=seg)
                    xbv = XCt[:, c8, :n].rearrange("p (s t) -> p s t", t=seg)
                    if off < 0:
                        o_sl, i_sl = xbv[:, :, -off:seg], xrv[:, :, 0:seg + off]
                    else:
                        o_sl, i_sl = xbv[:, :, 0:seg - off], xrv[:, :, off:seg]
                    S.op("dve", lambda e, o_sl=o_sl, i_sl=i_sl, c=c, j=j: e.scalar_tensor_tensor(out=o_sl, in0=i_sl, scalar=mcv[:, c, j:j + 1], in1=o_sl, op0=ALU.mult, op1=ALU.add),
                         reads=[XMt.tok(c8), mcv.tok(), XCt.tok(c8)], writes=[XCt.tok(c8)])
                S.op("act", lambda e, c8=c8: e.activation(out=XCt[:, c8, :n], in_=XCt[:, c8, :n], func=AF.Silu), reads=[XCt.tok(c8)], writes=[XCt.tok(c8)])
                S.op("pool", lambda e, c=c, c8=c8: e.tensor_copy(out=XCb[:, c, :n], in_=XCt[:, c8, :n]), reads=[XCt.tok(c8)], writes=[XCb.tok(c)])
                S.op("pool", lambda e, c=c, c8=c8: e.tensor_copy(out=XMb[:, c, :n], in_=XMt[:, c8, :n]), reads=[XMt.tok(c8)], writes=[XMb.tok(c)])
                if c8 == 7:
                    hc = c - 7
                    outs.append(S.dma("sp", lambda e, hc=hc: e.dma_start(out=xcv[:, hc:hc + 8, t0:t0 + n], in_=XCt[:, :, :n]), "XCt", reads=[XCt.tok(k) for k in range(8)]))
            for c in range(16):
                for (j, src, dst, scale) in ((0, XCb, QT, 1.0), (1, XCb, KTf, KSCALE), (2, XMb, VT, 1.0)):
                    p = pool.next()
                    S.op("pe", lambda e, p=p, j=j, c=c, src=src: e.matmul(p[:, :n], lhsT=wbdb[j][:, c, :], rhs=src[:, c, :n], start=True, stop=True),
                         reads=[wbdb[j].tok(), src.tok(c)], writes=[p.tok()])
                    if j == 1:
                        S.op("dve", lambda e, p=p, c=c, dst=dst, scale=scale: e.tensor_scalar(out=dst[:, c, :n], in0=p[:, :n], scalar1=scale, scalar2=None, op0=ALU.mult),
                             reads=[p.tok()], writes=[dst.tok(c)])
                    else:
                        S.op("act", lambda e, p=p, c=c, dst=dst: e.activation(out=dst[:, c, :n], in_=p[:, :n], func=AF.Copy), reads=[p.tok()], writes=[dst.tok(c)])

        def stage_a(g, bi, t0, cc):
            A = AT_[g % 2]
            QT = QTs[bi % 2]
            VV, KTk, gt, sp_, csp, eb, ev, dl, ATs = A["VV"], A["KTk"], A["gt"], A["sp"], A["csp"], A["eb"], A["ev"], A["dl"], A["ATs"]
            cs = slice(cc * 64, (cc + 1) * 64)
            pg = psm.next()
            k = 0
            for src in (QT, KTf, VT):
                for c in range(16):
                    S.op("pe", lambda e, k=k, c=c, src=src: e.matmul(pg[0:64, 0:8], lhsT=src[:, c, cs], rhs=wgb[:, k, :], start=(k == 0), stop=(k == 47)),
                         reads=[src.tok(c), wgb.tok()], writes=[pg.tok()])
                    k += 1
            S.op("dve", lambda e: e.tensor_tensor(out=gt[:], in0=pg[0:64, 0:8], in1=bgb[:], op=ALU.add), reads=[pg.tok(), bgb.tok()], writes=[gt.tok()])
            S.op("act", lambda e: e.activation(out=sp_[:], in_=gt[:, 4:8], func=AF.Exp, scale=-1.0), reads=[gt.tok()], writes=[sp_.tok()])
            S.op("act", lambda e: e.activation(out=sp_[:], in_=sp_[:], func=AF.Ln, bias=onec[0:64, 0:1], scale=1.0), reads=[sp_.tok(), onec.tok()], writes=[sp_.tok()])
            pb = psm.next()
            S.op("pe", lambda e: e.matmul(pb[0:64, 0:4], lhsT=mask4[:, 0:64], rhs=sp_[:, :], start=True, stop=True), reads=[mask4.tok(), sp_.tok()], writes=[pb.tok()])
            S.op("pe", lambda e: e.matmul(pb[:, 8:12], lhsT=ones32[0:64, :], rhs=sp_[:, :], start=True, stop=True), reads=[ones32.tok(), sp_.tok()], writes=[pb.tok()])
            S.op("dve", lambda e: e.tensor_copy(out=csp[:], in_=pb[0:64, 0:4]), reads=[pb.tok()], writes=[csp.tok()])
            S.op("act", lambda e: e.activation(out=eb[:], in_=pb[0:64, 0:4], func=AF.Exp, scale=-1.0), reads=[pb.tok()], writes=[eb.tok()])
            S.op("act", lambda e: e.activation(out=dl[:], in_=pb[:, 8:12], func=AF.Exp, scale=-1.0), reads=[pb.tok()], writes=[dl.tok()])
            S.op("dve", lambda e: e.tensor_tensor(out=ev[:], in0=gt[:, 0:4], in1=csp[:], op=ALU.add), reads=[gt.tok(), csp.tok()], writes=[ev.tok()])
            S.op("act", lambda e: e.activation(out=ev[:], in_=ev[:], func=AF.Exp), reads=[ev.tok()], writes=[ev.tok()])
            dlk = A["dlk"]
            S.op("dve", lambda e: e.tensor_scalar(out=dlk[:], in0=dl[0:64, :], scalar1=KSCALE, scalar2=None, op0=ALU.mult), reads=[dl.tok()], writes=[dlk.tok()])
            for h in range(4):
                p = pool.next()
                for j in range(4):
                    c = 4 * h + j
                    S.op("pe", lambda e, p=p, j=j, c=c: e.matmul(p[0:64, j * 128:(j + 1) * 128], lhsT=XMb[:, c, cs], rhs=wbdb[2][:, c, :], start=True, stop=True),
                         reads=[XMb.tok(c), wbdb[2].tok()], writes=[p.tok()])
                S.op("dve", lambda e, p=p, h=h: e.tensor_scalar(out=VV[:, h, 0:512], in0=p[0:64, :], scalar1=ev[:, h:h + 1], scalar2=None, op0=ALU.mult),
                     reads=[p.tok(), ev.tok()], writes=[VV.tok(h)])
                S.op("pool", lambda e, h=h: e.tensor_copy(out=VV[:, h, 512:513], in_=ev[:, h:h + 1]), reads=[ev.tok()], writes=[VV.tok(h)])
            for h in range(4):
                p = pool.next()
                for j in range(4):
                    c = 4 * h + j
                    S.op("pe", lambda e, p=p, j=j, c=c: e.matmul(p[0:64, j * 128:(j + 1) * 128], lhsT=XCb[:, c, cs], rhs=wbdb[1][:, c, :], start=True, stop=True),
                         reads=[XCb.tok(c), wbdb[1].tok()], writes=[p.tok()])
                S.op("act", lambda e, p=p, h=h: e.activation(out=KTk[:, h * 512:(h + 1) * 512], in_=p[0:64, :], func=AF.Copy, scale=dlk[:, h:h + 1]),
                     reads=[p.tok(), dlk.tok()], writes=[KTk.tok(h)])
            for h in range(4):
                for j in range(4):
                    c = 4 * h + j
                    S.op("pe", lambda e, h=h, j=j, c=c: e.matmul(pAT[0:64, h * 64:(h + 1) * 64], lhsT=KTf[:, c, cs], rhs=QT[:, c, cs], start=(j == 0), stop=(j == 3)),
                         reads=[KTf.tok(c), QT.tok(c)], writes=[pAT.tok()])
            S.op("dve", lambda e: e.tensor_tensor(out=ATs[:], in0=pAT[0:64, 0:256], in1=mask4[:], op=ALU.mult), reads=[pAT.tok(), mask4.tok()], writes=[ATs.tok()])

        def stage_b(g, bi, t0, cc):
            A = AT_[g % 2]
            QT = QTs[bi % 2]
            VV, KTk, eb, dl, ATs = A["VV"], A["KTk"], A["eb"], A["dl"], A["ATs"]
            cs = slice(cc * 64, (cc + 1) * 64)
            pd = psm.next()
            for h in range(4):
                S.op("pe", lambda e, h=h: e.matmul(pd[0:64, h:h + 1], lhsT=ATs[:, h * 64:(h + 1) * 64], rhs=VV[:, h, 512:513], start=True, stop=False),
                     reads=[ATs.tok(), VV.tok(h)], writes=[pd.tok()])
                for j in range(4):
                    c = 4 * h + j
                    S.op("pe", lambda e, h=h, j=j, c=c: e.matmul(pd[0:64, h:h + 1], lhsT=QT[:, c, cs], rhs=Cb[:, c, 512:513], start=False, stop=(j == 3)),
                         reads=[QT.tok(c), Cb.tok(c)], writes=[pd.tok()])
            S.op("dve", lambda e: e.tensor_tensor(out=dd[:], in0=pd[0:64, 0:4], in1=eb[:], op=ALU.mult), reads=[pd.tok(), eb.tok()], writes=[dd.tok()])
            S.op("dve", lambda e: e.tensor_scalar(out=scl[:], in0=dd[:], scalar1=-1.0, scalar2=None, op0=ALU.mult), reads=[dd.tok()], writes=[scl.tok()])
            S.op("dve", lambda e: e.tensor_tensor(out=dd[:], in0=dd[:], in1=scl[:], op=ALU.max), reads=[dd.tok(), scl.tok()], writes=[dd.tok()])
            S.op("dve", lambda e: e.tensor_scalar(out=dd[:], in0=dd[:], scalar1=1.0, scalar2=None, op0=ALU.max), reads=[dd.tok()], writes=[dd.tok()])
            S.op("dve", lambda e: e.reciprocal(out=dd[:], in_=dd[:]), reads=[dd.tok()], writes=[dd.tok()])
            S.op("dve", lambda e: e.tensor_tensor(out=scl[:], in0=dd[:], in1=eb[:], op=ALU.mult), reads=[dd.tok(), eb.tok()], writes=[scl.tok()])
            for h in range(4):
                p = pool.next()
                S.op("pe", lambda e, p=p, h=h: e.matmul(p[0:64, :], lhsT=ATs[:, h * 64:(h + 1) * 64], rhs=VV[:, h, 0:512], start=True, stop=False),
                     reads=[ATs.tok(), VV.tok(h)], writes=[p.tok()])
                for j in range(4):
                    c = 4 * h + j
                    S.op("pe", lambda e, p=p, j=j, c=c: e.matmul(p[0:64, :], lhsT=QT[:, c, cs], rhs=Cb[:, c, 0:512], start=False, stop=(j == 3)),
                         reads=[QT.tok(c), Cb.tok(c)], writes=[p.tok()])
                if h % 2 == 0:
                    S.op("act", lambda e, p=p, h=h: e.activation(out=HO[:, h * 512:(h + 1) * 512], in_=p[0:64, :], func=AF.Copy, scale=scl[:, h:h + 1]),
                         reads=[p.tok(), scl.tok()], writes=[HO.tok()])
                else:
                    S.op("dve", lambda e, p=p, h=h: e.tensor_scalar(out=HO[:, h * 512:(h + 1) * 512], in0=p[0:64, :], scalar1=scl[:, h:h + 1], scalar2=None, op0=ALU.mult),
                         reads=[p.tok(), scl.tok()], writes=[HO.tok()])
            r0 = t0 + cc * 64
            outs.append(S.dma("sp", lambda e: e.dma_start(out=hout[r0:r0 + 64, :], in_=HO[:]), "HO", reads=[HO.tok()]))
            for h in range(4):
                for j in range(4):
                    c = 4 * h + j
                    p = pool.next()
                    S.op("pe", lambda e, p=p, h=h, c=c: e.matmul(p[:, :], lhsT=KTk[:, c * 128:(c + 1) * 128], rhs=VV[:, h, 0:512], start=True, stop=True),
                         reads=[KTk.tok(h), VV.tok(h)], writes=[p.tok()])
                    pn = psm.next()
                    S.op("pe", lambda e, pn=pn, h=h, c=c: e.matmul(pn[:, 0:1], lhsT=KTk[:, c * 128:(c + 1) * 128], rhs=VV[:, h, 512:513], start=True, stop=True),
                         reads=[KTk.tok(h), VV.tok(h)], writes=[pn.tok()])
                    S.op("dve", lambda e, p=p, h=h, c=c: e.scalar_tensor_tensor(out=C32[:, c, 0:512], in0=C32[:, c, 0:512], scalar=dl[:, h:h + 1], in1=p[:, :], op0=ALU.mult, op1=ALU.add),
                         reads=[C32.tok(c), dl.tok(), p.tok()], writes=[C32.tok(c)])
                    S.op("dve", lambda e, pn=pn, h=h, c=c: e.scalar_tensor_tensor(out=C32[:, c, 512:513], in0=C32[:, c, 512:513], scalar=dl[:, h:h + 1], in1=pn[:, 0:1], op0=ALU.mult, op1=ALU.add),
                         reads=[C32.tok(c), dl.tok(), pn.tok()], writes=[C32.tok(c)])
                    if c % 2 == 0:
                        S.op("act", lambda e, c=c: e.activation(out=Cb[:, c, 0:513], in_=C32[:, c, 0:513], func=AF.Copy), reads=[C32.tok(c)], writes=[Cb.tok(c)])
                    else:
                        S.op("pool", lambda e, c=c: e.tensor_copy(out=Cb[:, c, 0:513], in_=C32[:, c, 0:513]), reads=[C32.tok(c)], writes=[Cb.tok(c)])

        chunks = []
        for bi, (t0, n, mi) in enumerate(seq_blocks()):
            for cc in range(n // 64):
                chunks.append((bi, t0, n, mi, cc))
        features(*chunks[0][:4])
        stage_a(0, chunks[0][0], chunks[0][1], chunks[0][4])
        for g, (bi, t0, n, mi, cc) in enumerate(chunks):
            if g + 1 < len(chunks):
                nb, nt0, nn, nmi, ncc = chunks[g + 1]
                if ncc == 0:
                    features(nb, nt0, nn, nmi)
                stage_a(g + 1, nb, nt0, ncc)
            stage_b(g, bi, t0, cc)
        S.wait_all_final("sp", outs)
        S.emit()
    return nc


def bd_tiles(w):
    out = np.zeros((128, 16, 128), np.float32)
    for c in range(16):
        for j in range(32):
            out[4 * j:4 * j + 4, c, 4 * j:4 * j + 4] = w[c * 32 + j]
    return out


def prep_p5(inp, mods1, cmods1, b, d):
    ml, mc = mods_split(mods1[b]), mods_split(cmods1)
    cw = inp["m_conv_w"][0]
    z = np.zeros_like(cw[0])
    taps = [cw[0], cw[1], cw[2], cw[3], z] if d == 0 else [z, cw[3], cw[2], cw[1], cw[0]]
    mcv = np.stack([col_layout(v) for v in taps + [inp["m_conv_b"][0]]], 2)
    wgt = inp["m_w_gates"][0, d]
    m = dict(wup=np.ascontiguousarray(inp["m_w_up"][0][:, :2048]),
             modv=modv_pack(inp["norm1_g"][1], ml, mc, "sc1", "sh1"), mcv=np.ascontiguousarray(mcv),
             wbd=np.ascontiguousarray(np.stack([bd_tiles(inp["m_wq"][0]), bd_tiles(inp["m_wk"][0]), bd_tiles(inp["m_wv"][0])], 0)),
             wgt=np.ascontiguousarray(wgt.reshape(48, 128, 8).transpose(1, 0, 2)),
             bg=np.ascontiguousarray(inp["m_b_gates"][0, d][None, :]))
    c = consts_p1()
    m["ones"] = c["ones"]; m["mask4"] = c["mask4"]
    return m


def build_p6():
    nc = bass.Bass("TRN2", target_bir_lowering=False)
    dt = lambda name, shape, kind="ExternalInput", d=F32: nc.dram_tensor(name, list(shape), d, kind=kind).ap()
    xT = dt("xT", [D, NTOK])
    hf_d, hb_d, xc_d = dt("hf", [2048, NTOK]), dt("hb", [2048, NTOK]), dt("xc", [2048, NTOK])
    wz = dt("wz", [D, 2048]); wdn = dt("wdn", [2048, D]); rw_d = dt("rw", [128, 8, 16])
    modv_d = dt("modv", [128, 5, 8]); modv2_d = dt("modv2", [128, 5, 8]); g1_d = dt("g1v", [128, 2, 8])
    sk_d = dt("skip", [128, 16]); ng_d = dt("ng", [128, 16]); ones_d = dt("ones", [128, 128])
    xmT = dt("xmT", [D, NTOK], kind="ExternalOutput")
    t2T = dt("t2T", [D, NTOK], kind="ExternalOutput")
    probs_d = dt("probs", [NTOK, 16], kind="ExternalOutput")
    with ExitStack() as st:
        S = Sched(nc, st)
        pool = PsumPool(S, ["pp0", "pp1", "pp2", "pp3", "pp4", "pp5"])
        ones32 = S.sbuf("ones32", [128, 128], F32); load_const(S, ones_d, ones32, "c_ones")
        modv = S.sbuf("modv", [128, 5, 8], F32); load_const(S, modv_d, modv, "c_modv")
        modv2 = S.sbuf("modv2", [128, 5, 8], F32); load_const(S, modv2_d, modv2, "c_modv2")
        g1v = S.sbuf("g1v", [128, 2, 8], F32); load_const(S, g1_d, g1v, "c_g1")
        skp = S.sbuf("skp", [128, 16], F32); load_const(S, sk_d, skp, "c_sk")
        ng = S.sbuf("ng", [128, 16], F32); load_const(S, ng_d, ng, "c_ng")
        rw = S.sbuf("rw", [128, 8, 16], F32); load_const(S, rw_d, rw, "c_rw")
        epsc = S.sbuf("epsc", [128, 1], F32)
        S.op("dve", lambda e: e.memset(epsc[:], EPS), writes=[epsc.tok()])
        gs1 = S.sbuf("gs1", [128, 2, 8], F32); make_gs(S, modv, gs1, 2)
        gs2 = S.sbuf("gs2", [128, 2, 8], F32); make_gs(S, modv2, gs2, 2)
        sh1 = S.sbuf("sh1", [128, 2, 8], F32); sh2 = S.sbuf("sh2", [128, 2, 8], F32)
        for i in range(2):
            S.op("dve", lambda e, i=i: e.tensor_copy(out=sh1[:, i, :], in_=modv[:, 2 + 2 * i, :]), reads=[modv.tok()], writes=[sh1.tok()])
            S.op("dve", lambda e, i=i: e.tensor_copy(out=sh2[:, i, :], in_=modv2[:, 2 + 2 * i, :]), reads=[modv2.tok()], writes=[sh2.tok()])
        X = [S.sbuf(f"X{i}", [128, 8, BLK], F32) for i in range(2)]
        wzb = S.sbuf("wzb", [128, 8, 2048], BF16); load_cast_weight(S, wz, 2048, wzb, X, "X")
        wdnb = S.sbuf("wdnb", [128, 16, 1024], BF16)
        wdv = wdn.rearrange("(c p) n -> p c n", p=128)
        for i, c0 in enumerate(range(0, 16, 4)):
            for j, n0 in enumerate(range(0, 1024, 512)):
                s_ = X[(2 * i + j) % 2]
                S.dma("sp", lambda e, s_=s_, c0=c0, n0=n0: e.dma_start(out=s_[:, :, :].rearrange("p a (b c) -> p (a b) c", b=2)[:, 0:4, :].rearrange("p a c -> p a c"), in_=wdv[:, c0:c0 + 4, n0:n0 + 512]) if False else
                      e.dma_start(out=s_[:, 0:4, :], in_=wdv[:, c0:c0 + 4, n0:n0 + 256]), f"X{(2 * i + j) % 2}", writes=[s_.tok()])
                S.dma("sp", lambda e, s_=s_, c0=c0, n0=n0: e.dma_start(out=s_[:, 4:8, :], in_=wdv[:, c0:c0 + 4, n0 + 256:n0 + 512]), f"X{(2 * i + j) % 2}", writes=[s_.tok()])
                S.op("dve", lambda e, s_=s_, c0=c0, n0=n0: e.tensor_copy(out=wdnb[:, c0:c0 + 4, n0:n0 + 256], in_=s_[:, 0:4, :]), reads=[s_.tok()], writes=[wdnb.tok()])
                S.op("pool", lambda e, s_=s_, c0=c0, n0=n0: e.tensor_copy(out=wdnb[:, c0:c0 + 4, n0 + 256:n0 + 512], in_=s_[:, 4:8, :]), reads=[s_.tok()], writes=[wdnb.tok()])
        NW = norm_work(S, pool, ones32, epsc)
        RW = router_work(S)
        XN = S.sbuf("XN", [128, 8, BLK], BF16)
        sz = S.sbuf("sz", [128, 16, BLK], F32)
        HF = S.sbuf("HF", [128, 16, BLK], F32); HB = S.sbuf("HB", [128, 16, BLK], F32); XC = S.sbuf("XC", [128, 16, BLK], F32)
        mean = S.sbuf("mean", [128, BLK], F32); hsd = S.sbuf("hsd", [128, BLK], F32); hrs = S.sbuf("hrs", [128, BLK], F32)
        sqh = S.sbuf("sqh", [128, 4, BLK], F32); t1 = S.sbuf("t1", [128, BLK], F32)
        mrg = S.sbuf("mrg", [128, 16, BLK], BF16)
        XM = [S.sbuf(f"XM{i}", [128, 8, BLK], F32) for i in range(2)]
        T2 = S.sbuf("T2", [128, 8, BLK], F32)
        xTv = xT.rearrange("(c p) n -> p c n", p=128)
        v4 = lambda a: a.rearrange("(c p) n -> p c n", p=128)
        outs = []

        def do_block(bi, t0, n, mi):
            b2 = bi % 2
            Xb, XMb = X[b2], XM[b2]
            S.dma("sp", lambda e: e.dma_start(out=Xb[:, :, :n], in_=xTv[:, :, t0:t0 + n]), f"X{b2}", writes=[Xb.tok()])
            for (src, dst, nm) in ((hf_d, HF, "HF"), (hb_d, HB, "HB"), (xc_d, XC, "XC")):
                S.dma("sp", lambda e, src=src, dst=dst: e.dma_start(out=dst[:, :, :n], in_=v4(src)[:, :, t0:t0 + n]), nm, writes=[dst.tok()])
            norm_fm(S, Xb, n, gs1, sh1, mi, XN, NW)
            for c in range(16):
                p = pool.next()
                for kc in range(8):
                    S.op("pe", lambda e, p=p, kc=kc, c=c: e.matmul(p[:, :n], lhsT=wzb[:, kc, c * 128:(c + 1) * 128], rhs=XN[:, kc, :n], start=(kc == 0), stop=(kc == 7)),
                         reads=[wzb.tok(), XN.tok()], writes=[p.tok()])
                S.op("act", lambda e, p=p, c=c: e.activation(out=sz[:, c, :n], in_=p[:, :n], func=AF.Silu), reads=[p.tok()], writes=[sz.tok()])
            S.op("pool", lambda e: e.tensor_tensor(out=HF[:, :, :n], in0=HF[:, :, :n], in1=HB[:, :, :n], op=ALU.add), reads=[HF.tok(), HB.tok()], writes=[HF.tok()])
            for c in range(16):
                S.op("pool", lambda e, c=c: e.tensor_scalar(out=XC[:, c, :n], in0=XC[:, c, :n], scalar1=skp[:, c:c + 1], scalar2=None, op0=ALU.mult), reads=[XC.tok(), skp.tok()], writes=[XC.tok()])
            for h in range(4):
                hs = slice(4 * h, 4 * h + 4)
                p = pool.next()
                for j in range(4):
                    S.op("pe", lambda e, p=p, j=j, h=h: e.matmul(p[:, :n], lhsT=ones32[:, :], rhs=HF[:, 4 * h + j, :n], start=(j == 0), stop=(j == 3)), reads=[ones32.tok(), HF.tok()], writes=[p.tok()])
                S.op("act", lambda e, p=p: e.activation(out=mean[:, :n], in_=p[:, :n], func=AF.Copy, scale=1.0 / 512), reads=[p.tok()], writes=[mean.tok()])
                for j in range(4):
                    S.op("dve", lambda e, j=j, h=h: e.tensor_tensor(out=HF[:, 4 * h + j, :n], in0=HF[:, 4 * h + j, :n], in1=mean[:, :n], op=ALU.subtract), reads=[HF.tok(), mean.tok()], writes=[HF.tok()])
                S.op("act", lambda e, hs=hs: e.activation(out=sqh[:, :, :n], in_=HF[:, hs, :n], func=AF.Square), reads=[HF.tok()], writes=[sqh.tok()])
                p = pool.next()
                for j in range(4):
                    S.op("pe", lambda e, p=p, j=j: e.matmul(p[:, :n], lhsT=ones32[:, :], rhs=sqh[:, j, :n], start=(j == 0), stop=(j == 3)), reads=[ones32.tok(), sqh.tok()], writes=[p.tok()])
                S.op("act", lambda e, p=p: e.activation(out=hsd[:, :n], in_=p[:, :n], func=AF.Sqrt, bias=epsc[:, 0:1], scale=1.0 / 512), reads=[p.tok(), epsc.tok()], writes=[hsd.tok()])
                S.op("dve", lambda e: e.reciprocal(out=hrs[:, :n], in_=hsd[:, :n]), reads=[hsd.tok()], writes=[hrs.tok()])
                for j in range(4):
                    c = 4 * h + j
                    S.op("dve", lambda e, c=c: e.tensor_tensor(out=t1[:, :n], in0=HF[:, c, :n], in1=hrs[:, :n], op=ALU.mult), reads=[HF.tok(), hrs.tok()], writes=[t1.tok()])
                    S.op("dve", lambda e, c=c: e.scalar_tensor_tensor(out=t1[:, :n], in0=t1[:, :n], scalar=ng[:, c:c + 1], in1=XC[:, c, :n], op0=ALU.mult, op1=ALU.add),
                         reads=[t1.tok(), ng.tok(), XC.tok()], writes=[t1.tok()])
                    S.op("pool", lambda e, c=c: e.tensor_tensor(out=mrg[:, c, :n], in0=t1[:, :n], in1=sz[:, c, :n], op=ALU.mult), reads=[t1.tok(), sz.tok()], writes=[mrg.tok()])
            for oc in range(8):
                p = pool.next()
                for c in range(16):
                    S.op("pe", lambda e, p=p, c=c, oc=oc: e.matmul(p[:, :n], lhsT=wdnb[:, c, oc * 128:(oc + 1) * 128], rhs=mrg[:, c, :n], start=(c == 0), stop=(c == 15)),
                         reads=[wdnb.tok(), mrg.tok()], writes=[p.tok()])
                S.op("dve", lambda e, p=p, oc=oc: e.scalar_tensor_tensor(out=XMb[:, oc, :n], in0=p[:, :n], scalar=g1v[:, mi, oc:oc + 1], in1=Xb[:, oc, :n], op0=ALU.mult, op1=ALU.add),
                     reads=[p.tok(), g1v.tok(), Xb.tok()], writes=[XMb.tok()])
            outs.append(S.dma("sp", lambda e: e.dma_start(out=v4(xmT)[:, :, t0:t0 + n], in_=XMb[:, :, :n]), f"XM{b2}", reads=[XMb.tok()]))
            norm_fm(S, XMb, n, gs2, sh2, mi, T2, NW)
            outs.append(S.dma("sp", lambda e: e.dma_start(out=v4(t2T)[:, :, t0:t0 + n], in_=T2[:, :, :n]), "T2", reads=[T2.tok()]))
            router_softmax(S, T2, n, t0, rw, probs_d, pool, RW, outs, "p6")

        for bi, (t0, n, mi) in enumerate(tok_blocks()):
            do_block(bi, t0, n, mi)
        S.wait_all_final("sp", outs)
        S.emit()
    return nc


def prep_p6(inp, mods1, cmods1, b, half, x_lat, x_ctx, hf, hb, xc):
    ml, mc = mods_split(mods1[b]), mods_split(cmods1)
    T = lambda lat, ctx: np.ascontiguousarray(half_tokens(lat, ctx, half).T)
    return dict(xT=T(x_lat, x_ctx), hf=T(hf["lat"], hf["ctx"]), hb=T(hb["lat"], hb["ctx"]), xc=T(xc["lat"], xc["ctx"]),
                wz=np.ascontiguousarray(inp["m_w_up"][0][:, 2048:]), wdn=np.ascontiguousarray(inp["m_w_down"][0]),
                rw=np.ascontiguousarray(inp["router_w"][1].reshape(8, 128, 16).transpose(1, 0, 2)),
                modv=modv_pack(inp["norm1_g"][1], ml, mc, "sc1", "sh1"), modv2=modv_pack(inp["norm2_g"][1], ml, mc, "sc2", "sh2"),
                g1v=np.ascontiguousarray(np.stack([col_layout(ml["g1"]), col_layout(mc["g1"])], 1)),
                skip=col_layout(inp["m_skip"][0]), ng=col_layout(inp["m_norm_g"][0]), ones=np.ones((128, 128), np.float32))


def build_pw():
    nc = bass.Bass("TRN2", target_bir_lowering=False)
    src = nc.dram_tensor("wsrc", [12, D, 1024], F32, kind="ExternalInput").ap()
    dst = nc.dram_tensor("wdst", [12, D, 1024], BF16, kind="ExternalOutput").ap()
    with ExitStack() as st:
        S = Sched(nc, st)
        stg = [S.sbuf(f"stg{i}", [128, 8, 512], F32) for i in range(3)]
        ob = [S.sbuf(f"ob{i}", [128, 8, 512], BF16) for i in range(3)]
        outs = []
        k = 0
        for m in range(12):
            sv = src[m].rearrange("(c p) n -> p c n", p=128)
            dv = dst[m].rearrange("(c p) n -> p c n", p=128)
            for c0 in range(0, 1024, 512):
                s_, o_ = stg[k % 3], ob[k % 3]
                S.dma("sp", lambda e, s_=s_, sv=sv, c0=c0: e.dma_start(out=s_[:], in_=sv[:, :, c0:c0 + 512]), f"stg{k % 3}", writes=[s_.tok()])
                eng = ("dve", "act", "pool")[k % 3]
                if eng == "act":
                    S.op("act", lambda e, s_=s_, o_=o_: e.activation(out=o_[:], in_=s_[:], func=AF.Copy), reads=[s_.tok()], writes=[o_.tok()])
                else:
                    S.op(eng, lambda e, s_=s_, o_=o_: e.tensor_copy(out=o_[:], in_=s_[:]), reads=[s_.tok()], writes=[o_.tok()])
                outs.append(S.dma("sp", lambda e, o_=o_, dv=dv, c0=c0: e.dma_start(out=dv[:, :, c0:c0 + 512], in_=o_[:]), f"ob{k % 3}", reads=[o_.tok()]))
                k += 1
        S.wait_all_final("sp", outs)
        S.emit()
    return nc


def run_pw(inp):
    names = ["exp_w_gate", "exp_w_up", "exp_w_down"]
    maps = []
    for i in range(8):
        mats = [inp[nm][l, 2 * i + j] for l in range(2) for j in range(2) for nm in names]
        maps.append(dict(wsrc=np.ascontiguousarray(np.stack(mats, 0))))
    res = _run(build_pw(), maps)
    out = [dict(), dict()]
    for l in range(2):
        for mi, nm in enumerate(names):
            out[l][nm] = np.ascontiguousarray(np.concatenate(
                [np.stack([res[i]["wdst"][(l * 2 + j) * 3 + mi] for j in range(2)], 0) for i in range(8)], 0))
    return out


def _run(nc, maps):
    return run_bass_kernel_spmd(nc, maps, core_ids=list(range(len(maps)))).results


def _moe_layer(inp, wq_l, mods_l, cmods_l, res_merge, final):
    maps = []
    for b in range(4):
        pr0, pr1 = res_merge[2 * b]["probs"], res_merge[2 * b + 1]["probs"]
        pl = np.concatenate([pr0[:4096], pr1[:4096]], 0)
        pc = np.concatenate([pr0[4096:], pr1[4096:]], 0)
        for half in range(2):
            r = res_merge[2 * b + half]
            maps.append(prep_p4(inp, wq_l, mods_l, cmods_l, b, half, np.ascontiguousarray(r["t2T"]), np.ascontiguousarray(r["xmT"]), pl, pc, r["probs"]))
    res = _run(build_p4(final), maps)
    return [np.ascontiguousarray(r["xoT"].T) for r in res]


def kernel(**inp):
    inp = {k: np.asarray(v) for k, v in inp.items()}
    mods = run_p0(inp)
    wq = run_pw(inp)
    mods0, cmods0, mods1, cmods1 = mods[0, :4], mods[0, 4], mods[1, :4], mods[1, 4]
    maps = [prep_p1(inp, mods0, cmods0, b, d) for b in range(4) for d in range(2)]
    r1 = _run(build_p1(), maps)
    del maps
    maps = []
    for b in range(4):
        og, hl = [], []
        for d in range(2):
            r = r1[2 * b + d]
            c, l = unseq(r["og"], d); og.append(dict(ctx=c, lat=l))
            c, l = unseq(r["hlT"].T, d); hl.append(dict(ctx=c, lat=l))
        for half in range(2):
            maps.append(prep_p2(inp, mods0, cmods0, b, half, og[0], og[1], hl[0], hl[1]))
    r2 = _run(build_p2(), maps)
    del maps, r1
    xo = _moe_layer(inp, wq[0], mods0, cmods0, r2, False)
    del r2
    x1 = [np.concatenate([xo[2 * b][:4096], xo[2 * b + 1][:4096]], 0) for b in range(4)]
    c1 = [np.concatenate([xo[2 * b][4096:], xo[2 * b + 1][4096:]], 0) for b in range(4)]
    maps = []
    for b in range(4):
        for d in range(2):
            m = prep_p5(inp, mods1, cmods1, b, d)
            m["xT"] = seq_T(c1[b], x1[b], d)
            maps.append(m)
    r5 = _run(build_p5(), maps)
    del maps
    maps = []
    for b in range(4):
        c, l = unseq(r5[2 * b]["hout"], 0); hf = dict(ctx=c, lat=l)
        c, l = unseq(r5[2 * b + 1]["hout"], 1); hb = dict(ctx=c, lat=l)
        c, l = unseq(r5[2 * b]["xcT"].T, 0); xc = dict(ctx=c, lat=l)
        for half in range(2):
            maps.append(prep_p6(inp, mods1, cmods1, b, half, x1[b], c1[b], hf, hb, xc))
    r6 = _run(build_p6(), maps)
    del maps, r5
    xo = _moe_layer(inp, wq[1], mods1, cmods1, r6, True)
    out = np.stack([np.concatenate([xo[2 * b][:4096], xo[2 * b + 1][:4096]], 0) for b in range(4)], 0)
    return np.ascontiguousarray(out.astype(np.float32))
```
